# Optimizing a Trainium2 kernel written in Bass

```python
import jax
import jax.numpy as jnp
from jax import lax
import numpy as np

D_MODEL = 1024
BATCH = 2
SEQ = 8192
DEPTH = 4

N_MIXERS = 3
N_MLA_LAYERS = (DEPTH + N_MIXERS - 1) // N_MIXERS
N_MLSTM_LAYERS = (DEPTH + N_MIXERS - 2) // N_MIXERS
N_RWKV_LAYERS = DEPTH // N_MIXERS

RMS_EPS = 1e-6
FFN_HIDDEN = 2816
MLA_HEADS = 8
MLA_Q_RANK = 512
MLA_KV_RANK = 256
MLA_NOPE = 128
MLA_ROPE = 64
MLA_V = 128
ROPE_THETA = 10000.0
Q_BLOCK = 128
MLSTM_HEADS = 8
MLSTM_QK = 64
MLSTM_V = 128
MLSTM_CONV = 4
MLSTM_CHUNK = 64
RWKV_HEAD = 64
RWKV_HEADS = D_MODEL // RWKV_HEAD
RWKV_DECAY_LORA = 64
RWKV_A_LORA = 64
RWKV_GATE_LORA = 128
RWKV_GN_EPS = 64e-5

kernel_name = 'hybrid_mla_mlstm_rwkv7_macaron'

F32 = jnp.float32


def rmsnorm(x, g, eps=RMS_EPS):
    xf = x.astype(F32)
    y = xf * lax.rsqrt(jnp.mean(xf * xf, axis=-1, keepdims=True) + eps)
    return (y * g.astype(F32)).astype(x.dtype)


def swiglu(h, w_gate, w_up, w_down):
    return (jax.nn.silu(h @ w_gate) * (h @ w_up)) @ w_down


def rope(t, cos, sin):
    t1, t2 = jnp.split(t, 2, axis=-1)
    out = jnp.concatenate([t1 * cos - t2 * sin, t1 * sin + t2 * cos], axis=-1)
    return out.astype(t.dtype)


def causal_block_attention(q, k, v, scale):
    B, S, H, Dk = q.shape
    Dv = v.shape[-1]
    nb = S // Q_BLOCK
    qb = q.reshape(B, nb, Q_BLOCK, H, Dk).transpose(1, 0, 3, 2, 4)
    kpos = jnp.arange(S)

    def one_block(args):
        q_blk, bi = args
        s = jnp.einsum('bhqd,bkhd->bhqk', q_blk, k).astype(F32) * scale
        qpos = bi * Q_BLOCK + jnp.arange(Q_BLOCK)
        mask = kpos[None, :] <= qpos[:, None]
        s = jnp.where(mask[None, None], s, -jnp.inf)
        p = jax.nn.softmax(s, axis=-1).astype(v.dtype)
        return jnp.einsum('bhqk,bkhd->bqhd', p, v)

    out = lax.map(one_block, (qb, jnp.arange(nb)))
    return out.transpose(1, 0, 2, 3, 4).reshape(B, S, H, Dv)


def mla(h, positions, w_a, q_norm, w_qb, kv_norm, w_kvb, w_o):
    B, S, _ = h.shape
    H = MLA_HEADS
    lat = h @ w_a
    c_q = lat[..., :MLA_Q_RANK]
    c_kv = lat[..., MLA_Q_RANK:MLA_Q_RANK + MLA_KV_RANK]
    k_pe = lat[..., MLA_Q_RANK + MLA_KV_RANK:]
    q = (rmsnorm(c_q, q_norm) @ w_qb).reshape(B, S, H, MLA_NOPE + MLA_ROPE)
    kv = (rmsnorm(c_kv, kv_norm) @ w_kvb).reshape(B, S, H, MLA_NOPE + MLA_V)
    q_nope, q_pe = q[..., :MLA_NOPE], q[..., MLA_NOPE:]
    k_nope, v = kv[..., :MLA_NOPE], kv[..., MLA_NOPE:]
    inv_freq = 1.0 / (ROPE_THETA ** (jnp.arange(0, MLA_ROPE, 2, dtype=F32) / MLA_ROPE))
    ang = positions.astype(F32)[..., None] * inv_freq
    cos, sin = jnp.cos(ang), jnp.sin(ang)
    q_pe = rope(q_pe, cos[:, :, None], sin[:, :, None])
    k_pe = rope(k_pe, cos, sin)
    q = jnp.concatenate([q_nope, q_pe], axis=-1)
    k = jnp.concatenate([k_nope, jnp.broadcast_to(k_pe[:, :, None], (B, S, H, MLA_ROPE))], axis=-1)
    o = causal_block_attention(q, k, v, (MLA_NOPE + MLA_ROPE) ** -0.5)
    return o.reshape(B, S, H * MLA_V) @ w_o


def causal_depthwise_conv(x, w, b):
    K, C = w.shape
    y = lax.conv_general_dilated(x, w[:, None, :].astype(x.dtype), window_strides=(1,),
                                 padding=((K - 1, 0),), dimension_numbers=('NWC', 'WIO', 'NWC'),
                                 feature_group_count=C)
    return y + b


def mlstm_chunkwise(q, k, v, i_pre, log_f):
    B, S, H, DK = q.shape
    DV = v.shape[-1]
    L = MLSTM_CHUNK
    nc = S // L
    to_c = lambda t: t.astype(F32).reshape(B, nc, L, H, -1).transpose(1, 0, 3, 2, 4)
    to_cg = lambda t: t.astype(F32).reshape(B, nc, L, H).transpose(1, 0, 3, 2)
    causal = jnp.tril(jnp.ones((L, L), dtype=bool))

    def step(carry, inp):
        C, n, m = carry
        q_, k_, v_, i_, lf = inp
        Fc = jnp.cumsum(lf, axis=-1)
        D = Fc[..., :, None] - Fc[..., None, :] + i_[..., None, :]
        D = jnp.where(causal, D, -jnp.inf)
        inter = Fc + m[..., None]
        m_t = jnp.maximum(inter, jnp.max(D, axis=-1))
        Dw = jnp.exp(D - m_t[..., None])
        inter_w = jnp.exp(inter - m_t)
        s = jnp.einsum('bhtd,bhsd->bhts', q_, k_) * Dw
        num = jnp.einsum('bhts,bhsv->bhtv', s, v_) + inter_w[..., None] * jnp.einsum('bhtd,bhdv->bhtv', q_, C)
        den = jnp.sum(s, axis=-1) + inter_w * jnp.einsum('bhtd,bhd->bht', q_, n)
        h = num / jnp.maximum(jnp.abs(den), jnp.exp(-m_t))[..., None]
        m_new = m_t[..., -1]
        carry_decay = jnp.exp(Fc[..., -1] + m - m_new)
        w_s = jnp.exp(Fc[..., -1:] - Fc + i_ - m_new[..., None])
        C_new = carry_decay[..., None, None] * C + jnp.einsum('bhs,bhsd,bhsv->bhdv', w_s, k_, v_)
        n_new = carry_decay[..., None] * n + jnp.einsum('bhs,bhsd->bhd', w_s, k_)
        return (C_new, n_new, m_new), h

    init = (jnp.zeros((B, H, DK, DV), F32), jnp.zeros((B, H, DK), F32), jnp.zeros((B, H), F32))
    _, hs = lax.scan(step, init, (to_c(q), to_c(k), to_c(v), to_cg(i_pre), to_cg(log_f)))
    return hs.transpose(1, 0, 3, 2, 4).reshape(B, S, H, DV)


def mlstm(h, w_in, b_if, conv_w, conv_b, out_norm, w_o):
    B, S, _ = h.shape
    H, DK, DV = MLSTM_HEADS, MLSTM_QK, MLSTM_V
    QK = 2 * H * DK
    V = H * DV
    p = h @ w_in
    qk = jax.nn.silu(causal_depthwise_conv(p[..., :QK], conv_w, conv_b))
    v = p[..., QK:QK + V].reshape(B, S, H, DV)
    o = p[..., QK + V:QK + 2 * V]
    gates = p[..., QK + 2 * V:].astype(F32) + b_if.astype(F32)
    q = qk[..., :H * DK].reshape(B, S, H, DK)
    k = qk[..., H * DK:].reshape(B, S, H, DK) * (DK ** -0.5)
    i_pre = gates[..., :H]
    log_f = jax.nn.log_sigmoid(gates[..., H:])
    hid = mlstm_chunkwise(q, k, v, i_pre, log_f)
    hid = hid * lax.rsqrt(jnp.mean(hid * hid, axis=-1, keepdims=True) + RMS_EPS)
    hid = hid.reshape(B, S, V) * out_norm.astype(F32) * jax.nn.sigmoid(o.astype(F32))
    return hid.astype(h.dtype) @ w_o


def rwkv7_recurrence(r, decay, k, v, a, b):
    B, S, H, N = r.shape

    def step(state, inp):
        r_, w_, k_, v_, a_, b_ = inp
        sa = jnp.einsum('bhij,bhj->bhi', state, a_)
        state = (state * w_[:, :, None, :] + sa[..., :, None] * b_[:, :, None, :]
                 + v_[..., :, None] * k_[:, :, None, :])
        return state, jnp.einsum('bhij,bhj->bhi', state, r_)

    tm = lambda t: jnp.moveaxis(t, 1, 0)
    _, ys = lax.scan(step, jnp.zeros((B, H, N, N), F32), (tm(r), tm(decay), tm(k), tm(v), tm(a), tm(b)))
    return jnp.moveaxis(ys, 0, 1)


def rwkv7(h, mu, w_r, w_k, w_v, w0, w1, w2, a0, a1, a2, g1, g2, k_k, k_a, r_k, ln_w, ln_b, w_o):
    B, S, D = h.shape
    H, N = RWKV_HEADS, RWKV_HEAD
    xx = jnp.pad(h, ((0, 0), (1, 0), (0, 0)))[:, :S] - h
    xr, xw, xk, xv, xa, xg = [h + xx * mu[j] for j in range(6)]
    r = (xr @ w_r).astype(F32)
    k = (xk @ w_k).astype(F32)
    v = (xv @ w_v).astype(F32)
    w_log = -jax.nn.softplus(-(w0 + jnp.tanh(xw @ w1) @ w2).astype(F32)) - 0.5
    decay = jnp.exp(-jnp.exp(w_log))
    a = jax.nn.sigmoid((a0 + (xa @ a1) @ a2).astype(F32))
    g = (jax.nn.sigmoid(xg @ g1) @ g2).astype(F32)
    kk = (k * k_k.astype(F32)).reshape(B, S, H, N)
    kk = kk / jnp.maximum(jnp.sqrt(jnp.sum(kk * kk, axis=-1, keepdims=True)), 1e-12)
    k = k * (1.0 + (a - 1.0) * k_a.astype(F32))
    rh, kh, vh = r.reshape(B, S, H, N), k.reshape(B, S, H, N), v.reshape(B, S, H, N)
    ah = a.reshape(B, S, H, N)
    y = rwkv7_recurrence(rh, decay.reshape(B, S, H, N), kh, vh, -kk, kk * ah)
    mean = jnp.mean(y, axis=-1, keepdims=True)
    var = jnp.mean(jnp.square(y - mean), axis=-1, keepdims=True)
    y = ((y - mean) * lax.rsqrt(var + RWKV_GN_EPS)).reshape(B, S, D) * ln_w.astype(F32) + ln_b.astype(F32)
    bonus = jnp.sum(rh * kh * r_k.astype(F32), axis=-1, keepdims=True) * vh
    y = y + bonus.reshape(B, S, D)
    return (y * g).astype(h.dtype) @ w_o


def setup_inputs(seed: int = 0) -> dict:
    key = jax.random.key(seed)
    keys = list(jax.random.split(key, 48))
    nk = lambda: keys.pop()
    D, F = D_MODEL, FFN_HIDDEN
    nA, nB, nC = N_MLA_LAYERS, N_MLSTM_LAYERS, N_RWKV_LAYERS

    def dense(shape, fan_in, scale=1.0):
        return jax.random.normal(nk(), shape, F32) * (scale * fan_in ** -0.5)

    def gain(shape):
        return 1.0 + 0.02 * jax.random.normal(nk(), shape, F32)

    def small(shape, s=0.02, c=0.0):
        return c + s * jax.random.normal(nk(), shape, F32)

    x = jax.random.normal(nk(), (BATCH, SEQ, D), F32)
    positions = (jnp.arange(SEQ, dtype=jnp.int32)[None, :]
                 + jax.random.randint(nk(), (BATCH, 1), 0, 4096, dtype=jnp.int32))
    Hm, DK, DV = MLSTM_HEADS, MLSTM_QK, MLSTM_V
    b_if = jnp.concatenate([small((nB, Hm), 0.1),
                            jnp.linspace(3.0, 6.0, Hm, dtype=F32)[None] + small((nB, Hm), 0.1)], axis=-1)
    return {
        'x': x,
        'positions': positions,
        'ffn_norm': gain((DEPTH, 2, D)),
        'ffn_w_gate': dense((DEPTH, 2, D, F), D),
        'ffn_w_up': dense((DEPTH, 2, D, F), D),
        'ffn_w_down': dense((DEPTH, 2, F, D), F),
        'mix_norm': gain((DEPTH, D)),
        'final_norm': gain((D,)),
        'mla_w_a': dense((nA, D, MLA_Q_RANK + MLA_KV_RANK + MLA_ROPE), D),
        'mla_q_norm': gain((nA, MLA_Q_RANK)),
        'mla_w_qb': dense((nA, MLA_Q_RANK, MLA_HEADS * (MLA_NOPE + MLA_ROPE)), MLA_Q_RANK),
        'mla_kv_norm': gain((nA, MLA_KV_RANK)),
        'mla_w_kvb': dense((nA, MLA_KV_RANK, MLA_HEADS * (MLA_NOPE + MLA_V)), MLA_KV_RANK),
        'mla_w_o': dense((nA, MLA_HEADS * MLA_V, D), MLA_HEADS * MLA_V),
        'ml_w_in': dense((nB, D, 2 * Hm * DK + 2 * Hm * DV + 2 * Hm), D),
        'ml_b_if': b_if,
        'ml_conv_w': dense((nB, MLSTM_CONV, 2 * Hm * DK), MLSTM_CONV),
        'ml_conv_b': small((nB, 2 * Hm * DK)),
        'ml_out_norm': gain((nB, Hm * DV)),
        'ml_w_o': dense((nB, Hm * DV, D), Hm * DV),
        'rw_mu': jax.random.uniform(nk(), (nC, 6, D), F32),
        'rw_w_r': dense((nC, D, D), D),
        'rw_w_k': dense((nC, D, D), D),
        'rw_w_v': dense((nC, D, D), D),
        'rw_w0': jnp.linspace(-6.0, 1.0, D, dtype=F32)[None] + small((nC, D), 0.1),
        'rw_w1': dense((nC, D, RWKV_DECAY_LORA), D),
        'rw_w2': dense((nC, RWKV_DECAY_LORA, D), RWKV_DECAY_LORA, 0.1),
        'rw_a0': small((nC, D), 0.1),
        'rw_a1': dense((nC, D, RWKV_A_LORA), D),
        'rw_a2': dense((nC, RWKV_A_LORA, D), RWKV_A_LORA, 0.1),
        'rw_g1': dense((nC, D, RWKV_GATE_LORA), D),
        'rw_g2': dense((nC, RWKV_GATE_LORA, D), RWKV_GATE_LORA),
        'rw_k_k': small((nC, D), 0.05, 0.85),
        'rw_k_a': small((nC, D), 0.05, 1.0),
        'rw_r_k': small((nC, RWKV_HEADS, RWKV_HEAD), 0.1, -0.04),
        'rw_ln_w': gain((nC, D)),
        'rw_ln_b': small((nC, D)),
        'rw_w_o': dense((nC, D, D), D),
    }


def reference(x, positions, ffn_norm, ffn_w_gate, ffn_w_up, ffn_w_down, mix_norm, final_norm,
              mla_w_a, mla_q_norm, mla_w_qb, mla_kv_norm, mla_w_kvb, mla_w_o,
              ml_w_in, ml_b_if, ml_conv_w, ml_conv_b, ml_out_norm, ml_w_o,
              rw_mu, rw_w_r, rw_w_k, rw_w_v, rw_w0, rw_w1, rw_w2, rw_a0, rw_a1, rw_a2,
              rw_g1, rw_g2, rw_k_k, rw_k_a, rw_r_k, rw_ln_w, rw_ln_b, rw_w_o):
    for layer in range(DEPTH):
        x = x + 0.5 * swiglu(rmsnorm(x, ffn_norm[layer, 0]), ffn_w_gate[layer, 0],
                             ffn_w_up[layer, 0], ffn_w_down[layer, 0])
        h = rmsnorm(x, mix_norm[layer])
        kind = layer % N_MIXERS
        j = layer // N_MIXERS
        if kind == 0:
            y = mla(h, positions, mla_w_a[j], mla_q_norm[j], mla_w_qb[j], mla_kv_norm[j],
                    mla_w_kvb[j], mla_w_o[j])
        elif kind == 1:
            y = mlstm(h, ml_w_in[j], ml_b_if[j], ml_conv_w[j], ml_conv_b[j], ml_out_norm[j], ml_w_o[j])
        else:
            y = rwkv7(h, rw_mu[j], rw_w_r[j], rw_w_k[j], rw_w_v[j], rw_w0[j], rw_w1[j], rw_w2[j],
                      rw_a0[j], rw_a1[j], rw_a2[j], rw_g1[j], rw_g2[j], rw_k_k[j], rw_k_a[j],
                      rw_r_k[j], rw_ln_w[j], rw_ln_b[j], rw_w_o[j])
        x = x + y.astype(x.dtype)
        x = x + 0.5 * swiglu(rmsnorm(x, ffn_norm[layer, 1]), ffn_w_gate[layer, 1],
                             ffn_w_up[layer, 1], ffn_w_down[layer, 1])
    return rmsnorm(x, final_norm)
```

```python
import math
from concourse.bass_utils import run_bass_kernel_spmd
import numpy as np
import concourse.bass as bass
import concourse.mybir as mybir

F32 = mybir.dt.float32
BF16 = mybir.dt.bfloat16
I32 = mybir.dt.int32
ALU = mybir.AluOpType
AF = mybir.ActivationFunctionType
AX = mybir.AxisListType

SAME_ENG_SYNC = True


class _Op:
    __slots__ = ("eng", "fn", "deps", "dma", "marked", "val", "sem", "inc", "phase")


class _Rec:
    def __init__(self):
        self.calls = []

    def __getattr__(self, name):
        def f(*a, **k):
            self.calls.append((name, a, k))
            return self
        return f


def _replay(calls):
    def fn(eng):
        ins = None
        for name, a, k in calls:
            ins = getattr(eng, name)(*a, **k)
        return ins
    return fn


class Prog:
    ENG = ("pe", "act", "dve", "pool", "sp")

    def __init__(self, nc):
        self.nc = nc
        self.ops = {e: [] for e in self.ENG}
        self.res_w = {}
        self.res_r = {}
        self.out_dmas = []
        self.all = []
        self.phase = 0
        self.bar_deps = []
        self.passed = set()
        self.last_compute = {}
        self.last_dma = {}

    def op(self, eng, fn, r=(), w=(), dma=None, out=False, inc=None):
        o = _Op()
        o.eng = eng
        rec = _Rec()
        fn(rec)
        o.fn = _replay(rec.calls)
        o.dma = dma
        o.inc = inc if inc is not None else (16 if dma is not None else 1)
        o.marked = dma is not None
        o.val = 0
        o.sem = None
        deps = []
        for x in r:
            d = self.res_w.get(x)
            if d is not None:
                deps.append((d, 0))
        for x in w:
            d = self.res_w.get(x)
            if d is not None:
                deps.append((d, 0))
            for d in self.res_r.get(x, ()):
                deps.append((d, 1))
        for x in w:
            self.res_w[x] = o
            self.res_r[x] = []
        for x in r:
            if x not in w:
                self.res_r.setdefault(x, []).append(o)
        if eng not in self.passed:
            self.passed.add(eng)
            deps.extend((d, 0) for d in self.bar_deps)
        o.deps = deps
        o.phase = self.phase
        if dma is not None:
            self.last_dma[dma] = o
        else:
            self.last_compute[eng] = o
        self.ops[eng].append(o)
        self.all.append(o)
        if out:
            self.out_dmas.append(o)
        return o

    def barrier(self):
        self.bar_deps = list(self.last_compute.values()) + list(self.last_dma.values())
        self.passed = set()
        self.res_w = {}
        self.res_r = {}
        self.phase += 1

    def pe(self, fn, r=(), w=()):
        return self.op("pe", fn, r, w)

    def act(self, fn, r=(), w=()):
        return self.op("act", fn, r, w)

    def dve(self, fn, r=(), w=()):
        return self.op("dve", fn, r, w)

    def pool(self, fn, r=(), w=()):
        return self.op("pool", fn, r, w)

    def dma(self, q, out_ap, in_ap, key, r=(), w=(), out=False, **kw):
        return self.op(q, lambda e: e.dma_start(out=out_ap, in_=in_ap, **kw), r, w, dma=key, out=out)

    def _need(self, o, d, kind):
        if d is o:
            return False
        if d.dma is not None:
            return True
        if d.eng != o.eng:
            return True
        if o.eng == "pe":
            return False
        if kind == 1:
            return False
        return SAME_ENG_SYNC

    def emit(self):
        nc = self.nc
        fin = _Op()
        fin.eng = "sp"; fin.fn = None; fin.dma = None; fin.marked = False; fin.val = 0; fin.sem = None; fin.inc = 1
        fin.deps = [(d, 0) for d in self.out_dmas] + [(d, 0) for d in self.last_dma.values()]
        fin.phase = self.phase
        self.ops["sp"].append(fin)
        for e in self.ENG:
            for o in self.ops[e]:
                o.deps = [(d, k) for (d, k) in o.deps if self._need(o, d, k)]
                for d, k in o.deps:
                    d.marked = True
        import contextlib
        stack = contextlib.ExitStack()
        esem = {}
        dsem = {}
        dcnt = {}
        cnt = {}
        for e in ("all",):
            for o in self.all:
                e = o.eng
                if o.dma is not None:
                    if o.dma not in dsem:
                        dsem[o.dma] = stack.enter_context(nc.semaphore("d_" + o.dma))
                        dcnt[o.dma] = 0
                    dcnt[o.dma] += o.inc
                    o.sem = dsem[o.dma]
                    o.val = dcnt[o.dma]
                elif o.marked:
                    ek = (e, o.phase // 4)
                    if ek not in esem:
                        esem[ek] = stack.enter_context(nc.semaphore("s_%s_%d" % ek))
                        cnt[ek] = 0
                    cnt[ek] += 1
                    o.sem = esem[ek]
                    o.val = cnt[ek]
        self.nsem = len(dsem) + len(esem)
        engobj = {"pe": "tensor", "act": "scalar", "dve": "vector", "pool": "gpsimd", "sp": "sync"}
        ops = self.ops

        def run(ename, eng):
            seen = {}
            for o in ops[ename]:
                waits = {}
                for d, k in o.deps:
                    key = d.sem.name
                    if seen.get(key, 0) < d.val:
                        seen[key] = d.val
                        waits[key] = (d.sem, d.val)
                for key, (s, v) in waits.items():
                    eng.wait_ge(s, v)
                if o.fn is None:
                    continue
                ins = o.fn(eng)
                if o.marked:
                    ins.then_inc(o.sem, o.inc)

        with nc.Block() as block:
            @block.tensor
            def _(e):
                run("pe", e)

            @block.scalar
            def _(e):
                run("act", e)

            @block.vector
            def _(e):
                run("dve", e)

            @block.gpsimd
            def _(e):
                run("pool", e)

            @block.sync
            def _(e):
                run("sp", e)
        stack.close()


class _T:
    def __init__(self, ap):
        self._ap = ap

    def ap(self):
        return self._ap


class Ctx:
    def __init__(self, nc, arena_f32=51000):
        self.nc = nc
        self.P = Prog(nc)
        self.big = nc.alloc_sbuf_tensor("arena", [128, arena_f32], F32).ap()
        self.cap = arena_f32
        self.off = 0
        self.ps = [nc.alloc_psum_tensor("ps%d" % i, [128, 512], F32).ap() for i in range(8)]

    def reset(self):
        self.off = 0

    def sb(self, name, shape, dtype):
        p = shape[0]
        n = 1
        for d in shape[1:]:
            n *= d
        esz = 4 if dtype in (F32, I32, mybir.dt.uint32) else 2
        n4 = (n * esz + 3) // 4
        n4 = (n4 + 7) // 8 * 8
        assert self.off + n4 <= self.cap, ("SBUF arena overflow", name, self.off, n4, self.cap)
        ap = self.big[0:p, self.off:self.off + n4]
        self.off += n4
        if dtype != F32:
            ap = ap.bitcast(dtype)
        ap = ap[:, 0:n]
        if len(shape) == 3:
            ap = ap.rearrange("p (a b) -> p a b", a=shape[1])
        elif len(shape) != 2:
            raise ValueError(shape)
        return _T(ap)


D = 1024
FF = 2816
T = 2048
TT = 512
NTT = T // TT
FG = 256
NFG = FF // FG
EPS = 1e-6


def emit_tp(CX, io, n_ffn, do_wo, do_mix, do_final):
    nc = CX.nc
    P = CX.P
    x_in = io["x_in"]
    if do_wo:
        oall = io["oall"]
        wo_in = io["wo"]
        idx_in = io["idx"]
    ffw = io["ffw"]
    n_gain = n_ffn + (1 if (do_mix or do_final) else 0)
    g_in = io["gains"]
    if do_final:
        y_out = io["y_out"]
    else:
        x_out = io["x_out"]
    if do_mix:
        h_out = io["h_out"]

    sb = CX.sb
    x = sb("x", [128, 8, T], F32).ap()
    h = sb("h", [128, 8, T], BF16).ap()
    gains = sb("gains_sb", [128, max(n_gain, 1) * 8], F32).ap()
    ones = sb("ones", [128, 128], BF16).ap()
    sq = sb("sq", [128, 8, TT], BF16).ap()
    sd = sb("sd", [128, TT], F32).ap()
    rstd = sb("rstd", [128, TT], F32).ap()
    wst = [[sb(f"wst{i}_{j}", [128, 8 * FG], F32).ap() for j in range(3)] for i in range(2)]
    wbf = [[sb(f"wbf{i}_{j}", [128, 8 * FG], BF16).ap() for j in range(3)] for i in range(2)]
    a_sb = [sb(f"a{i}", [128, TT], F32).ap() for i in range(2)]
    z = [sb(f"z{i}", [128, 2, TT], BF16).ap() for i in range(2)]
    ps = CX.ps
    idx = sb("idx", [128, 8], mybir.dt.uint32).ap()

    P.pool(lambda e: e.memset(ones, 1.0), w=["ones"])
    P.dma("sp", gains, g_in, "gains", w=["gains"])
    for tt in range(NTT):
        P.dma("sp", x[:, :, tt * TT:(tt + 1) * TT], x_in[:, :, tt * TT:(tt + 1) * TT], f"x{tt}", w=[f"x{tt}"])

    def tsl(tt):
        return slice(tt * TT, (tt + 1) * TT)

    def norm_stats(tt):
        P.act(lambda e: e.activation(out=sq, in_=x[:, :, tsl(tt)], func=AF.Square), r=[f"x{tt}"], w=["sq"])
        for dc in range(8):
            P.pe(lambda e, dc=dc: e.matmul(ps[6], lhsT=ones, rhs=sq[:, dc, :], start=(dc == 0), stop=(dc == 7)),
                 r=["ones", "sq"], w=["ps6"])
        P.act(lambda e: e.activation(out=sd, in_=ps[6], func=AF.Sqrt, bias=EPS, scale=1.0 / D), r=["ps6"], w=["sd"])
        P.dve(lambda e: e.reciprocal(out=rstd, in_=sd), r=["sd"], w=["rstd"])

    def norm_to_h(tt, gi):
        norm_stats(tt)
        for dc in range(8):
            P.dve(lambda e, dc=dc: e.scalar_tensor_tensor(
                out=h[:, dc, tsl(tt)], in0=x[:, dc, tsl(tt)], scalar=gains[:, gi * 8 + dc:gi * 8 + dc + 1],
                in1=rstd, op0=ALU.mult, op1=ALU.mult),
                r=[f"x{tt}", "gains", "rstd"], w=[f"h{tt}"])

    if do_wo:
        P.dma("sp", idx, idx_in, "idx", w=["idx"])
        for c in range(8):
            P.op("pool", lambda e: e.indirect_dma_start(out=h[:, c, :], out_offset=None, in_=oall,
                                                         in_offset=bass.IndirectOffsetOnAxis(ap=idx[:, c:c + 1], axis=0)),
                 r=["idx"], w=[f"h{tt}" for tt in range(NTT)], dma="ogat")
        for g in range(4):
            b = g % 2
            P.dma("sp", wst[b][0].rearrange("p (c f) -> p c f", c=8), wo_in[:, :, g * FG:(g + 1) * FG],
                  f"wst{b}0", w=[f"wst{b}0"])
            P.pool(lambda e, b=b: e.tensor_copy(out=wbf[b][0], in_=wst[b][0]), r=[f"wst{b}0"], w=[f"wbf{b}0"])
            wv = wbf[b][0].rearrange("p (c f) -> p c f", c=8)
            for tt in range(NTT):
                for j in range(2):
                    dc_out = g * 2 + j
                    pb = (tt * 2 + j) % 2
                    for kc in range(8):
                        P.pe(lambda e, kc=kc, j=j, tt=tt, pb=pb, wv=wv: e.matmul(
                            ps[4 + pb], lhsT=wv[:, kc, j * 128:(j + 1) * 128], rhs=h[:, kc, tsl(tt)],
                            start=(kc == 0), stop=(kc == 7)),
                            r=[f"wbf{b}0", f"h{tt}"], w=[f"ps{4 + pb}"])
                    P.dve(lambda e, dc_out=dc_out, tt=tt, pb=pb: e.tensor_tensor(
                        out=x[:, dc_out, tsl(tt)], in0=ps[4 + pb], in1=x[:, dc_out, tsl(tt)], op=ALU.add),
                        r=[f"ps{4 + pb}", f"x{tt}"], w=[f"x{tt}"])

    for s in range(n_ffn):
        wg, wu, wd = ffw[s]
        for tt in range(NTT):
            norm_to_h(tt, s)
        for g in range(NFG):
            b = g % 2
            fs = slice(g * FG, (g + 1) * FG)
            P.dma("sp", wst[b][0].rearrange("p (c f) -> p c f", c=8), wg[:, :, fs], f"wst{b}0", w=[f"wst{b}0"])
            P.dma("sp", wst[b][1].rearrange("p (c f) -> p c f", c=8), wu[:, :, fs], f"wst{b}1", w=[f"wst{b}1"])
            P.dma("sp", wst[b][2].rearrange("p (c f) -> p c f", c=2), wd[:, 2 * g:2 * g + 2, :], f"wst{b}2",
                  w=[f"wst{b}2"])
            for j in range(3):
                P.pool(lambda e, b=b, j=j: e.tensor_copy(out=wbf[b][j], in_=wst[b][j]),
                       r=[f"wst{b}{j}"], w=[f"wbf{b}{j}"])
            wgv = wbf[b][0].rearrange("p (c f) -> p c f", c=8)
            wuv = wbf[b][1].rearrange("p (c f) -> p c f", c=8)
            wdv = wbf[b][2].rearrange("p (c f) -> p c f", c=2)
            for tt in range(NTT):
                zb = tt % 2
                for fc in range(2):
                    pg = 2 * fc
                    pu = 2 * fc + 1
                    for dc in range(8):
                        P.pe(lambda e, dc=dc, fc=fc, tt=tt, pg=pg, wgv=wgv: e.matmul(
                            ps[pg], lhsT=wgv[:, dc, fc * 128:(fc + 1) * 128], rhs=h[:, dc, tsl(tt)],
                            start=(dc == 0), stop=(dc == 7)),
                            r=[f"wbf{b}0", f"h{tt}"], w=[f"ps{pg}"])
                    for dc in range(8):
                        P.pe(lambda e, dc=dc, fc=fc, tt=tt, pu=pu, wuv=wuv: e.matmul(
                            ps[pu], lhsT=wuv[:, dc, fc * 128:(fc + 1) * 128], rhs=h[:, dc, tsl(tt)],
                            start=(dc == 0), stop=(dc == 7)),
                            r=[f"wbf{b}1", f"h{tt}"], w=[f"ps{pu}"])
                    P.act(lambda e, fc=fc, pg=pg: e.activation(out=a_sb[fc], in_=ps[pg], func=AF.Silu),
                          r=[f"ps{pg}"], w=[f"a{fc}"])
                    P.dve(lambda e, fc=fc, pu=pu, zb=zb: e.tensor_tensor(
                        out=z[zb][:, fc, :], in0=a_sb[fc], in1=ps[pu], op=ALU.mult),
                        r=[f"a{fc}", f"ps{pu}"], w=[f"z{zb}"])
                for dc in range(8):
                    pb = 4 + dc % 2
                    for fc in range(2):
                        P.pe(lambda e, dc=dc, fc=fc, pb=pb, zb=zb, wdv=wdv: e.matmul(
                            ps[pb], lhsT=wdv[:, fc, dc * 128:(dc + 1) * 128], rhs=z[zb][:, fc, :],
                            start=(fc == 0), stop=(fc == 1)),
                            r=[f"wbf{b}2", f"z{zb}"], w=[f"ps{pb}"])
                    P.dve(lambda e, dc=dc, tt=tt, pb=pb: e.scalar_tensor_tensor(
                        out=x[:, dc, tsl(tt)], in0=ps[pb], scalar=0.5, in1=x[:, dc, tsl(tt)],
                        op0=ALU.mult, op1=ALU.add),
                        r=[f"ps{pb}", f"x{tt}"], w=[f"x{tt}"])

    if do_mix:
        for tt in range(NTT):
            norm_to_h(tt, n_ffn)
            P.dma("sp", h_out[:, :, tsl(tt)], h[:, :, tsl(tt)], "hout", r=[f"h{tt}"])
    if do_final:
        for tt in range(NTT):
            norm_stats(tt)
            for dc in range(8):
                P.dve(lambda e, dc=dc, tt=tt: e.scalar_tensor_tensor(
                    out=x[:, dc, tsl(tt)], in0=x[:, dc, tsl(tt)],
                    scalar=gains[:, n_ffn * 8 + dc:n_ffn * 8 + dc + 1],
                    in1=rstd, op0=ALU.mult, op1=ALU.mult),
                    r=[f"x{tt}", "gains", "rstd"], w=[f"x{tt}"])
            P.dma("sp", y_out[:, :, tsl(tt)], x[:, :, tsl(tt)], "xout", r=[f"x{tt}"], out=True)
    else:
        for tt in range(NTT):
            P.dma("sp", x_out[:, :, tsl(tt)], x[:, :, tsl(tt)], "xout", r=[f"x{tt}"])

import math

D = 1024
S = 8192
TT = 512
NT = S // TT
QR, KVR, ROPE, NOPE, DV = 512, 256, 64, 128, 128
LATC = 896
EPS = 1e-6
SCALE = (NOPE + ROPE) ** -0.5


def emit_mla(CX, io):
    nc = CX.nc
    P = CX.P
    hsrc = io["hsrc"]
    pos_in = io["pos"]
    wa_in = io["wa"]
    wqb_in = io["wqb"]
    wkvb_in = io["wkvb"]
    c_in = io["consts"]
    masks_in = io["masks"]
    o_out = io["o_out"]

    sb = CX.sb
    stage = sb("stage", [128, 4 * LATC], F32).ap()
    wa = sb("wa_sb", [128, 8, LATC], BF16).ap()
    wqb = sb("wqb_sb", [128, 4, 512], BF16).ap()
    wkvb = sb("wkvb_sb", [128, 2, 512], BF16).ap()
    consts = sb("consts_sb", [128, 8], F32).ap()
    masks = sb("masks_sb", [128, 4, 512], BF16).ap()
    ones = sb("ones", [128, 128], BF16).ap()
    hT = [sb(f"hT{i}", [128, 8, TT], BF16).ap() for i in range(2)]
    lat = sb("lat", [128, 6, TT], F32).ap()
    sq = sb("sq", [128, 6, TT], BF16).ap()
    sd = sb("sd", [128, TT], F32).ap()
    rq = sb("rq", [128, TT], F32).ap()
    rkv = sb("rkv", [128, TT], F32).ap()
    cqn = sb("cqn", [128, 4, TT], BF16).ap()
    ckvn = sb("ckvn", [128, 2, TT], BF16).ap()
    posi = sb("posi", [64, TT], I32).ap()
    ui = sb("ui", [64, TT], I32).ap()
    u = sb("u", [64, TT], F32).ap()
    uc = sb("uc", [64, TT], F32).ap()
    wr = sb("wr", [64, TT], F32).ap()
    cos2 = sb("cos2", [64, TT], F32).ap()
    sin2 = sb("sin2", [64, TT], F32).ap()
    t1 = sb("t1", [64, TT], F32).ap()
    t2 = sb("t2", [64, TT], F32).ap()
    Kn = [sb(f"Kn{i}", [128, S], BF16).ap() for i in range(2)]
    Kr = sb("Kr", [64, S], BF16).ap()
    V = [sb(f"V{i}", [128, S // 128, DV], BF16).ap() for i in range(2)]
    qn = [sb(f"qn{i}", [128, TT], BF16).ap() for i in range(2)]
    qr = [sb(f"qr{i}", [64, TT], BF16).ap() for i in range(2)]
    pT = [sb(f"pT{i}", [128, TT], BF16).ap() for i in range(3)]
    rden = sb("rden", [128, TT], F32).ap()
    osb = [sb(f"osb{i}", [128, TT], BF16).ap() for i in range(2)]
    ps = CX.ps

    P.pool(lambda e: e.memset(ones, 1.0), w=["ones"])
    P.dma("sp", consts, c_in, "consts", w=["consts"])
    P.dma("sp", masks, masks_in, "masks", w=["masks"])
    for hf in range(2):
        P.dma("sp", stage.rearrange("p (c f) -> p c f", c=4), wa_in[:, 4 * hf:4 * hf + 4, :], "stage", w=["stage"])
        P.pool(lambda e, hf=hf: e.tensor_copy(out=wa[:, 4 * hf:4 * hf + 4, :].rearrange("p c f -> p (c f)"), in_=stage),
               r=["stage"], w=["wa"])
    P.dma("sp", stage[:, 0:2048].rearrange("p (c f) -> p c f", c=4), wqb_in, "stage", w=["stage"])
    P.pool(lambda e: e.tensor_copy(out=wqb.rearrange("p c f -> p (c f)"), in_=stage[:, 0:2048]), r=["stage"], w=["wqb"])
    P.dma("sp", stage[:, 0:1024].rearrange("p (c f) -> p c f", c=2), wkvb_in, "stage", w=["stage"])
    P.pool(lambda e: e.tensor_copy(out=wkvb.rearrange("p c f -> p (c f)"), in_=stage[:, 0:1024]), r=["stage"], w=["wkvb"])

    pcount = [0]

    def pbank():
        pcount[0] += 1
        return pcount[0] % 2

    def tsl(t):
        return slice(t * TT, (t + 1) * TT)

    def rope(src_ps, src_sw_ps, dst, rd, wt):
        P.dve(lambda e: e.tensor_tensor(out=t1, in0=src_ps, in1=cos2, op=ALU.mult), r=rd + ["cos2"], w=["t1"])
        P.dve(lambda e: e.tensor_tensor(out=t2, in0=src_sw_ps, in1=sin2, op=ALU.mult), r=rd + ["sin2"], w=["t2"])
        P.dve(lambda e: e.tensor_tensor(out=dst, in0=t1, in1=t2, op=ALU.add), r=["t1", "t2"], w=wt)

    for t in range(NT):
        hb = t % 2
        hsrc(hT[hb], t, f"hT{hb}", [f"hT{hb}"])
        P.dma("sp", posi, pos_in[:, tsl(t)].partition_broadcast(64), "posi", w=["posi"])
        P.dve(lambda e: e.tensor_copy(out=t1, in_=posi), r=["posi"], w=["t1"])
        P.dve(lambda e: e.tensor_scalar(out=u, in0=t1, scalar1=consts[0:64, 6:7], scalar2=None, op0=ALU.mult),
              r=["t1", "consts"], w=["u"])
        P.dve(lambda e: e.tensor_copy(out=ui, in_=u), r=["u"], w=["ui"])
        P.dve(lambda e: e.tensor_copy(out=t2, in_=ui), r=["ui"], w=["t2"])
        P.dve(lambda e: e.tensor_tensor(out=u, in0=u, in1=t2, op=ALU.subtract), r=["u", "t2"], w=["u"])
        P.dve(lambda e: e.tensor_single_scalar(out=wr, in_=u, scalar=0.5, op=ALU.is_gt), r=["u"], w=["wr"])
        P.dve(lambda e: e.tensor_tensor(out=u, in0=u, in1=wr, op=ALU.subtract), r=["u", "wr"], w=["u"])
        P.dve(lambda e: e.tensor_single_scalar(out=wr, in_=u, scalar=-0.5, op=ALU.is_lt), r=["u"], w=["wr"])
        P.dve(lambda e: e.tensor_tensor(out=u, in0=u, in1=wr, op=ALU.add), r=["u", "wr"], w=["u"])
        P.dve(lambda e: e.tensor_scalar_add(out=uc, in0=u, scalar1=0.25), r=["u"], w=["uc"])
        P.dve(lambda e: e.tensor_single_scalar(out=wr, in_=uc, scalar=0.5, op=ALU.is_gt), r=["uc"], w=["wr"])
        P.dve(lambda e: e.tensor_tensor(out=uc, in0=uc, in1=wr, op=ALU.subtract), r=["uc", "wr"], w=["uc"])
        P.act(lambda e: e.activation(out=sin2, in_=u, func=AF.Sin, scale=2 * math.pi), r=["u"], w=["sin2"])
        P.act(lambda e: e.activation(out=cos2, in_=uc, func=AF.Sin, scale=2 * math.pi), r=["uc"], w=["cos2"])
        P.dve(lambda e: e.tensor_scalar(out=sin2, in0=sin2, scalar1=consts[0:64, 7:8], scalar2=None, op0=ALU.mult),
              r=["sin2", "consts"], w=["sin2"])
        for c in range(6):
            pb = pbank()
            for dc in range(8):
                P.pe(lambda e, c=c, dc=dc, pb=pb: e.matmul(ps[pb], lhsT=wa[:, dc, c * 128:(c + 1) * 128],
                                                            rhs=hT[hb][:, dc, :], start=(dc == 0), stop=(dc == 7)),
                     r=["wa", f"hT{hb}"], w=[f"ps{pb}"])
            P.act(lambda e, c=c, pb=pb: e.copy(out=lat[:, c, :], in_=ps[pb]), r=[f"ps{pb}"], w=[f"lat{c}"])
        pbs = []
        for c2 in range(2):
            pb = pbank()
            pbs.append(pb)
            for dc in range(8):
                P.pe(lambda e, c2=c2, dc=dc, pb=pb: e.matmul(ps[pb][0:64, :], lhsT=wa[:, dc, 768 + 64 * c2:832 + 64 * c2],
                                                              rhs=hT[hb][:, dc, :], start=(dc == 0), stop=(dc == 7)),
                     r=["wa", f"hT{hb}"], w=[f"ps{pb}"])
        rope(ps[pbs[0]][0:64, :], ps[pbs[1]][0:64, :], Kr[:, tsl(t)], [f"ps{pbs[0]}", f"ps{pbs[1]}"], [f"Kr{t}"])
        P.act(lambda e: e.activation(out=sq, in_=lat[:, 0:6, :], func=AF.Square),
              r=[f"lat{c}" for c in range(6)], w=["sq"])
        pb = pbank()
        for c in range(4):
            P.pe(lambda e, c=c, pb=pb: e.matmul(ps[pb], lhsT=ones, rhs=sq[:, c, :], start=(c == 0), stop=(c == 3)),
                 r=["ones", "sq"], w=[f"ps{pb}"])
        P.act(lambda e, pb=pb: e.activation(out=sd, in_=ps[pb], func=AF.Sqrt, bias=EPS, scale=1.0 / QR),
              r=[f"ps{pb}"], w=["sd"])
        P.dve(lambda e: e.reciprocal(out=rq, in_=sd), r=["sd"], w=["rq"])
        pb = pbank()
        for c in range(2):
            P.pe(lambda e, c=c, pb=pb: e.matmul(ps[pb], lhsT=ones, rhs=sq[:, 4 + c, :], start=(c == 0), stop=(c == 1)),
                 r=["ones", "sq"], w=[f"ps{pb}"])
        P.act(lambda e, pb=pb: e.activation(out=sd, in_=ps[pb], func=AF.Sqrt, bias=EPS, scale=1.0 / KVR),
              r=[f"ps{pb}"], w=["sd"])
        P.dve(lambda e: e.reciprocal(out=rkv, in_=sd), r=["sd"], w=["rkv"])
        for c in range(4):
            P.dve(lambda e, c=c: e.scalar_tensor_tensor(out=cqn[:, c, :], in0=lat[:, c, :], scalar=consts[:, c:c + 1],
                                                        in1=rq, op0=ALU.mult, op1=ALU.mult),
                  r=[f"lat{c}", "rq", "consts"], w=["cqn"])
        for c in range(2):
            P.dve(lambda e, c=c: e.scalar_tensor_tensor(out=ckvn[:, c, :], in0=lat[:, 4 + c, :],
                                                        scalar=consts[:, 4 + c:5 + c],
                                                        in1=rkv, op0=ALU.mult, op1=ALU.mult),
                  r=[f"lat{4 + c}", "rkv", "consts"], w=["ckvn"])
        for hh in range(2):
            qo = hh * 256
            pb = pbank()
            for c in range(4):
                P.pe(lambda e, c=c, pb=pb, qo=qo: e.matmul(ps[pb], lhsT=wqb[:, c, qo:qo + 128], rhs=cqn[:, c, :],
                                                            start=(c == 0), stop=(c == 3)),
                     r=["wqb", "cqn"], w=[f"ps{pb}"])
            P.act(lambda e, pb=pb, hh=hh: e.copy(out=qn[hh], in_=ps[pb]), r=[f"ps{pb}"], w=[f"qn{hh}"])
            pbs = []
            for c2 in range(2):
                pb = pbank()
                pbs.append(pb)
                for c in range(4):
                    P.pe(lambda e, c=c, c2=c2, pb=pb, qo=qo: e.matmul(
                        ps[pb][0:64, :], lhsT=wqb[:, c, qo + 128 + 64 * c2:qo + 192 + 64 * c2], rhs=cqn[:, c, :],
                        start=(c == 0), stop=(c == 3)),
                        r=["wqb", "cqn"], w=[f"ps{pb}"])
            rope(ps[pbs[0]][0:64, :], ps[pbs[1]][0:64, :], qr[hh], [f"ps{pbs[0]}", f"ps{pbs[1]}"], [f"qr{hh}"])
            ko = hh * 256
            pb = pbank()
            for c in range(2):
                P.pe(lambda e, c=c, pb=pb, ko=ko: e.matmul(ps[pb], lhsT=wkvb[:, c, ko:ko + 128], rhs=ckvn[:, c, :],
                                                            start=(c == 0), stop=(c == 1)),
                     r=["wkvb", "ckvn"], w=[f"ps{pb}"])
            P.act(lambda e, pb=pb, hh=hh, t=t: e.copy(out=Kn[hh][:, tsl(t)], in_=ps[pb]), r=[f"ps{pb}"],
                  w=[f"Kn{hh}_{t}"])
            pb = pbank()
            for j in range(4):
                for c in range(2):
                    P.pe(lambda e, c=c, j=j, pb=pb, ko=ko: e.matmul(
                        ps[pb][:, j * 128:(j + 1) * 128], lhsT=ckvn[:, c, j * 128:(j + 1) * 128],
                        rhs=wkvb[:, c, ko + 128:ko + 256], start=(c == 0), stop=(c == 1)),
                        r=["wkvb", "ckvn"], w=[f"ps{pb}"])
            P.act(lambda e, pb=pb, hh=hh, t=t: e.copy(
                out=V[hh][:, 4 * t:4 * t + 4, :], in_=ps[pb].rearrange("p (j d) -> p j d", j=4)),
                r=[f"ps{pb}"], w=[f"V{hh}_{t}"])
        for hh in range(2):
            ob = 4 + 2 * ((2 * t + hh) % 2)
            nkb = 4 * t + 4
            for kb in range(nkb):
                sbk = 2 + (kb % 2)
                j = kb - 4 * t
                q0 = max(j, 0) * 128
                tk = kb // 4
                ksl = slice(kb * 128, (kb + 1) * 128)
                P.pe(lambda e, sbk=sbk, hh=hh, ksl=ksl, q0=q0: e.matmul(
                    ps[sbk][:, q0:], lhsT=Kn[hh][:, ksl], rhs=qn[hh][:, q0:], start=True, stop=False),
                    r=[f"Kn{hh}_{tk}", f"qn{hh}"], w=[f"ps{sbk}"])
                P.pe(lambda e, sbk=sbk, hh=hh, ksl=ksl, q0=q0: e.matmul(
                    ps[sbk][:, q0:], lhsT=Kr[:, ksl], rhs=qr[hh][:, q0:], start=False, stop=True),
                    r=[f"Kr{tk}", f"qr{hh}"], w=[f"ps{sbk}"])
                pi = kb % 3
                P.act(lambda e, sbk=sbk, pi=pi, q0=q0: e.activation(out=pT[pi][:, q0:], in_=ps[sbk][:, q0:],
                                                                     func=AF.Exp, scale=SCALE),
                      r=[f"ps{sbk}"], w=[f"pT{pi}"])
                if j >= 0:
                    P.pool(lambda e, pi=pi, j=j, q0=q0: e.tensor_tensor(out=pT[pi][:, q0:], in0=pT[pi][:, q0:],
                                                                        in1=masks[:, j, q0:], op=ALU.mult),
                           r=[f"pT{pi}", "masks"], w=[f"pT{pi}"])
                P.pe(lambda e, ob=ob, hh=hh, kb=kb, pi=pi, q0=q0, nkb=nkb: e.matmul(
                    ps[ob][:, q0:], lhsT=V[hh][:, kb, :], rhs=pT[pi][:, q0:], start=(kb == 0), stop=(kb == nkb - 1)),
                    r=[f"V{hh}_{tk}", f"pT{pi}"], w=[f"ps{ob}"])
                P.pe(lambda e, ob=ob, kb=kb, pi=pi, q0=q0, nkb=nkb: e.matmul(
                    ps[ob + 1][:, q0:], lhsT=ones, rhs=pT[pi][:, q0:], start=(kb == 0), stop=(kb == nkb - 1)),
                    r=["ones", f"pT{pi}"], w=[f"ps{ob + 1}"])
            P.dve(lambda e, ob=ob: e.reciprocal(out=rden, in_=ps[ob + 1]), r=[f"ps{ob + 1}"], w=["rden"])
            P.dve(lambda e, ob=ob, hh=hh: e.tensor_tensor(out=osb[hh], in0=ps[ob], in1=rden, op=ALU.mult),
                  r=[f"ps{ob}", "rden"], w=[f"osb{hh}"])
            P.dma("sp", o_out[hh * DV:(hh + 1) * DV, tsl(t)], osb[hh], "oout", r=[f"osb{hh}"])


D = 1024
S = 8192
TT = 512
NT = S // TT
LM = 128
NCM = S // LM
EPS = 1e-6
WC = 772


def emit_mls(CX, io):
    nc = CX.nc
    P = CX.P
    hsrc = io["hsrc"]
    w_in = io["w"]
    c_in = io["consts"]
    cm_in = io["cm"]
    sel_in = io["sel"]
    o_out = io["o_out"]

    sb = CX.sb
    stage = sb("stage", [128, 4 * WC], F32).ap()
    w = sb("w_sb", [128, 8, WC], BF16).ap()
    consts = sb("consts_sb", [128, 16], F32).ap()
    cm = sb("cm_sb", [128, 256], BF16).ap()
    ident = cm[:, 0:128]
    tril = cm[:, 128:256]
    sel = sb("sel_sb", [2, 386], F32).ap()
    ones = sb("ones", [128, 128], BF16).ap()
    hT = [sb(f"hT{i}", [128, 8, TT], BF16).ap() for i in range(2)]
    gi = sb("gi", [2, S], F32).ap()
    gf = sb("gf", [2, S], F32).ap()
    mu = sb("mu", [2, S], F32).ap()
    Gcol = sb("Gcol", [128, NCM, 2], F32).ap()
    nmu = [sb(f"nmu{i}", [128, NCM + 1], F32).ap() for i in range(2)]
    pmuq = sb("pmuq", [128, NCM + 1], F32).ap()
    mub = [sb(f"mub{i}", [128, TT], F32).ap() for i in range(2)]
    muq = sb("muq", [128, TT], F32).ap()
    emt = [sb(f"emt{i}", [128, TT], F32).ap() for i in range(2)]
    xq = sb("xq", [128, 3 + TT], F32).ap()
    xk = sb("xk", [128, 3 + TT], F32).ap()
    cacc = sb("cacc", [128, TT], F32).ap()
    csil = sb("csil", [128, TT], F32).ap()
    qT = sb("qT", [128, TT], BF16).ap()
    qsT = sb("qsT", [128, TT], BF16).ap()
    kT = sb("kT", [128, TT], BF16).ap()
    ktok = sb("ktok", [128, 4, 128], F32).ap()
    kw = sb("kw", [128, 128], BF16).ap()
    vtok = [sb(f"vtok{i}", [128, 4, 128], BF16).ap() for i in range(2)]
    sigo = [sb(f"sigo{i}", [128, TT], F32).ap() for i in range(2)]
    Wt = sb("Wt", [128, 128], F32).ap()
    Wm = sb("Wm", [128, 128], F32).ap()
    pT = sb("pT", [128, 128], BF16).ap()
    rb = sb("rb", [128, TT], F32).ap()
    wcol = sb("wcol", [128, 2], F32).ap()
    carry = sb("carry", [128, 1], F32).ap()
    C = sb("C", [128, 128], F32).ap()
    Cb = sb("Cb", [128, 128], BF16).ap()
    Nb = sb("Nb", [128, 128], F32).ap()
    Nbb = sb("Nbb", [128, 128], BF16).ap()
    dn = sb("dn", [128, 128], F32).ap()
    hid = [sb(f"hid{i}", [128, TT], F32).ap() for i in range(2)]
    hsq = sb("hsq", [128, TT], BF16).ap()
    sd = sb("sd", [128, TT], F32).ap()
    rstd = sb("rstd", [128, TT], F32).ap()
    ho = [sb(f"ho{i}", [128, TT], BF16).ap() for i in range(2)]
    ps = CX.ps

    def tsl(t):
        return slice(t * TT, (t + 1) * TT)

    P.pool(lambda e: e.memset(ones, 1.0), w=["ones"])
    P.pool(lambda e: e.memset(C, 0.0), w=["C"])
    P.pool(lambda e: e.memset(Cb, 0.0), w=["Cb"])
    P.pool(lambda e: e.memset(Nb, 0.0), w=["Nb"])
    P.pool(lambda e: e.memset(Nbb, 0.0), w=["Nbb"])
    P.pool(lambda e: e.memset(xq[:, 0:3], 0.0), w=["xq"])
    P.pool(lambda e: e.memset(xk[:, 0:3], 0.0), w=["xk"])
    for i in range(2):
        P.pool(lambda e, i=i: e.memset(nmu[i][:, 0:1], 0.0), w=[f"nmu{i}"])
    P.pool(lambda e: e.memset(pmuq[:, 0:1], 0.0), w=["pmuq"])
    P.dma("sp", consts, c_in, "consts", w=["consts"])
    P.dma("sp", cm, cm_in, "cm", w=["cm"])
    P.dma("sp", sel, sel_in, "sel", w=["sel"])
    for hf in range(2):
        P.dma("sp", stage.rearrange("p (c f) -> p c f", c=4), w_in[:, 4 * hf:4 * hf + 4, :], "stage", w=["stage"])
        P.pool(lambda e: e.tensor_copy(out=w[:, 4 * hf:4 * hf + 4, :].rearrange("p c f -> p (c f)"), in_=stage),
               r=["stage"], w=["w"])

    for t in range(NT):
        hb = t % 2
        hsrc(hT[hb], t, f"hT{hb}", [f"hT{hb}"])
        for g2 in range(2):
            pb = g2
            for dc in range(8):
                P.pe(lambda e: e.matmul(ps[pb][0:2, :], lhsT=w[:, dc, 768 + 2 * g2:770 + 2 * g2], rhs=hT[hb][:, dc, :],
                                        start=(dc == 0), stop=(dc == 7)),
                     r=["w", f"hT{hb}"], w=[f"ps{pb}"])
        P.act(lambda e: e.activation(out=gi[:, tsl(t)], in_=ps[0][0:2, :], func=AF.Identity, bias=consts[0:2, 12:13]),
              r=["ps0", "consts"], w=["gi"])
        P.act(lambda e: e.activation(out=gf[:, tsl(t)], in_=ps[1][0:2, :], func=AF.Exp, bias=consts[0:2, 13:14], scale=-1.0),
              r=["ps1", "consts"], w=["gf"])
    P.act(lambda e: e.activation(out=gf, in_=gf, func=AF.Ln, bias=1.0, scale=1.0), r=["gf"], w=["gf"])
    P.dve(lambda e: e.tensor_scalar(out=gf, in0=gf, scalar1=-0.5, scalar2=None, op0=ALU.mult), r=["gf"], w=["gf"])
    P.dve(lambda e: e.tensor_tensor_scan(out=gf, data0=gf, data1=gf, initial=0.0, op0=ALU.add, op1=ALU.add),
          r=["gf"], w=["gf"])
    P.dve(lambda e: e.tensor_tensor(out=gi, in0=gi, in1=gf, op=ALU.subtract), r=["gi", "gf"], w=["gi"])
    P.dve(lambda e: e.tensor_tensor_scan(out=mu, data0=gi, data1=gi, initial=0.0, op0=ALU.max, op1=ALU.max),
          r=["gi"], w=["mu"])
    P.dve(lambda e: e.tensor_tensor(out=gf, in0=gf, in1=mu, op=ALU.add), r=["gf", "mu"], w=["gf"])
    fm = gf
    id2 = sel[:, 384:386]
    for c in range(NCM):
        P.pe(lambda e: e.matmul(ps[2][:, 2 * c:2 * c + 2], lhsT=gi[:, c * LM:(c + 1) * LM], rhs=id2, start=True, stop=True),
             r=["gi", "sel"], w=["ps2"])
    P.act(lambda e: e.copy(out=Gcol.rearrange("p c h -> p (c h)"), in_=ps[2][:, 0:2 * NCM]), r=["ps2"], w=["Gcol"])

    pcnt = [0]

    def pbank():
        pcnt[0] += 1
        return pcnt[0] % 2

    for t in range(NT):
        hb = t % 2
        hsrc(hT[hb], t, f"hT{hb}", [f"hT{hb}"])
        for hh in range(2):
            P.pe(lambda e: e.matmul(ps[2], lhsT=sel[:, hh * 128:(hh + 1) * 128], rhs=mu[:, tsl(t)], start=True, stop=True),
                 r=["sel", "mu"], w=["ps2"])
            P.act(lambda e: e.copy(out=mub[hh], in_=ps[2]), r=["ps2"], w=[f"mub{hh}"])
            P.act(lambda e: e.activation(out=nmu[hh][:, 4 * t + 1:4 * t + 5], in_=ps[2][:, LM - 1::LM], func=AF.Copy, scale=-1.0),
                  r=["ps2"], w=[f"nmu{hh}"])
            P.pe(lambda e: e.matmul(ps[2], lhsT=sel[:, hh * 128:(hh + 1) * 128], rhs=fm[:, tsl(t)], start=True, stop=True),
                 r=["sel", "gf"], w=["ps2"])
            P.act(lambda e: e.activation(out=emt[hh], in_=ps[2], func=AF.Exp, scale=-1.0), r=["ps2"], w=[f"emt{hh}"])
        P.pe(lambda e: e.matmul(ps[2], lhsT=sel[:, 256:384], rhs=mu[:, tsl(t)], start=True, stop=True),
             r=["sel", "mu"], w=["ps2"])
        P.act(lambda e: e.copy(out=muq, in_=ps[2]), r=["ps2"], w=["muq"])
        P.act(lambda e: e.copy(out=pmuq[:, 4 * t + 1:4 * t + 5], in_=ps[2][:, LM - 1::LM]), r=["ps2"], w=["pmuq"])
        for which, xbuf, dst in ((0, xq, qT), (1, xk, kT)):
            pb = pbank()
            xn = "xq" if which == 0 else "xk"
            for dc in range(8):
                P.pe(lambda e: e.matmul(ps[pb], lhsT=w[:, dc, which * 128:(which + 1) * 128], rhs=hT[hb][:, dc, :],
                                        start=(dc == 0), stop=(dc == 7)),
                     r=["w", f"hT{hb}"], w=[f"ps{pb}"])
            P.act(lambda e: e.copy(out=xbuf[:, 3:3 + TT], in_=ps[pb]), r=[f"ps{pb}"], w=[xn])
            cw = 4 * which
            P.dve(lambda e: e.tensor_scalar(out=cacc, in0=xbuf[:, 0:TT], scalar1=consts[:, cw:cw + 1],
                                            scalar2=consts[:, 8 + which:9 + which], op0=ALU.mult, op1=ALU.add),
                  r=[xn, "consts"], w=["cacc"])
            for j in range(1, 4):
                P.dve(lambda e: e.scalar_tensor_tensor(out=cacc, in0=xbuf[:, j:j + TT], scalar=consts[:, cw + j:cw + j + 1],
                                                       in1=cacc, op0=ALU.mult, op1=ALU.add),
                      r=[xn, "consts", "cacc"], w=["cacc"])
            P.pool(lambda e: e.tensor_copy(out=xbuf[:, 0:3], in_=xbuf[:, TT:TT + 3]), r=[xn, "cacc"], w=[xn])
            P.act(lambda e: e.activation(out=csil, in_=cacc, func=AF.Silu), r=["cacc"], w=["csil"])
            if which == 0:
                P.dve(lambda e: e.tensor_copy(out=dst, in_=csil), r=["csil"], w=["qT"])
            else:
                P.dve(lambda e: e.tensor_scalar(out=dst, in0=csil, scalar1=0.125, scalar2=None, op0=ALU.mult),
                      r=["csil"], w=["kT"])
        for hh in range(2):
            pb = pbank()
            for dc in range(8):
                P.pe(lambda e: e.matmul(ps[pb], lhsT=w[:, dc, 512 + hh * 128:640 + hh * 128], rhs=hT[hb][:, dc, :],
                                        start=(dc == 0), stop=(dc == 7)),
                     r=["w", f"hT{hb}"], w=[f"ps{pb}"])
            P.act(lambda e: e.activation(out=sigo[hh], in_=ps[pb], func=AF.Sigmoid), r=[f"ps{pb}"], w=[f"sigo{hh}"])
        for hh in range(2):
            pb = pbank()
            for j in range(4):
                for dc in range(8):
                    P.pe(lambda e: e.matmul(ps[pb][:, j * 128:(j + 1) * 128], lhsT=hT[hb][:, dc, j * 128:(j + 1) * 128],
                                            rhs=w[:, dc, 256 + hh * 128:384 + hh * 128], start=(dc == 0), stop=(dc == 7)),
                         r=["w", f"hT{hb}"], w=[f"ps{pb}"])
            P.act(lambda e: e.copy(out=vtok[hh].rearrange("p j d -> p (j d)"), in_=ps[pb]), r=[f"ps{pb}"], w=[f"vtok{hh}"])
        pb = pbank()
        for j in range(4):
            P.pe(lambda e: e.matmul(ps[pb][:, j * 128:(j + 1) * 128], lhsT=kT[:, j * 128:(j + 1) * 128], rhs=ident,
                                    start=True, stop=True),
                 r=["kT", "cm"], w=[f"ps{pb}"])
        P.act(lambda e: e.copy(out=ktok.rearrange("p j d -> p (j d)"), in_=ps[pb]), r=[f"ps{pb}"], w=["ktok"])
        for j in range(4):
            c = 4 * t + j
            P.act(lambda e: e.activation(out=rb[:, j * LM:(j + 1) * LM], in_=muq[:, j * LM:(j + 1) * LM], func=AF.Exp,
                                         bias=pmuq[:, c:c + 1], scale=-1.0),
                  r=["muq", "pmuq"], w=["rb"])
        P.dve(lambda e: e.tensor_tensor(out=qsT, in0=qT, in1=rb, op=ALU.mult), r=["qT", "rb"], w=["qsT"])
        for j in range(4):
            c = 4 * t + j
            cs = slice(j * LM, (j + 1) * LM)
            for hh in range(2):
                hp = slice(hh * 64, (hh + 1) * 64)
                P.pe(lambda e: e.matmul(ps[3][:, 0:128], lhsT=kT[hp, cs], rhs=qT[hp, cs], start=True, stop=True),
                     r=["kT", "qT"], w=["ps3"])
                P.act(lambda e: e.activation(out=Wt, in_=mub[hh][:, cs], func=AF.Exp, bias=Gcol[:, c, hh:hh + 1], scale=-1.0),
                      r=[f"mub{hh}", "Gcol"], w=["Wt"])
                P.pool(lambda e: e.tensor_tensor(out=Wm, in0=Wt, in1=tril, op=ALU.mult), r=["Wt", "cm"], w=["Wm"])
                P.dve(lambda e: e.tensor_tensor(out=pT, in0=ps[3][:, 0:128], in1=Wm, op=ALU.mult), r=["ps3", "Wm"], w=["pT"])
                P.pe(lambda e: e.matmul(ps[4][:, 0:128], lhsT=vtok[hh][:, j, :], rhs=pT, start=True, stop=False),
                     r=[f"vtok{hh}", "pT"], w=["ps4"])
                P.pe(lambda e: e.matmul(ps[4][:, 0:128], lhsT=Cb[hp, :], rhs=qsT[hp, cs], start=False, stop=True),
                     r=["Cb", "qsT"], w=["ps4"])
                P.pe(lambda e: e.matmul(ps[5][:, 0:128], lhsT=ones, rhs=pT, start=True, stop=False),
                     r=["ones", "pT"], w=["ps5"])
                P.pe(lambda e: e.matmul(ps[5][:, 0:128], lhsT=Nbb[hp, :], rhs=qsT[hp, cs], start=False, stop=True),
                     r=["Nbb", "qsT"], w=["ps5"])
                P.act(lambda e: e.activation(out=dn, in_=ps[5][:, 0:128], func=AF.Abs), r=["ps5"], w=["dn"])
                P.dve(lambda e: e.tensor_tensor(out=dn, in0=dn, in1=emt[hh][:, cs], op=ALU.max),
                      r=["dn", f"emt{hh}"], w=["dn"])
                P.dve(lambda e: e.reciprocal(out=dn, in_=dn), r=["dn"], w=["dn"])
                P.dve(lambda e: e.tensor_tensor(out=hid[hh][:, cs], in0=ps[4][:, 0:128], in1=dn, op=ALU.mult),
                      r=["ps4", "dn"], w=[f"hid{hh}"])
                P.act(lambda e: e.activation(out=wcol[:, hh:hh + 1], in_=Gcol[:, c, hh:hh + 1], func=AF.Exp,
                                             bias=nmu[hh][:, c + 1:c + 2], scale=1.0),
                      r=["Gcol", f"nmu{hh}"], w=["wcol"])
                P.pool(lambda e: e.tensor_scalar(out=kw[:, hp], in0=ktok[:, j, hp], scalar1=wcol[:, hh:hh + 1], scalar2=None,
                                                 op0=ALU.mult),
                       r=["ktok", "wcol"], w=["kw"])
            P.act(lambda e: e.activation(out=carry, in_=pmuq[:, c + 1:c + 2], func=AF.Exp, bias=pmuq[:, c:c + 1], scale=-1.0),
                  r=["pmuq"], w=["carry"])
            for hh in range(2):
                hp = slice(hh * 64, (hh + 1) * 64)
                P.pe(lambda e: e.matmul(ps[6][hp, 0:128], lhsT=kw[:, hp], rhs=vtok[hh][:, j, :], start=True, stop=True),
                     r=["kw", f"vtok{hh}"], w=["ps6"])
            P.pe(lambda e: e.matmul(ps[7][:, 0:128], lhsT=kw, rhs=ones, start=True, stop=True), r=["kw", "ones"], w=["ps7"])
            P.dve(lambda e: e.scalar_tensor_tensor(out=C, in0=C, scalar=carry, in1=ps[6][:, 0:128], op0=ALU.mult, op1=ALU.add),
                  r=["C", "carry", "ps6"], w=["C"])
            P.pool(lambda e: e.tensor_copy(out=Cb, in_=C), r=["C"], w=["Cb"])
            P.dve(lambda e: e.scalar_tensor_tensor(out=Nb, in0=Nb, scalar=carry, in1=ps[7][:, 0:128], op0=ALU.mult, op1=ALU.add),
                  r=["Nb", "carry", "ps7"], w=["Nb"])
            P.pool(lambda e: e.tensor_copy(out=Nbb, in_=Nb), r=["Nb"], w=["Nbb"])
        for hh in range(2):
            P.act(lambda e: e.activation(out=hsq, in_=hid[hh], func=AF.Square), r=[f"hid{hh}"], w=["hsq"])
            pb = pbank()
            P.pe(lambda e: e.matmul(ps[pb], lhsT=ones, rhs=hsq, start=True, stop=True), r=["ones", "hsq"], w=[f"ps{pb}"])
            P.act(lambda e: e.activation(out=sd, in_=ps[pb], func=AF.Sqrt, bias=EPS, scale=1.0 / 128), r=[f"ps{pb}"], w=["sd"])
            P.dve(lambda e: e.reciprocal(out=rstd, in_=sd), r=["sd"], w=["rstd"])
            P.dve(lambda e: e.scalar_tensor_tensor(out=rstd, in0=rstd, scalar=consts[:, 10 + hh:11 + hh], in1=sigo[hh],
                                                   op0=ALU.mult, op1=ALU.mult),
                  r=["rstd", "consts", f"sigo{hh}"], w=["rstd"])
            P.dve(lambda e: e.tensor_tensor(out=ho[hh], in0=hid[hh], in1=rstd, op=ALU.mult), r=[f"hid{hh}", "rstd"], w=[f"ho{hh}"])
            P.dma("sp", o_out[hh * 128:(hh + 1) * 128, tsl(t)], ho[hh], "oout", r=[f"ho{hh}"])

import math

D = 1024
S = 8192
TT = 512
NT = S // TT
LR = 64
NJ = TT // LR
NCR = S // LR
GN_EPS = 64e-5
DEC = math.exp(-0.5)


def emit_rwk(CX, io, nt=NT):
    nc = CX.nc
    P = CX.P
    hsrc = io["hsrc"]
    wbig_in = io["wbig"]
    w2c_in = io["w2c"]
    g2c_in = io["g2c"]
    mu_in = io["mu"]
    cvec_in = io["cvec"]
    lnb_in = io["lnwb"]
    cf_in = io["cf32"]
    o_out = io["o_out"]

    sb = CX.sb
    stage = sb("stage", [128, 4096], F32).ap()
    wbig = sb("wbig_sb", [128, 8, 1024], BF16).ap()
    w2c = sb("w2c_sb", [64, 512], BF16).ap()
    g2c = sb("g2c_sb", [128, 256], BF16).ap()
    mu = sb("mu_sb", [128, 48], F32).ap()
    cvec = sb("cvec_sb", [128, 16], F32).ap()
    lnwb = sb("lnwb_sb", [128, 256], F32).ap()
    cf = sb("cf_sb", [128, 1025], F32).ap()
    mask320 = cf[:, 0:320]
    identS = cf[:, 320:384]
    onescol = cf[:, 384:385]
    blockones = cf[:, 385:513]
    keep = cf[:, 513:1025]
    hT = [sb(f"hT{i}", [128, 8, 1 + TT], BF16).ap() for i in range(2)]
    xx = sb("xx", [128, 8, TT], F32).ap()
    xm = [sb(f"xm{i}", [128, 8, TT], BF16).ap() for i in range(2)]
    lora = sb("lora", [128, TT], BF16).ap()
    sg = sb("sg", [128, TT], BF16).ap()
    names = ["r", "k", "v", "lg", "ag", "kk", "km", "lp", "tmp", "tmp2", "bt", "kt", "rkr", "ssq"]
    A_ = [{n: sb(f"{n}{p}", [128, TT], F32).ap() for n in names} for p in range(2)]
    ar = [sb(f"ar{p}", [128, NJ, 128], F32).ap() for p in range(2)]
    S0T = [sb(f"S0T{p}", [128, 64], F32).ap() for p in range(2)]
    AM = sb("AM", [128, 320], F32).ap()
    bkP = sb("bkP", [128, 128], F32).ap()
    tok = sb("tok", [128, 192], F32).ap()
    Y = sb("Y", [128, 64], F32).ap()
    An = [sb(f"An{i}", [128, 128], F32).ap() for i in range(2)]
    st6 = sb("st6", [128, 6], F32).ap()
    mv = sb("mv", [128, 2], F32).ap()
    rs = sb("rs", [128, 1], F32).ap()
    bsc = sb("bsc", [128, 1], F32).ap()
    yn = sb("yn", [128, 64], F32).ap()
    yo = sb("yo", [128, 64], F32).ap()
    ofm = [sb(f"ofm{p}", [128, TT], BF16).ap() for p in range(2)]
    ps = CX.ps

    def tsl(t):
        return slice(t * TT, (t + 1) * TT)

    for p in range(2):
        P.pool(lambda e: e.memset(S0T[p], 0.0), w=[f"S0T{p}"])
    P.pool(lambda e: e.memset(hT[1][:, :, 0:1], 0.0), w=["hT1"])
    P.dma("sp", mu, mu_in, "mu", w=["mu"])
    P.dma("sp", cvec, cvec_in, "cvec", w=["cvec"])
    P.dma("sp", lnwb, lnb_in, "lnwb", w=["lnwb"])
    P.dma("sp", cf, cf_in, "cf", w=["cf"])
    for hf in range(2):
        P.dma("sp", stage.rearrange("p (c f) -> p c f", c=4), wbig_in[:, 4 * hf:4 * hf + 4, :], "stage", w=["stage"])
        P.pool(lambda e: e.tensor_copy(out=wbig[:, 4 * hf:4 * hf + 4, :].rearrange("p c f -> p (c f)"), in_=stage),
               r=["stage"], w=["wbig"])
    P.dma("sp", stage[0:64, 0:512], w2c_in, "stage", w=["stage"])
    P.pool(lambda e: e.tensor_copy(out=w2c, in_=stage[0:64, 0:512]), r=["stage"], w=["w2c"])
    P.dma("sp", stage[:, 0:256], g2c_in, "stage", w=["stage"])
    P.pool(lambda e: e.tensor_copy(out=g2c, in_=stage[:, 0:256]), r=["stage"], w=["g2c"])

    pcnt = [0]

    def pbank():
        pcnt[0] += 1
        return pcnt[0] % 2

    xcnt = [0]

    def mix(j, hb):
        xcnt[0] += 1
        b = xcnt[0] % 2
        for dc in range(8):
            P.dve(lambda e: e.scalar_tensor_tensor(out=xm[b][:, dc, :], in0=xx[:, dc, :], scalar=mu[:, j * 8 + dc:j * 8 + dc + 1],
                                                   in1=hT[hb][:, dc, 1:1 + TT], op0=ALU.mult, op1=ALU.add),
                  r=["xx", "mu", f"hT{hb}"], w=[f"xm{b}"])
        return b

    def proj(b, c0, m, hb):
        pb = pbank()
        for dc in range(8):
            P.pe(lambda e: e.matmul(ps[pb][0:m, :], lhsT=wbig[:, dc, c0:c0 + m], rhs=xm[b][:, dc, :],
                                    start=(dc == 0), stop=(dc == 7)),
                 r=["wbig", f"xm{b}"], w=[f"ps{pb}"])
        return pb

    for t in range(nt):
        hb = t % 2
        ob = 1 - hb
        hsrc(hT[hb][:, :, 1:1 + TT], t, f"hT{hb}", [f"hT{hb}"])
        if t > 0:
            P.pool(lambda e: e.tensor_copy(out=hT[hb][:, :, 0:1], in_=hT[ob][:, :, TT:TT + 1]), r=[f"hT{ob}"], w=[f"hT{hb}"])
        else:
            P.pool(lambda e: e.memset(hT[hb][:, :, 0:1], 0.0), w=[f"hT{hb}"])
        P.dve(lambda e: e.tensor_tensor(out=xx, in0=hT[hb][:, :, 0:TT], in1=hT[hb][:, :, 1:1 + TT], op=ALU.subtract),
              r=[f"hT{hb}"], w=["xx"])
        for j, nm, c0 in ((0, "r", 0), (2, "k", 256), (3, "v", 512)):
            b = mix(j, hb)
            for p in range(2):
                pb = proj(b, c0 + 128 * p, 128, hb)
                P.act(lambda e: e.copy(out=A_[p][nm], in_=ps[pb]), r=[f"ps{pb}"], w=[f"{nm}{p}"])
        b = mix(1, hb)
        pb = proj(b, 768, 64, hb)
        P.act(lambda e: e.activation(out=lora[0:64, :], in_=ps[pb][0:64, :], func=AF.Tanh), r=[f"ps{pb}"], w=["lora"])
        for p in range(2):
            pb = pbank()
            P.pe(lambda e: e.matmul(ps[pb], lhsT=w2c[:, 128 * p:128 * p + 128], rhs=lora[0:64, :], start=True, stop=True),
                 r=["w2c", "lora"], w=[f"ps{pb}"])
            P.act(lambda e: e.activation(out=A_[p]["lg"], in_=ps[pb], func=AF.Sigmoid, bias=cvec[:, 8 * p:8 * p + 1]),
                  r=[f"ps{pb}", "cvec"], w=[f"lg{p}"])
            P.pool(lambda e: e.tensor_scalar(out=A_[p]["lg"], in0=A_[p]["lg"], scalar1=-DEC, scalar2=None, op0=ALU.mult),
                   r=[f"lg{p}"], w=[f"lg{p}"])
        b = mix(4, hb)
        pb = proj(b, 832, 64, hb)
        P.act(lambda e: e.copy(out=lora[0:64, :], in_=ps[pb][0:64, :]), r=[f"ps{pb}"], w=["lora"])
        for p in range(2):
            pb = pbank()
            P.pe(lambda e: e.matmul(ps[pb], lhsT=w2c[:, 256 + 128 * p:256 + 128 * p + 128], rhs=lora[0:64, :], start=True, stop=True),
                 r=["w2c", "lora"], w=[f"ps{pb}"])
            P.act(lambda e: e.activation(out=A_[p]["ag"], in_=ps[pb], func=AF.Sigmoid, bias=cvec[:, 8 * p + 1:8 * p + 2]),
                  r=[f"ps{pb}", "cvec"], w=[f"ag{p}"])
        b = mix(5, hb)
        pb = proj(b, 896, 128, hb)
        P.act(lambda e: e.activation(out=sg, in_=ps[pb], func=AF.Sigmoid), r=[f"ps{pb}"], w=["sg"])

        for p in range(2):
            a = A_[p]
            cv = lambda i: cvec[:, 8 * p + i:8 * p + i + 1]
            R = lambda *n: [f"{x}{p}" for x in n]
            P.pool(lambda e: e.tensor_scalar(out=a["kk"], in0=a["k"], scalar1=cv(2), scalar2=None, op0=ALU.mult),
                   r=R("k") + ["cvec"], w=R("kk"))
            P.pool(lambda e: e.tensor_tensor(out=a["tmp"], in0=a["kk"], in1=a["kk"], op=ALU.mult), r=R("kk"), w=R("tmp"))
            pb = pbank()
            P.pe(lambda e: e.matmul(ps[pb], lhsT=blockones, rhs=a["tmp"], start=True, stop=True), r=["cf"] + R("tmp"), w=[f"ps{pb}"])
            P.act(lambda e: e.activation(out=a["ssq"], in_=ps[pb], func=AF.Sqrt), r=[f"ps{pb}"], w=R("ssq"))
            P.dve(lambda e: e.tensor_scalar_max(out=a["ssq"], in0=a["ssq"], scalar1=1e-12), r=R("ssq"), w=R("ssq"))
            P.dve(lambda e: e.reciprocal(out=a["ssq"], in_=a["ssq"]), r=R("ssq"), w=R("ssq"))
            P.dve(lambda e: e.tensor_tensor(out=a["kk"], in0=a["kk"], in1=a["ssq"], op=ALU.mult), r=R("kk", "ssq"), w=R("kk"))
            P.dve(lambda e: e.tensor_scalar(out=a["km"], in0=a["ag"], scalar1=cv(3), scalar2=cv(4), op0=ALU.mult, op1=ALU.add),
                  r=R("ag") + ["cvec"], w=R("km"))
            P.dve(lambda e: e.tensor_tensor(out=a["km"], in0=a["km"], in1=a["k"], op=ALU.mult), r=R("km", "k"), w=R("km"))
            P.dve(lambda e: e.scalar_tensor_tensor(out=a["rkr"], in0=a["r"], scalar=cv(5), in1=a["km"], op0=ALU.mult, op1=ALU.mult),
                  r=R("r", "km") + ["cvec"], w=R("rkr"))
            P.dve(lambda e: e.tensor_tensor_scan(out=a["lp"], data0=keep, data1=a["lg"], initial=0.0, op0=ALU.mult, op1=ALU.add),
                  r=["cf"] + R("lg"), w=R("lp"))
            arv = ar[p]
            v3 = lambda x: x.rearrange("p (j l) -> p j l", l=LR)
            P.act(lambda e: e.activation(out=a["tmp"], in_=a["lp"], func=AF.Exp), r=R("lp"), w=R("tmp"))
            P.dve(lambda e: e.tensor_tensor(out=arv[:, :, 64:128], in0=v3(a["r"]), in1=v3(a["tmp"]), op=ALU.mult),
                  r=R("r", "tmp"), w=R("ar"))
            P.pool(lambda e: e.tensor_tensor(out=a["tmp2"], in0=a["lp"], in1=a["lg"], op=ALU.subtract), r=R("lp", "lg"), w=R("tmp2"))
            P.act(lambda e: e.activation(out=a["tmp2"], in_=a["tmp2"], func=AF.Exp), r=R("tmp2"), w=R("tmp2"))
            P.dve(lambda e: e.scalar_tensor_tensor(out=arv[:, :, 0:64], in0=v3(a["kk"]), scalar=-1.0, in1=v3(a["tmp2"]),
                                                   op0=ALU.mult, op1=ALU.mult),
                  r=R("kk", "tmp2"), w=R("ar"))
            P.act(lambda e: e.activation(out=a["tmp2"], in_=a["lp"], func=AF.Exp, scale=-1.0), r=R("lp"), w=R("tmp2"))
            P.pool(lambda e: e.tensor_tensor(out=a["bt"], in0=a["kk"], in1=a["ag"], op=ALU.mult), r=R("kk", "ag"), w=R("bt"))
            P.pool(lambda e: e.tensor_tensor(out=a["bt"], in0=a["bt"], in1=a["tmp2"], op=ALU.mult), r=R("bt", "tmp2"), w=R("bt"))
            P.dve(lambda e: e.tensor_tensor(out=a["kt"], in0=a["km"], in1=a["tmp2"], op=ALU.mult), r=R("km", "tmp2"), w=R("kt"))

            for j in range(NJ):
                c = NJ * t + j
                cs = slice(j * LR, (j + 1) * LR)
                PL = a["tmp"][:, j * LR + LR - 1:j * LR + LR]
                H = [slice(0, 64), slice(64, 128)]
                for hp in H:
                    P.pe(lambda e: e.matmul(ps[2][hp, 0:128], lhsT=a["bt"][hp, cs], rhs=arv[hp, j, :], start=True, stop=True),
                         r=R("bt", "ar"), w=["ps2"])
                    P.pe(lambda e: e.matmul(ps[2][hp, 128:256], lhsT=a["kt"][hp, cs], rhs=arv[hp, j, :], start=True, stop=True),
                         r=R("kt", "ar"), w=["ps2"])
                    P.pe(lambda e: e.matmul(ps[2][hp, 256:320], lhsT=arv[hp, j, 0:64], rhs=a["bt"][hp, cs], start=True, stop=True),
                         r=R("bt", "ar"), w=["ps2"])
                P.dve(lambda e: e.tensor_tensor(out=AM, in0=ps[2][:, 0:320], in1=mask320, op=ALU.mult), r=["ps2", "cf"], w=["AM"])
                P.pool(lambda e: e.tensor_scalar(out=bkP[:, 0:64], in0=a["bt"][:, cs], scalar1=PL, scalar2=None, op0=ALU.mult),
                       r=R("bt", "tmp"), w=["bkP"])
                P.pool(lambda e: e.tensor_scalar(out=bkP[:, 64:128], in0=a["kt"][:, cs], scalar1=PL, scalar2=None, op0=ALU.mult),
                       r=R("kt", "tmp"), w=["bkP"])
                for hp in H:
                    P.pe(lambda e: e.matmul(ps[3][hp, 0:64], lhsT=a["v"][hp, cs], rhs=identS[hp, :], start=True, stop=True),
                         r=R("v") + ["cf"], w=["ps3"])
                    P.pe(lambda e: e.matmul(ps[3][hp, 64:128], lhsT=bkP[hp, 0:64], rhs=identS[hp, :], start=True, stop=True),
                         r=["bkP", "cf"], w=["ps3"])
                    P.pe(lambda e: e.matmul(ps[3][hp, 128:192], lhsT=bkP[hp, 64:128], rhs=identS[hp, :], start=True, stop=True),
                         r=["bkP", "cf"], w=["ps3"])
                P.act(lambda e: e.copy(out=tok, in_=ps[3][:, 0:192]), r=["ps3"], w=["tok"])
                vtok = tok[:, 0:64]
                btPtok = tok[:, 64:128]
                ktPtok = tok[:, 128:192]
                for hp in H:
                    P.pe(lambda e: e.matmul(ps[4][hp, 0:64], lhsT=arv[hp, j, 0:64], rhs=S0T[p][hp, :], start=True, stop=False),
                         r=R("ar", "S0T"), w=["ps4"])
                    P.pe(lambda e: e.matmul(ps[4][hp, 0:64], lhsT=AM[hp, 128:192], rhs=vtok[hp, :], start=False, stop=True),
                         r=["AM", "tok"], w=["ps4"])
                P.act(lambda e: e.copy(out=Y, in_=ps[4][:, 0:64]), r=["ps4"], w=["Y"])
                Acur = AM[:, 0:64]
                ATcur = AM[:, 256:320]
                an, atn = "AM", "AM"
                for kq in range(6):
                    for hp in H:
                        P.pe(lambda e: e.matmul(ps[4][hp, 0:64], lhsT=Acur[hp, :], rhs=Y[hp, :], start=True, stop=True),
                             r=[an, "Y"], w=["ps4"])
                    P.dve(lambda e: e.tensor_tensor(out=Y, in0=ps[4][:, 0:64], in1=Y, op=ALU.add), r=["ps4", "Y"], w=["Y"])
                    if kq < 5:
                        for hp in H:
                            P.pe(lambda e: e.matmul(ps[5][hp, 0:64], lhsT=ATcur[hp, :], rhs=Acur[hp, :], start=True, stop=True),
                                 r=[an], w=["ps5"])
                            P.pe(lambda e: e.matmul(ps[5][hp, 64:128], lhsT=Acur[hp, :], rhs=ATcur[hp, :], start=True, stop=True),
                                 r=[an], w=["ps5"])
                        nb = kq % 2
                        P.act(lambda e: e.copy(out=An[nb], in_=ps[5][:, 0:128]), r=["ps5"], w=[f"An{nb}"])
                        Acur = An[nb][:, 0:64]
                        ATcur = An[nb][:, 64:128]
                        an = f"An{nb}"
                for hi, hp in enumerate(H):
                    P.pe(lambda e: e.matmul(ps[6][hp, 0:64], lhsT=arv[hp, j, 64:128], rhs=S0T[p][hp, :], start=True, stop=False),
                         r=R("ar", "S0T"), w=["ps6"])
                    P.pe(lambda e: e.matmul(ps[6][hp, 0:64], lhsT=AM[hp, 64:128], rhs=Y[hp, :], start=False, stop=False),
                         r=["AM", "Y"], w=["ps6"])
                    P.pe(lambda e: e.matmul(ps[6][hp, 0:64], lhsT=AM[hp, 192:256], rhs=vtok[hp, :], start=False, stop=True),
                         r=["AM", "tok"], w=["ps6"])
                    P.pe(lambda e: e.matmul(ps[6][hp, 64:65], lhsT=a["rkr"][hp, cs], rhs=onescol[hp, :], start=True, stop=True),
                         r=R("rkr") + ["cf"], w=["ps6"])
                    P.pe(lambda e: e.matmul(ps[6][hp, 128:192], lhsT=sg[:, cs], rhs=g2c[:, 128 * p + 64 * hi:128 * p + 64 * hi + 64],
                                            start=True, stop=True),
                         r=["sg", "g2c"], w=["ps6"])
                for hp in H:
                    P.pe(lambda e: e.matmul(ps[7][hp, 0:64], lhsT=btPtok[hp, :], rhs=Y[hp, :], start=True, stop=False),
                         r=["tok", "Y"], w=["ps7"])
                    P.pe(lambda e: e.matmul(ps[7][hp, 0:64], lhsT=ktPtok[hp, :], rhs=vtok[hp, :], start=False, stop=True),
                         r=["tok"], w=["ps7"])
                P.dve(lambda e: e.scalar_tensor_tensor(out=S0T[p], in0=S0T[p], scalar=PL, in1=ps[7][:, 0:64], op0=ALU.mult, op1=ALU.add),
                      r=R("S0T", "tmp") + ["ps7"], w=R("S0T"))
                P.dve(lambda e: e.bn_stats(out=st6, in_=ps[6][:, 0:64]), r=["ps6"], w=["st6"])
                P.dve(lambda e: e.bn_aggr(out=mv, in_=st6), r=["st6"], w=["mv"])
                P.act(lambda e: e.activation(out=rs, in_=mv[:, 1:2], func=AF.Sqrt, bias=GN_EPS), r=["mv"], w=["rs"])
                P.dve(lambda e: e.reciprocal(out=rs, in_=rs), r=["rs"], w=["rs"])
                P.dve(lambda e: e.tensor_scalar(out=yn, in0=ps[6][:, 0:64], scalar1=mv[:, 0:1], scalar2=rs, op0=ALU.subtract, op1=ALU.mult),
                      r=["ps6", "mv", "rs"], w=["yn"])
                P.pool(lambda e: e.tensor_tensor(out=yn, in0=yn, in1=lnwb[:, 128 * p:128 * p + 64], op=ALU.mult), r=["yn", "lnwb"], w=["yn"])
                P.pool(lambda e: e.tensor_tensor(out=yn, in0=yn, in1=lnwb[:, 128 * p + 64:128 * p + 128], op=ALU.add), r=["yn", "lnwb"], w=["yn"])
                P.act(lambda e: e.copy(out=bsc, in_=ps[6][:, 64:65]), r=["ps6"], w=["bsc"])
                P.dve(lambda e: e.scalar_tensor_tensor(out=yn, in0=vtok, scalar=bsc, in1=yn, op0=ALU.mult, op1=ALU.add),
                      r=["tok", "bsc", "yn"], w=["yn"])
                P.dve(lambda e: e.tensor_tensor(out=yo, in0=ps[6][:, 128:192], in1=yn, op=ALU.mult),
                      r=["ps6", "yn"], w=["yo"])
                for hp in H:
                    P.pe(lambda e: e.matmul(ps[3][hp, 256:320], lhsT=yo[hp, :], rhs=identS[hp, :], start=True, stop=True),
                         r=["yo", "cf"], w=["ps3"])
                P.act(lambda e: e.copy(out=ofm[p][:, cs], in_=ps[3][:, 256:320]), r=["ps3"], w=[f"ofm{p}"])
            P.dma("sp", o_out[128 * p:128 * p + 128, tsl(t)], ofm[p], "oout", r=[f"ofm{p}"])

import ml_dtypes as _mld

_PROGS = {}


def _prog(key, fn):
    if key not in _PROGS:
        _PROGS[key] = fn()
    return _PROGS[key]


def _gl(g):
    return np.ascontiguousarray(np.asarray(g, np.float32).reshape(8, 128).T)


def _mla_inputs(z, j, hT_b, pos_b, c):
    hp = c % 4
    wa = z['mla_w_a'][j]
    sw = np.concatenate([np.arange(32, 64), np.arange(0, 32)])
    wa_ext = np.concatenate([wa, wa[:, 768:832][:, sw]], axis=1)
    wqb = z['mla_w_qb'][j]
    wkvb = z['mla_w_kvb'][j]
    cols = []
    for hh in range(2):
        hd = 2 * hp + hh
        blk = wqb[:, hd * 192:(hd + 1) * 192]
        cols.append(np.concatenate([blk[:, :128], blk[:, 128:192], blk[:, 128:192][:, sw]], axis=1))
    wqb_c = np.concatenate(cols, axis=1)
    wkvb_c = np.concatenate([wkvb[:, (2 * hp + hh) * 256:(2 * hp + hh + 1) * 256] for hh in range(2)], axis=1)
    consts = np.zeros((128, 8), np.float32)
    consts[:, 0:4] = z['mla_q_norm'][j].reshape(4, 128).T
    consts[:, 4:6] = z['mla_kv_norm'][j].reshape(2, 128).T
    inv = 1.0 / (10000.0 ** (np.arange(0, 64, 2, dtype=np.float32) / 64))
    consts[0:64, 6] = np.concatenate([inv, inv]) / (2 * math.pi)
    consts[0:64, 7] = np.concatenate([-np.ones(32), np.ones(32)])
    k = np.arange(128)[:, None]
    q = np.arange(512)[None, :]
    masks = np.stack([((jj * 128 + k) <= q) for jj in range(4)], axis=1).astype(_mld.bfloat16)
    return {'h_in': hT_b, 'pos': np.ascontiguousarray(pos_b.reshape(1, -1).astype(np.int32)),
            'wa': np.ascontiguousarray(wa_ext), 'wqb': np.ascontiguousarray(wqb_c),
            'wkvb': np.ascontiguousarray(wkvb_c), 'consts': consts, 'masks': np.ascontiguousarray(masks)}


def _mls_inputs(z, j, hT_b, c):
    hp = c % 4
    hA, hB = 2 * hp, 2 * hp + 1
    W = z['ml_w_in'][j]
    cols = np.concatenate([np.arange(hA * 64, hA * 64 + 128), 512 + np.arange(hA * 64, hA * 64 + 128),
                           1024 + np.arange(hA * 128, hA * 128 + 256), 2048 + np.arange(hA * 128, hA * 128 + 256),
                           np.array([3072 + hA, 3072 + hB, 3080 + hA, 3080 + hB])])
    w_c = np.ascontiguousarray(W[:, cols])
    consts = np.zeros((128, 16), np.float32)
    cw = z['ml_conv_w'][j]
    cb = z['ml_conv_b'][j]
    consts[:, 0:4] = cw[:, hA * 64:hA * 64 + 128].T
    consts[:, 4:8] = cw[:, 512 + hA * 64:512 + hA * 64 + 128].T
    consts[:, 8] = cb[hA * 64:hA * 64 + 128]
    consts[:, 9] = cb[512 + hA * 64:512 + hA * 64 + 128]
    on = z['ml_out_norm'][j]
    consts[:, 10] = on[hA * 128:(hA + 1) * 128]
    consts[:, 11] = on[hB * 128:(hB + 1) * 128]
    bif = z['ml_b_if'][j]
    consts[0:2, 12] = bif[[hA, hB]]
    consts[0:2, 13] = -bif[[8 + hA, 8 + hB]]
    cm = np.zeros((128, 256), np.float32)
    cm[:, 0:128] = np.eye(128)
    s = np.arange(128)[:, None]
    t = np.arange(128)[None, :]
    cm[:, 128:256] = (s <= t)
    sel = np.zeros((2, 386), np.float32)
    sel[0, 0:128] = 1
    sel[1, 128:256] = 1
    sel[0, 256:320] = 1
    sel[1, 320:384] = 1
    sel[0, 384] = 1
    sel[1, 385] = 1
    return {'h_in': hT_b, 'w': w_c, 'consts': consts, 'cm': cm.astype(_mld.bfloat16), 'sel': sel}


def _rwk_consts():
    cf = np.zeros((128, 1025), np.float32)
    s = np.arange(64)[:, None]
    t = np.arange(64)[None, :]
    su = (s < t).astype(np.float32)
    ui = (s <= t).astype(np.float32)
    m = np.concatenate([su, ui, su, ui, su.T], axis=1)
    cf[:, 0:320] = np.concatenate([m, m], axis=0)
    cf[:, 320:384] = np.concatenate([np.eye(64), np.eye(64)], axis=0)
    cf[:, 384] = 1.0
    bo = np.zeros((128, 128), np.float32)
    bo[:64, :64] = 1
    bo[64:, 64:] = 1
    cf[:, 385:513] = bo
    keep = np.ones(512, np.float32)
    keep[::64] = 0
    cf[:, 513:1025] = keep[None, :]
    return cf


def _rwk_inputs(z, j, hT_b, c):
    q = c % 4
    ch = slice(q * 256, (q + 1) * 256)
    wbig = np.concatenate([z['rw_w_r'][j][:, ch], z['rw_w_k'][j][:, ch], z['rw_w_v'][j][:, ch],
                           z['rw_w1'][j], z['rw_a1'][j], z['rw_g1'][j]], axis=1)
    w2c = np.concatenate([z['rw_w2'][j][:, ch], z['rw_a2'][j][:, ch]], axis=1)
    g2c = z['rw_g2'][j][:, ch]
    mu = z['rw_mu'][j]
    mu_l = np.ascontiguousarray(mu.reshape(6, 8, 128).transpose(2, 0, 1).reshape(128, 48))
    cvec = np.zeros((128, 16), np.float32)
    rk = z['rw_r_k'][j].reshape(-1)
    one = np.ones(128, np.float32)
    for p in range(2):
        cc = slice(q * 256 + p * 128, q * 256 + (p + 1) * 128)
        cvec[:, 8 * p + 0] = z['rw_w0'][j][cc]
        cvec[:, 8 * p + 1] = z['rw_a0'][j][cc]
        cvec[:, 8 * p + 2] = z['rw_k_k'][j][cc]
        cvec[:, 8 * p + 3] = z['rw_k_a'][j][cc]
        cvec[:, 8 * p + 4] = 1.0 - z['rw_k_a'][j][cc]
        cvec[:, 8 * p + 5] = rk[cc]
    lnwb = np.zeros((128, 256), np.float32)
    for p in range(2):
        for h in range(2):
            cc = slice(q * 256 + p * 128 + h * 64, q * 256 + p * 128 + (h + 1) * 64)
            lnwb[h * 64:(h + 1) * 64, p * 128:p * 128 + 64] = z['rw_ln_w'][j][cc][None, :]
            lnwb[h * 64:(h + 1) * 64, p * 128 + 64:p * 128 + 128] = z['rw_ln_b'][j][cc][None, :]
    return {'h_in': hT_b, 'wbig': np.ascontiguousarray(wbig), 'w2c': np.ascontiguousarray(w2c),
            'g2c': np.ascontiguousarray(g2c), 'mu': mu_l, 'cvec': cvec, 'lnwb': lnwb, 'cf32': _rwk_consts()}


def _build_fused(nl=4):
    nc = bass.Bass("TRN2", target_bir_lowering=False)
    C = Ctx(nc)
    P = C.P
    dt = nc.dram_tensor
    RG = [[0, 1, 2, 3], [4, 5, 6, 7]]
    U32 = mybir.dt.uint32
    fm = lambda ap: ap.rearrange("(c p) t -> p c t", p=128)
    x_ext = fm(dt("x_in", [1024, 2048], F32, kind="ExternalInput").ap())
    y_ext = fm(dt("y_out", [1024, 2048], F32, kind="ExternalOutput").ap())
    idx_in = dt("idx", [128, 8], U32, kind="ExternalInput").ap()
    xbuf = fm(dt("xbuf", [1024, 2048], F32).ap())
    hbuf_t = dt("hbuf", [1024, 2048], BF16).ap()
    hall_t = dt("hall", [4096, 2048], BF16).ap()
    opart_t = dt("opart", [256, 8192], BF16).ap()
    oall_t = dt("oall", [1024, 8192], BF16).ap()
    hall_v = hall_t.rearrange("(k q c p) f -> q p k c f", k=4, q=4, p=128)
    oall_rows = oall_t.rearrange("r (q f) -> (r q) f", q=4)

    def hsrc(dst, t, key, wn):
        for k in range(4):
            P.dma("sp", dst[:, 2 * k:2 * k + 2, :], hall_v[t // 4][:, k, :, (t % 4) * 512:(t % 4 + 1) * 512], key, w=wn)

    def ffw_decl(l, i):
        wv = lambda ap: ap.rearrange("(c p) f -> p c f", p=128)
        return (wv(dt(f"wg_{l}_{i}", [1024, 2816], F32, kind="ExternalInput").ap()),
                wv(dt(f"wu_{l}_{i}", [1024, 2816], F32, kind="ExternalInput").ap()),
                wv(dt(f"wd_{l}_{i}", [2816, 1024], F32, kind="ExternalInput").ap()))

    def gather_h():
        P.barrier()
        for k in range(4):
            P.op("pool", lambda e: e.collective_compute("AllGather", ALU.bypass, replica_groups=RG,
                                                         ins=[hbuf_t[k * 256:(k + 1) * 256, :].opt()],
                                                         outs=[hall_t[k * 1024:(k + 1) * 1024, :].opt()]),
                 r=[], w=[f"hall{k}"], dma="cc", inc=1)
        P.barrier()
        C.reset()

    def gather_o():
        P.barrier()
        for k in range(4):
            P.op("pool", lambda e: e.collective_compute("AllGather", ALU.bypass, replica_groups=RG,
                                                         ins=[opart_t[k * 64:(k + 1) * 64, :].opt()],
                                                         outs=[oall_t[k * 256:(k + 1) * 256, :].opt()]),
                 r=[], w=[f"oall{k}"], dma="cc", inc=1)
        P.barrier()
        C.reset()

    g0 = dt("gains_a", [128, 16], F32, kind="ExternalInput").ap()
    emit_tp(C, {"x_in": x_ext, "ffw": [ffw_decl(0, 0)], "gains": g0, "x_out": xbuf, "h_out": fm(hbuf_t)}, 1, False, True, False)
    gather_h()
    for l in range(nl):
        kind = l % 3
        wv = lambda ap: ap.rearrange("(c p) f -> p c f", p=128)
        if kind == 0:
            io = {"hsrc": hsrc, "pos": dt(f"m{l}_pos", [1, 8192], I32, kind="ExternalInput").ap(),
                  "wa": wv(dt(f"m{l}_wa", [1024, 896], F32, kind="ExternalInput").ap()),
                  "wqb": wv(dt(f"m{l}_wqb", [512, 512], F32, kind="ExternalInput").ap()),
                  "wkvb": wv(dt(f"m{l}_wkvb", [256, 512], F32, kind="ExternalInput").ap()),
                  "consts": dt(f"m{l}_consts", [128, 8], F32, kind="ExternalInput").ap(),
                  "masks": dt(f"m{l}_masks", [128, 4, 512], BF16, kind="ExternalInput").ap(),
                  "o_out": opart_t}
            emit_mla(C, io)
        elif kind == 1:
            io = {"hsrc": hsrc, "w": wv(dt(f"m{l}_w", [1024, 772], F32, kind="ExternalInput").ap()),
                  "consts": dt(f"m{l}_consts", [128, 16], F32, kind="ExternalInput").ap(),
                  "cm": dt(f"m{l}_cm", [128, 256], BF16, kind="ExternalInput").ap(),
                  "sel": dt(f"m{l}_sel", [2, 386], F32, kind="ExternalInput").ap(),
                  "o_out": opart_t}
            emit_mls(C, io)
        else:
            io = {"hsrc": hsrc, "wbig": wv(dt(f"m{l}_wbig", [1024, 1024], F32, kind="ExternalInput").ap()),
                  "w2c": dt(f"m{l}_w2c", [64, 512], F32, kind="ExternalInput").ap(),
                  "g2c": dt(f"m{l}_g2c", [128, 256], F32, kind="ExternalInput").ap(),
                  "mu": dt(f"m{l}_mu", [128, 48], F32, kind="ExternalInput").ap(),
                  "cvec": dt(f"m{l}_cvec", [128, 16], F32, kind="ExternalInput").ap(),
                  "lnwb": dt(f"m{l}_lnwb", [128, 256], F32, kind="ExternalInput").ap(),
                  "cf32": dt(f"m{l}_cf32", [128, 1025], F32, kind="ExternalInput").ap(),
                  "o_out": opart_t}
            emit_rwk(C, io)
        gather_o()
        wo = wv(dt(f"wo_{l}", [1024, 1024], F32, kind="ExternalInput").ap())
        if l < nl - 1:
            gk = dt(f"gains_{l}", [128, 24], F32, kind="ExternalInput").ap()
            emit_tp(C, {"x_in": xbuf, "oall": oall_rows, "wo": wo, "idx": idx_in, "ffw": [ffw_decl(l, 1), ffw_decl(l + 1, 0)],
                        "gains": gk, "x_out": xbuf, "h_out": fm(hbuf_t)}, 2, True, True, False)
            gather_h()
        else:
            gk = dt(f"gains_{l}", [128, 16], F32, kind="ExternalInput").ap()
            emit_tp(C, {"x_in": xbuf, "oall": oall_rows, "wo": wo, "idx": idx_in, "ffw": [ffw_decl(l, 1)],
                        "gains": gk, "y_out": y_ext}, 1, True, False, True)
    P.emit()
    return nc


def kernel(nl=4, **z):
    z = {k: np.asarray(v) for k, v in z.items()}
    cores = list(range(8))
    x = z['x'].astype(np.float32)
    nc = _prog(('fused', nl), lambda: _build_fused(nl))
    ims = []
    pp = np.arange(128)[:, None]
    cc = np.arange(8)[None, :]
    for c in cores:
        b, q = c // 4, c % 4
        m = {'x_in': np.ascontiguousarray(x[b, q * 2048:(q + 1) * 2048, :].T),
             'idx': (((((cc * 128 + pp) % 256) // 64) * 256 + ((cc * 128 + pp) // 256) * 64 + (cc * 128 + pp) % 64) * 4 + q).astype(np.uint32)}
        for l in range(nl):
            for i in range(2):
                if l == nl - 1 and i == 1 and False:
                    continue
                m[f'wg_{l}_{i}'] = z['ffn_w_gate'][l, i]
                m[f'wu_{l}_{i}'] = z['ffn_w_up'][l, i]
                m[f'wd_{l}_{i}'] = z['ffn_w_down'][l, i]
        m['gains_a'] = np.concatenate([_gl(z['ffn_norm'][0, 0]), _gl(z['mix_norm'][0])], axis=1)
        dummy_h = None
        for l in range(nl):
            kind, j = l % 3, l // 3
            if kind == 0:
                d = _mla_inputs(z, j, dummy_h, z['positions'][b], c)
                wo = z['mla_w_o'][j]
            elif kind == 1:
                d = _mls_inputs(z, j, dummy_h, c)
                wo = z['ml_w_o'][j]
            else:
                d = _rwk_inputs(z, j, dummy_h, c)
                wo = z['rw_w_o'][j]
            for k, v in d.items():
                if k != 'h_in':
                    m[f'm{l}_{k}'] = v
            m[f'wo_{l}'] = np.ascontiguousarray(wo)
            if l < nl - 1:
                m[f'gains_{l}'] = np.concatenate([_gl(z['ffn_norm'][l, 1]), _gl(z['ffn_norm'][l + 1, 0]),
                                                  _gl(z['mix_norm'][l + 1])], axis=1)
            else:
                m[f'gains_{l}'] = np.concatenate([_gl(z['ffn_norm'][l, 1]), _gl(z['final_norm'])], axis=1)
        ims.append(m)
    res = run_bass_kernel_spmd(nc, ims, core_ids=cores)
    out = np.zeros((2, 8192, 1024), np.float32)
    for c in cores:
        out[c // 4, (c % 4) * 2048:(c % 4 + 1) * 2048, :] = res.results[c]['y_out'].T
    return out
```

```python
import math
from concourse.bass_utils import run_bass_kernel_spmd
import numpy as np
import concourse.bass as bass
import concourse.mybir as mybir

F32 = mybir.dt.float32
BF16 = mybir.dt.bfloat16
I32 = mybir.dt.int32
ALU = mybir.AluOpType
AF = mybir.ActivationFunctionType
AX = mybir.AxisListType

SAME_ENG_SYNC = True


class _Op:
    __slots__ = ("eng", "fn", "deps", "dma", "marked", "val", "sem", "inc", "phase")


class _Rec:
    def __init__(self):
        self.calls = []

    def __getattr__(self, name):
        def f(*a, **k):
            self.calls.append((name, a, k))
            return self
        return f


def _replay(calls):
    def fn(eng):
        ins = None
        for name, a, k in calls:
            ins = getattr(eng, name)(*a, **k)
        return ins
    return fn


class Prog:
    ENG = ("pe", "act", "dve", "pool", "sp")

    def __init__(self, nc):
        self.nc = nc
        self.ops = {e: [] for e in self.ENG}
        self.res_w = {}
        self.res_r = {}
        self.out_dmas = []
        self.all = []
        self.phase = 0
        self.bar_deps = []
        self.passed = set()
        self.last_compute = {}
        self.last_dma = {}

    def op(self, eng, fn, r=(), w=(), dma=None, out=False, inc=None):
        o = _Op()
        o.eng = eng
        rec = _Rec()
        fn(rec)
        o.fn = _replay(rec.calls)
        o.dma = dma
        o.inc = inc if inc is not None else (16 if dma is not None else 1)
        o.marked = dma is not None
        o.val = 0
        o.sem = None
        deps = []
        for x in r:
            d = self.res_w.get(x)
            if d is not None:
                deps.append((d, 0))
        for x in w:
            d = self.res_w.get(x)
            if d is not None:
                deps.append((d, 0))
            for d in self.res_r.get(x, ()):
                deps.append((d, 1))
        for x in w:
            self.res_w[x] = o
            self.res_r[x] = []
        for x in r:
            if x not in w:
                self.res_r.setdefault(x, []).append(o)
        if eng not in self.passed:
            self.passed.add(eng)
            deps.extend((d, 0) for d in self.bar_deps)
        o.deps = deps
        o.phase = self.phase
        if dma is not None:
            self.last_dma[dma] = o
        else:
            self.last_compute[eng] = o
        self.ops[eng].append(o)
        self.all.append(o)
        if out:
            self.out_dmas.append(o)
        return o

    def barrier(self):
        self.bar_deps = list(self.last_compute.values()) + list(self.last_dma.values())
        self.passed = set()
        self.res_w = {}
        self.res_r = {}
        self.phase += 1

    def pe(self, fn, r=(), w=()):
        return self.op("pe", fn, r, w)

    def act(self, fn, r=(), w=()):
        return self.op("act", fn, r, w)

    def dve(self, fn, r=(), w=()):
        return self.op("dve", fn, r, w)

    def pool(self, fn, r=(), w=()):
        return self.op("pool", fn, r, w)

    def dma(self, q, out_ap, in_ap, key, r=(), w=(), out=False, **kw):
        return self.op(q, lambda e: e.dma_start(out=out_ap, in_=in_ap, **kw), r, w, dma=key, out=out)

    def _need(self, o, d, kind):
        if d is o:
            return False
        if d.dma is not None:
            return True
        if d.eng != o.eng:
            return True
        if o.eng == "pe":
            return False
        if kind == 1:
            return False
        return SAME_ENG_SYNC

    def emit(self):
        nc = self.nc
        fin = _Op()
        fin.eng = "sp"; fin.fn = None; fin.dma = None; fin.marked = False; fin.val = 0; fin.sem = None; fin.inc = 1
        fin.deps = [(d, 0) for d in self.out_dmas] + [(d, 0) for d in self.last_dma.values()]
        fin.phase = self.phase
        self.ops["sp"].append(fin)
        for e in self.ENG:
            for o in self.ops[e]:
                o.deps = [(d, k) for (d, k) in o.deps if self._need(o, d, k)]
                for d, k in o.deps:
                    d.marked = True
        import contextlib
        stack = contextlib.ExitStack()
        esem = {}
        dsem = {}
        dcnt = {}
        cnt = {}
        for e in ("all",):
            for o in self.all:
                e = o.eng
                if o.dma is not None:
                    if o.dma not in dsem:
                        dsem[o.dma] = stack.enter_context(nc.semaphore("d_" + o.dma))
                        dcnt[o.dma] = 0
                    dcnt[o.dma] += o.inc
                    o.sem = dsem[o.dma]
                    o.val = dcnt[o.dma]
                elif o.marked:
                    ek = (e, o.phase // 4)
                    if ek not in esem:
                        esem[ek] = stack.enter_context(nc.semaphore("s_%s_%d" % ek))
                        cnt[ek] = 0
                    cnt[ek] += 1
                    o.sem = esem[ek]
                    o.val = cnt[ek]
        self.nsem = len(dsem) + len(esem)
        engobj = {"pe": "tensor", "act": "scalar", "dve": "vector", "pool": "gpsimd", "sp": "sync"}
        ops = self.ops

        def run(ename, eng):
            seen = {}
            for o in ops[ename]:
                waits = {}
                for d, k in o.deps:
                    key = d.sem.name
                    if seen.get(key, 0) < d.val:
                        seen[key] = d.val
                        waits[key] = (d.sem, d.val)
                for key, (s, v) in waits.items():
                    eng.wait_ge(s, v)
                if o.fn is None:
                    continue
                ins = o.fn(eng)
                if o.marked:
                    ins.then_inc(o.sem, o.inc)

        with nc.Block() as block:
            @block.tensor
            def _(e):
                run("pe", e)

            @block.scalar
            def _(e):
                run("act", e)

            @block.vector
            def _(e):
                run("dve", e)

            @block.gpsimd
            def _(e):
                run("pool", e)

            @block.sync
            def _(e):
                run("sp", e)
        stack.close()


class _T:
    def __init__(self, ap):
        self._ap = ap

    def ap(self):
        return self._ap


class Ctx:
    def __init__(self, nc, arena_f32=51000):
        self.nc = nc
        self.P = Prog(nc)
        self.big = nc.alloc_sbuf_tensor("arena", [128, arena_f32], F32).ap()
        self.cap = arena_f32
        self.off = 0
        self.ps = [nc.alloc_psum_tensor("ps%d" % i, [128, 512], F32).ap() for i in range(8)]

    def reset(self):
        self.off = 0

    def sb(self, name, shape, dtype):
        p = shape[0]
        n = 1
        for d in shape[1:]:
            n *= d
        esz = 4 if dtype in (F32, I32, mybir.dt.uint32) else 2
        n4 = (n * esz + 3) // 4
        n4 = (n4 + 7) // 8 * 8
        assert self.off + n4 <= self.cap, ("SBUF arena overflow", name, self.off, n4, self.cap)
        ap = self.big[0:p, self.off:self.off + n4]
        self.off += n4
        if dtype != F32:
            ap = ap.bitcast(dtype)
        ap = ap[:, 0:n]
        if len(shape) == 3:
            ap = ap.rearrange("p (a b) -> p a b", a=shape[1])
        elif len(shape) != 2:
            raise ValueError(shape)
        return _T(ap)


D = 1024
FF = 2816
T = 2048
TT = 512
NTT = T // TT
FG = 256
NFG = FF // FG
EPS = 1e-6


def emit_tp(CX, io, n_ffn, do_wo, do_mix, do_final):
    nc = CX.nc
    P = CX.P
    x_in = io["x_in"]
    if do_wo:
        oall = io["oall"]
        wo_in = io["wo"]
        idx_in = io["idx"]
    ffw = io["ffw"]
    n_gain = n_ffn + (1 if (do_mix or do_final) else 0)
    g_in = io["gains"]
    if do_final:
        y_out = io["y_out"]
    else:
        x_out = io["x_out"]
    if do_mix:
        h_out = io["h_out"]

    sb = CX.sb
    x = sb("x", [128, 8, T], F32).ap()
    h = sb("h", [128, 8, T], BF16).ap()
    gains = sb("gains_sb", [128, max(n_gain, 1) * 8], F32).ap()
    ones = sb("ones", [128, 128], BF16).ap()
    sq = sb("sq", [128, 8, TT], BF16).ap()
    sd = sb("sd", [128, TT], F32).ap()
    rstd = sb("rstd", [128, TT], F32).ap()
    wst = [[sb(f"wst{i}_{j}", [128, 8 * FG], F32).ap() for j in range(3)] for i in range(2)]
    wbf = [[sb(f"wbf{i}_{j}", [128, 8 * FG], BF16).ap() for j in range(3)] for i in range(2)]
    a_sb = [sb(f"a{i}", [128, TT], F32).ap() for i in range(2)]
    z = [sb(f"z{i}", [128, 2, TT], BF16).ap() for i in range(2)]
    ps = CX.ps
    idx = sb("idx", [128, 8], mybir.dt.uint32).ap()

    P.pool(lambda e: e.memset(ones, 1.0), w=["ones"])
    P.dma("sp", gains, g_in, "gains", w=["gains"])
    for tt in range(NTT):
        P.dma("sp", x[:, :, tt * TT:(tt + 1) * TT], x_in[:, :, tt * TT:(tt + 1) * TT], f"x{tt}", w=[f"x{tt}"])

    def tsl(tt):
        return slice(tt * TT, (tt + 1) * TT)

    def norm_stats(tt):
        P.act(lambda e: e.activation(out=sq, in_=x[:, :, tsl(tt)], func=AF.Square), r=[f"x{tt}"], w=["sq"])
        for dc in range(8):
            P.pe(lambda e, dc=dc: e.matmul(ps[6], lhsT=ones, rhs=sq[:, dc, :], start=(dc == 0), stop=(dc == 7)),
                 r=["ones", "sq"], w=["ps6"])
        P.act(lambda e: e.activation(out=sd, in_=ps[6], func=AF.Sqrt, bias=EPS, scale=1.0 / D), r=["ps6"], w=["sd"])
        P.dve(lambda e: e.reciprocal(out=rstd, in_=sd), r=["sd"], w=["rstd"])

    def norm_to_h(tt, gi):
        norm_stats(tt)
        for dc in range(8):
            P.dve(lambda e, dc=dc: e.scalar_tensor_tensor(
                out=h[:, dc, tsl(tt)], in0=x[:, dc, tsl(tt)], scalar=gains[:, gi * 8 + dc:gi * 8 + dc + 1],
                in1=rstd, op0=ALU.mult, op1=ALU.mult),
                r=[f"x{tt}", "gains", "rstd"], w=[f"h{tt}"])

    if do_wo:
        P.dma("sp", idx, idx_in, "idx", w=["idx"])
        for c in range(8):
            P.op("pool", lambda e: e.indirect_dma_start(out=h[:, c, :], out_offset=None, in_=oall,
                                                         in_offset=bass.IndirectOffsetOnAxis(ap=idx[:, c:c + 1], axis=0)),
                 r=["idx"], w=[f"h{tt}" for tt in range(NTT)], dma="ogat")
        for g in range(4):
            b = g % 2
            P.dma("sp", wst[b][0].rearrange("p (c f) -> p c f", c=8), wo_in[:, :, g * FG:(g + 1) * FG],
                  f"wst{b}0", w=[f"wst{b}0"])
            P.pool(lambda e, b=b: e.tensor_copy(out=wbf[b][0], in_=wst[b][0]), r=[f"wst{b}0"], w=[f"wbf{b}0"])
            wv = wbf[b][0].rearrange("p (c f) -> p c f", c=8)
            for tt in range(NTT):
                for j in range(2):
                    dc_out = g * 2 + j
                    pb = (tt * 2 + j) % 2
                    for kc in range(8):
                        P.pe(lambda e, kc=kc, j=j, tt=tt, pb=pb, wv=wv: e.matmul(
                            ps[4 + pb], lhsT=wv[:, kc, j * 128:(j + 1) * 128], rhs=h[:, kc, tsl(tt)],
                            start=(kc == 0), stop=(kc == 7)),
                            r=[f"wbf{b}0", f"h{tt}"], w=[f"ps{4 + pb}"])
                    P.dve(lambda e, dc_out=dc_out, tt=tt, pb=pb: e.tensor_tensor(
                        out=x[:, dc_out, tsl(tt)], in0=ps[4 + pb], in1=x[:, dc_out, tsl(tt)], op=ALU.add),
                        r=[f"ps{4 + pb}", f"x{tt}"], w=[f"x{tt}"])

    pend = [None]
    for s in range(n_ffn):
        wg, wu, wd = ffw[s]
        for tt in range(NTT):
            norm_to_h(tt, s)
        for g in range(NFG):
            b = g % 2
            fs = slice(g * FG, (g + 1) * FG)
            P.dma("sp", wst[b][0].rearrange("p (c f) -> p c f", c=8), wg[:, :, fs], f"wst{b}0", w=[f"wst{b}0"])
            P.dma("sp", wst[b][1].rearrange("p (c f) -> p c f", c=8), wu[:, :, fs], f"wst{b}1", w=[f"wst{b}1"])
            P.dma("sp", wst[b][2].rearrange("p (c f) -> p c f", c=2), wd[:, 2 * g:2 * g + 2, :], f"wst{b}2",
                  w=[f"wst{b}2"])
            for j in range(3):
                P.pool(lambda e, b=b, j=j: e.tensor_copy(out=wbf[b][j], in_=wst[b][j]),
                       r=[f"wst{b}{j}"], w=[f"wbf{b}{j}"])
            wgv = wbf[b][0].rearrange("p (c f) -> p c f", c=8)
            wuv = wbf[b][1].rearrange("p (c f) -> p c f", c=8)
            wdv = wbf[b][2].rearrange("p (c f) -> p c f", c=2)
            for tt in range(NTT):
                zb = tt % 2
                for fc in range(2):
                    pg = 2 * fc
                    pu = 2 * fc + 1
                    for dc in range(8):
                        P.pe(lambda e, dc=dc, fc=fc, tt=tt, pg=pg, wgv=wgv: e.matmul(
                            ps[pg], lhsT=wgv[:, dc, fc * 128:(fc + 1) * 128], rhs=h[:, dc, tsl(tt)],
                            start=(dc == 0), stop=(dc == 7)),
                            r=[f"wbf{b}0", f"h{tt}"], w=[f"ps{pg}"])
                    for dc in range(8):
                        P.pe(lambda e, dc=dc, fc=fc, tt=tt, pu=pu, wuv=wuv: e.matmul(
                            ps[pu], lhsT=wuv[:, dc, fc * 128:(fc + 1) * 128], rhs=h[:, dc, tsl(tt)],
                            start=(dc == 0), stop=(dc == 7)),
                            r=[f"wbf{b}1", f"h{tt}"], w=[f"ps{pu}"])
                    P.act(lambda e, fc=fc, pg=pg: e.activation(out=a_sb[fc], in_=ps[pg], func=AF.Silu),
                          r=[f"ps{pg}"], w=[f"a{fc}"])
                    P.dve(lambda e, fc=fc, pu=pu, zb=zb: e.tensor_tensor(
                        out=z[zb][:, fc, :], in0=a_sb[fc], in1=ps[pu], op=ALU.mult),
                        r=[f"a{fc}", f"ps{pu}"], w=[f"z{zb}"])
                if pend[0] is not None:
                    pend[0]()

                def down(tt=tt, zb=zb, wdv=wdv, b=b):
                    for dc in range(8):
                        pb = 4 + dc % 2
                        for fc in range(2):
                            P.pe(lambda e: e.matmul(
                                ps[pb], lhsT=wdv[:, fc, dc * 128:(dc + 1) * 128], rhs=z[zb][:, fc, :],
                                start=(fc == 0), stop=(fc == 1)),
                                r=[f"wbf{b}2", f"z{zb}"], w=[f"ps{pb}"])
                        P.dve(lambda e: e.scalar_tensor_tensor(
                            out=x[:, dc, tsl(tt)], in0=ps[pb], scalar=0.5, in1=x[:, dc, tsl(tt)],
                            op0=ALU.mult, op1=ALU.add),
                            r=[f"ps{pb}", f"x{tt}"], w=[f"x{tt}"])
                pend[0] = down
        pend[0]()
        pend[0] = None

    if do_mix:
        for tt in range(NTT):
            norm_to_h(tt, n_ffn)
            P.dma("sp", h_out[:, :, tsl(tt)], h[:, :, tsl(tt)], "hout", r=[f"h{tt}"])
    if do_final:
        for tt in range(NTT):
            norm_stats(tt)
            for dc in range(8):
                P.dve(lambda e, dc=dc, tt=tt: e.scalar_tensor_tensor(
                    out=x[:, dc, tsl(tt)], in0=x[:, dc, tsl(tt)],
                    scalar=gains[:, n_ffn * 8 + dc:n_ffn * 8 + dc + 1],
                    in1=rstd, op0=ALU.mult, op1=ALU.mult),
                    r=[f"x{tt}", "gains", "rstd"], w=[f"x{tt}"])
            P.dma("sp", y_out[:, :, tsl(tt)], x[:, :, tsl(tt)], "xout", r=[f"x{tt}"], out=True)
    else:
        for tt in range(NTT):
            P.dma("sp", x_out[:, :, tsl(tt)], x[:, :, tsl(tt)], "xout", r=[f"x{tt}"])

import math

D = 1024
S = 8192
TT = 512
NT = S // TT
QR, KVR, ROPE, NOPE, DV = 512, 256, 64, 128, 128
LATC = 896
EPS = 1e-6
SCALE = (NOPE + ROPE) ** -0.5


def emit_mla(CX, io):
    nc = CX.nc
    P = CX.P
    hsrc = io["hsrc"]
    pos_in = io["pos"]
    wa_in = io["wa"]
    wqb_in = io["wqb"]
    wkvb_in = io["wkvb"]
    c_in = io["consts"]
    masks_in = io["masks"]
    o_out = io["o_out"]

    sb = CX.sb
    stage = sb("stage", [128, 4 * LATC], F32).ap()
    wa = sb("wa_sb", [128, 8, LATC], BF16).ap()
    wqb = sb("wqb_sb", [128, 4, 512], BF16).ap()
    wkvb = sb("wkvb_sb", [128, 2, 512], BF16).ap()
    consts = sb("consts_sb", [128, 8], F32).ap()
    masks = sb("masks_sb", [128, 4, 512], BF16).ap()
    ones = sb("ones", [128, 128], BF16).ap()
    hT = [sb(f"hT{i}", [128, 8, TT], BF16).ap() for i in range(2)]
    lat = sb("lat", [128, 6, TT], F32).ap()
    sq = sb("sq", [128, 6, TT], BF16).ap()
    sd = sb("sd", [128, TT], F32).ap()
    rq = sb("rq", [128, TT], F32).ap()
    rkv = sb("rkv", [128, TT], F32).ap()
    cqn = sb("cqn", [128, 4, TT], BF16).ap()
    ckvn = sb("ckvn", [128, 2, TT], BF16).ap()
    posi = sb("posi", [64, TT], I32).ap()
    ui = sb("ui", [64, TT], I32).ap()
    u = sb("u", [64, TT], F32).ap()
    uc = sb("uc", [64, TT], F32).ap()
    wr = sb("wr", [64, TT], F32).ap()
    cos2 = sb("cos2", [64, TT], F32).ap()
    sin2 = sb("sin2", [64, TT], F32).ap()
    t1 = sb("t1", [64, TT], F32).ap()
    t2 = sb("t2", [64, TT], F32).ap()
    Kn = [sb(f"Kn{i}", [128, S], BF16).ap() for i in range(2)]
    Kr = sb("Kr", [64, S], BF16).ap()
    V = [sb(f"V{i}", [128, S // 128, DV], BF16).ap() for i in range(2)]
    qn = [sb(f"qn{i}", [128, TT], BF16).ap() for i in range(2)]
    qr = [sb(f"qr{i}", [64, TT], BF16).ap() for i in range(2)]
    pT = [sb(f"pT{i}", [128, TT], BF16).ap() for i in range(4)]
    rden = sb("rden", [128, TT], F32).ap()
    osb = [sb(f"osb{i}", [128, TT], BF16).ap() for i in range(2)]
    ps = CX.ps

    P.pool(lambda e: e.memset(ones, 1.0), w=["ones"])
    P.dma("sp", consts, c_in, "consts", w=["consts"])
    P.dma("sp", masks, masks_in, "masks", w=["masks"])
    for hf in range(2):
        P.dma("sp", stage.rearrange("p (c f) -> p c f", c=4), wa_in[:, 4 * hf:4 * hf + 4, :], "stage", w=["stage"])
        P.pool(lambda e, hf=hf: e.tensor_copy(out=wa[:, 4 * hf:4 * hf + 4, :].rearrange("p c f -> p (c f)"), in_=stage),
               r=["stage"], w=["wa"])
    P.dma("sp", stage[:, 0:2048].rearrange("p (c f) -> p c f", c=4), wqb_in, "stage", w=["stage"])
    P.pool(lambda e: e.tensor_copy(out=wqb.rearrange("p c f -> p (c f)"), in_=stage[:, 0:2048]), r=["stage"], w=["wqb"])
    P.dma("sp", stage[:, 0:1024].rearrange("p (c f) -> p c f", c=2), wkvb_in, "stage", w=["stage"])
    P.pool(lambda e: e.tensor_copy(out=wkvb.rearrange("p c f -> p (c f)"), in_=stage[:, 0:1024]), r=["stage"], w=["wkvb"])

    pcount = [0]

    def pbank():
        pcount[0] += 1
        return pcount[0] % 2

    def tsl(t):
        return slice(t * TT, (t + 1) * TT)

    def rope(src_ps, src_sw_ps, dst, rd, wt):
        P.dve(lambda e: e.tensor_tensor(out=t1, in0=src_ps, in1=cos2, op=ALU.mult), r=rd + ["cos2"], w=["t1"])
        P.dve(lambda e: e.tensor_tensor(out=t2, in0=src_sw_ps, in1=sin2, op=ALU.mult), r=rd + ["sin2"], w=["t2"])
        P.dve(lambda e: e.tensor_tensor(out=dst, in0=t1, in1=t2, op=ALU.add), r=["t1", "t2"], w=wt)

    for t in range(NT):
        hb = t % 2
        hsrc(hT[hb], t, f"hT{hb}", [f"hT{hb}"])
        P.dma("sp", posi, pos_in[:, tsl(t)].partition_broadcast(64), "posi", w=["posi"])
        P.dve(lambda e: e.tensor_copy(out=t1, in_=posi), r=["posi"], w=["t1"])
        P.dve(lambda e: e.tensor_scalar(out=u, in0=t1, scalar1=consts[0:64, 6:7], scalar2=None, op0=ALU.mult),
              r=["t1", "consts"], w=["u"])
        P.dve(lambda e: e.tensor_copy(out=ui, in_=u), r=["u"], w=["ui"])
        P.dve(lambda e: e.tensor_copy(out=t2, in_=ui), r=["ui"], w=["t2"])
        P.dve(lambda e: e.tensor_tensor(out=u, in0=u, in1=t2, op=ALU.subtract), r=["u", "t2"], w=["u"])
        P.dve(lambda e: e.tensor_single_scalar(out=wr, in_=u, scalar=0.5, op=ALU.is_gt), r=["u"], w=["wr"])
        P.dve(lambda e: e.tensor_tensor(out=u, in0=u, in1=wr, op=ALU.subtract), r=["u", "wr"], w=["u"])
        P.dve(lambda e: e.tensor_single_scalar(out=wr, in_=u, scalar=-0.5, op=ALU.is_lt), r=["u"], w=["wr"])
        P.dve(lambda e: e.tensor_tensor(out=u, in0=u, in1=wr, op=ALU.add), r=["u", "wr"], w=["u"])
        P.dve(lambda e: e.tensor_scalar_add(out=uc, in0=u, scalar1=0.25), r=["u"], w=["uc"])
        P.dve(lambda e: e.tensor_single_scalar(out=wr, in_=uc, scalar=0.5, op=ALU.is_gt), r=["uc"], w=["wr"])
        P.dve(lambda e: e.tensor_tensor(out=uc, in0=uc, in1=wr, op=ALU.subtract), r=["uc", "wr"], w=["uc"])
        P.act(lambda e: e.activation(out=sin2, in_=u, func=AF.Sin, scale=2 * math.pi), r=["u"], w=["sin2"])
        P.act(lambda e: e.activation(out=cos2, in_=uc, func=AF.Sin, scale=2 * math.pi), r=["uc"], w=["cos2"])
        P.dve(lambda e: e.tensor_scalar(out=sin2, in0=sin2, scalar1=consts[0:64, 7:8], scalar2=None, op0=ALU.mult),
              r=["sin2", "consts"], w=["sin2"])
        for c in range(6):
            pb = pbank()
            for dc in range(8):
                P.pe(lambda e, c=c, dc=dc, pb=pb: e.matmul(ps[pb], lhsT=wa[:, dc, c * 128:(c + 1) * 128],
                                                            rhs=hT[hb][:, dc, :], start=(dc == 0), stop=(dc == 7)),
                     r=["wa", f"hT{hb}"], w=[f"ps{pb}"])
            P.act(lambda e, c=c, pb=pb: e.copy(out=lat[:, c, :], in_=ps[pb]), r=[f"ps{pb}"], w=[f"lat{c}"])
        pbs = []
        for c2 in range(2):
            pb = pbank()
            pbs.append(pb)
            for dc in range(8):
                P.pe(lambda e, c2=c2, dc=dc, pb=pb: e.matmul(ps[pb][0:64, :], lhsT=wa[:, dc, 768 + 64 * c2:832 + 64 * c2],
                                                              rhs=hT[hb][:, dc, :], start=(dc == 0), stop=(dc == 7)),
                     r=["wa", f"hT{hb}"], w=[f"ps{pb}"])
        rope(ps[pbs[0]][0:64, :], ps[pbs[1]][0:64, :], Kr[:, tsl(t)], [f"ps{pbs[0]}", f"ps{pbs[1]}"], [f"Kr{t}"])
        P.act(lambda e: e.activation(out=sq, in_=lat[:, 0:6, :], func=AF.Square),
              r=[f"lat{c}" for c in range(6)], w=["sq"])
        pb = pbank()
        for c in range(4):
            P.pe(lambda e, c=c, pb=pb: e.matmul(ps[pb], lhsT=ones, rhs=sq[:, c, :], start=(c == 0), stop=(c == 3)),
                 r=["ones", "sq"], w=[f"ps{pb}"])
        P.act(lambda e, pb=pb: e.activation(out=sd, in_=ps[pb], func=AF.Sqrt, bias=EPS, scale=1.0 / QR),
              r=[f"ps{pb}"], w=["sd"])
        P.dve(lambda e: e.reciprocal(out=rq, in_=sd), r=["sd"], w=["rq"])
        pb = pbank()
        for c in range(2):
            P.pe(lambda e, c=c, pb=pb: e.matmul(ps[pb], lhsT=ones, rhs=sq[:, 4 + c, :], start=(c == 0), stop=(c == 1)),
                 r=["ones", "sq"], w=[f"ps{pb}"])
        P.act(lambda e, pb=pb: e.activation(out=sd, in_=ps[pb], func=AF.Sqrt, bias=EPS, scale=1.0 / KVR),
              r=[f"ps{pb}"], w=["sd"])
        P.dve(lambda e: e.reciprocal(out=rkv, in_=sd), r=["sd"], w=["rkv"])
        for c in range(4):
            P.dve(lambda e, c=c: e.scalar_tensor_tensor(out=cqn[:, c, :], in0=lat[:, c, :], scalar=consts[:, c:c + 1],
                                                        in1=rq, op0=ALU.mult, op1=ALU.mult),
                  r=[f"lat{c}", "rq", "consts"], w=["cqn"])
        for c in range(2):
            P.dve(lambda e, c=c: e.scalar_tensor_tensor(out=ckvn[:, c, :], in0=lat[:, 4 + c, :],
                                                        scalar=consts[:, 4 + c:5 + c],
                                                        in1=rkv, op0=ALU.mult, op1=ALU.mult),
                  r=[f"lat{4 + c}", "rkv", "consts"], w=["ckvn"])
        for hh in range(2):
            qo = hh * 256
            pb = pbank()
            for c in range(4):
                P.pe(lambda e, c=c, pb=pb, qo=qo: e.matmul(ps[pb], lhsT=wqb[:, c, qo:qo + 128], rhs=cqn[:, c, :],
                                                            start=(c == 0), stop=(c == 3)),
                     r=["wqb", "cqn"], w=[f"ps{pb}"])
            P.act(lambda e, pb=pb, hh=hh: e.copy(out=qn[hh], in_=ps[pb]), r=[f"ps{pb}"], w=[f"qn{hh}"])
            pbs = []
            for c2 in range(2):
                pb = pbank()
                pbs.append(pb)
                for c in range(4):
                    P.pe(lambda e, c=c, c2=c2, pb=pb, qo=qo: e.matmul(
                        ps[pb][0:64, :], lhsT=wqb[:, c, qo + 128 + 64 * c2:qo + 192 + 64 * c2], rhs=cqn[:, c, :],
                        start=(c == 0), stop=(c == 3)),
                        r=["wqb", "cqn"], w=[f"ps{pb}"])
            rope(ps[pbs[0]][0:64, :], ps[pbs[1]][0:64, :], qr[hh], [f"ps{pbs[0]}", f"ps{pbs[1]}"], [f"qr{hh}"])
            ko = hh * 256
            pb = pbank()
            for c in range(2):
                P.pe(lambda e, c=c, pb=pb, ko=ko: e.matmul(ps[pb], lhsT=wkvb[:, c, ko:ko + 128], rhs=ckvn[:, c, :],
                                                            start=(c == 0), stop=(c == 1)),
                     r=["wkvb", "ckvn"], w=[f"ps{pb}"])
            P.act(lambda e, pb=pb, hh=hh, t=t: e.copy(out=Kn[hh][:, tsl(t)], in_=ps[pb]), r=[f"ps{pb}"],
                  w=[f"Kn{hh}_{t}"])
            pb = pbank()
            for j in range(4):
                for c in range(2):
                    P.pe(lambda e, c=c, j=j, pb=pb, ko=ko: e.matmul(
                        ps[pb][:, j * 128:(j + 1) * 128], lhsT=ckvn[:, c, j * 128:(j + 1) * 128],
                        rhs=wkvb[:, c, ko + 128:ko + 256], start=(c == 0), stop=(c == 1)),
                        r=["wkvb", "ckvn"], w=[f"ps{pb}"])
            P.act(lambda e, pb=pb, hh=hh, t=t: e.copy(
                out=V[hh][:, 4 * t:4 * t + 4, :], in_=ps[pb].rearrange("p (j d) -> p j d", j=4)),
                r=[f"ps{pb}"], w=[f"V{hh}_{t}"])
        SB = [2, 3, 6, 7]
        LA = 2
        for hh in range(2):
            ob = 4
            nkb = 4 * t + 4

            def qk(kb):
                sbk = SB[kb % 4]
                j = kb - 4 * t
                q0 = max(j, 0) * 128
                tk = kb // 4
                ksl = slice(kb * 128, (kb + 1) * 128)
                P.pe(lambda e: e.matmul(ps[sbk][:, q0:], lhsT=Kn[hh][:, ksl], rhs=qn[hh][:, q0:], start=True, stop=False),
                     r=[f"Kn{hh}_{tk}", f"qn{hh}"], w=[f"ps{sbk}"])
                P.pe(lambda e: e.matmul(ps[sbk][:, q0:], lhsT=Kr[:, ksl], rhs=qr[hh][:, q0:], start=False, stop=True),
                     r=[f"Kr{tk}", f"qr{hh}"], w=[f"ps{sbk}"])
                pi = kb % 4
                P.act(lambda e: e.activation(out=pT[pi][:, q0:], in_=ps[sbk][:, q0:], func=AF.Exp, scale=SCALE),
                      r=[f"ps{sbk}"], w=[f"pT{pi}"])
                if j >= 0:
                    P.pool(lambda e: e.tensor_tensor(out=pT[pi][:, q0:], in0=pT[pi][:, q0:], in1=masks[:, j, q0:], op=ALU.mult),
                           r=[f"pT{pi}", "masks"], w=[f"pT{pi}"])

            def pv(kb):
                j = kb - 4 * t
                q0 = max(j, 0) * 128
                tk = kb // 4
                pi = kb % 4
                P.pe(lambda e: e.matmul(ps[ob][:, q0:], lhsT=V[hh][:, kb, :], rhs=pT[pi][:, q0:], start=(kb == 0), stop=(kb == nkb - 1)),
                     r=[f"V{hh}_{tk}", f"pT{pi}"], w=[f"ps{ob}"])
                P.pe(lambda e: e.matmul(ps[ob + 1][:, q0:], lhsT=ones, rhs=pT[pi][:, q0:], start=(kb == 0), stop=(kb == nkb - 1)),
                     r=["ones", f"pT{pi}"], w=[f"ps{ob + 1}"])

            for i in range(nkb + LA):
                if i < nkb:
                    qk(i)
                if i - LA >= 0:
                    pv(i - LA)
            P.dve(lambda e: e.reciprocal(out=rden, in_=ps[ob + 1]), r=[f"ps{ob + 1}"], w=["rden"])
            P.dve(lambda e: e.tensor_tensor(out=osb[hh], in0=ps[ob], in1=rden, op=ALU.mult),
                  r=[f"ps{ob}", "rden"], w=[f"osb{hh}"])
            P.dma("sp", o_out[hh * DV:(hh + 1) * DV, tsl(t)], osb[hh], "oout", r=[f"osb{hh}"])


D = 1024
S = 8192
TT = 512
NT = S // TT
LM = 128
NCM = S // LM
EPS = 1e-6
WC = 772


def emit_mls(CX, io):
    nc = CX.nc
    P = CX.P
    hsrc = io["hsrc"]
    w_in = io["w"]
    c_in = io["consts"]
    cm_in = io["cm"]
    sel_in = io["sel"]
    o_out = io["o_out"]

    sb = CX.sb
    stage = sb("stage", [128, 4 * WC], F32).ap()
    w = sb("w_sb", [128, 8, WC], BF16).ap()
    consts = sb("consts_sb", [128, 16], F32).ap()
    cm = sb("cm_sb", [128, 256], BF16).ap()
    ident = cm[:, 0:128]
    tril = cm[:, 128:256]
    sel = sb("sel_sb", [2, 386], F32).ap()
    ones = sb("ones", [128, 128], BF16).ap()
    hT = [sb(f"hT{i}", [128, 8, TT], BF16).ap() for i in range(2)]
    gi = sb("gi", [2, S], F32).ap()
    gf = sb("gf", [2, S], F32).ap()
    mu = sb("mu", [2, S], F32).ap()
    Gcol = sb("Gcol", [128, NCM, 2], F32).ap()
    nmu = [sb(f"nmu{i}", [128, NCM + 1], F32).ap() for i in range(2)]
    pmuq = sb("pmuq", [128, NCM + 1], F32).ap()
    mub = [sb(f"mub{i}", [128, TT], F32).ap() for i in range(2)]
    muq = sb("muq", [128, TT], F32).ap()
    emt = [sb(f"emt{i}", [128, TT], F32).ap() for i in range(2)]
    xq = sb("xq", [128, 3 + TT], F32).ap()
    xk = sb("xk", [128, 3 + TT], F32).ap()
    cacc = sb("cacc", [128, TT], F32).ap()
    csil = sb("csil", [128, TT], F32).ap()
    qT = sb("qT", [128, TT], BF16).ap()
    qsT = sb("qsT", [128, TT], BF16).ap()
    kT = sb("kT", [128, TT], BF16).ap()
    ktok = sb("ktok", [128, 4, 128], F32).ap()
    kw = sb("kw", [128, 128], BF16).ap()
    vtok = [sb(f"vtok{i}", [128, 4, 128], BF16).ap() for i in range(2)]
    sigo = [sb(f"sigo{i}", [128, TT], F32).ap() for i in range(2)]
    Wt = sb("Wt", [128, 128], F32).ap()
    Wm = sb("Wm", [128, 128], F32).ap()
    pT = sb("pT", [128, 128], BF16).ap()
    rb = sb("rb", [128, TT], F32).ap()
    wcol = sb("wcol", [128, 2], F32).ap()
    carry = sb("carry", [128, 1], F32).ap()
    C = sb("C", [128, 128], F32).ap()
    Cb = sb("Cb", [128, 128], BF16).ap()
    Nb = sb("Nb", [128, 128], F32).ap()
    Nbb = sb("Nbb", [128, 128], BF16).ap()
    dn = sb("dn", [128, 128], F32).ap()
    hid = [sb(f"hid{i}", [128, TT], F32).ap() for i in range(2)]
    hsq = sb("hsq", [128, TT], BF16).ap()
    sd = sb("sd", [128, TT], F32).ap()
    rstd = sb("rstd", [128, TT], F32).ap()
    ho = [sb(f"ho{i}", [128, TT], BF16).ap() for i in range(2)]
    ps = CX.ps

    def tsl(t):
        return slice(t * TT, (t + 1) * TT)

    P.pool(lambda e: e.memset(ones, 1.0), w=["ones"])
    P.pool(lambda e: e.memset(C, 0.0), w=["C"])
    P.pool(lambda e: e.memset(Cb, 0.0), w=["Cb"])
    P.pool(lambda e: e.memset(Nb, 0.0), w=["Nb"])
    P.pool(lambda e: e.memset(Nbb, 0.0), w=["Nbb"])
    P.pool(lambda e: e.memset(xq[:, 0:3], 0.0), w=["xq"])
    P.pool(lambda e: e.memset(xk[:, 0:3], 0.0), w=["xk"])
    for i in range(2):
        P.pool(lambda e, i=i: e.memset(nmu[i][:, 0:1], 0.0), w=[f"nmu{i}"])
    P.pool(lambda e: e.memset(pmuq[:, 0:1], 0.0), w=["pmuq"])
    P.dma("sp", consts, c_in, "consts", w=["consts"])
    P.dma("sp", cm, cm_in, "cm", w=["cm"])
    P.dma("sp", sel, sel_in, "sel", w=["sel"])
    for hf in range(2):
        P.dma("sp", stage.rearrange("p (c f) -> p c f", c=4), w_in[:, 4 * hf:4 * hf + 4, :], "stage", w=["stage"])
        P.pool(lambda e: e.tensor_copy(out=w[:, 4 * hf:4 * hf + 4, :].rearrange("p c f -> p (c f)"), in_=stage),
               r=["stage"], w=["w"])

    for t in range(NT):
        hb = t % 2
        hsrc(hT[hb], t, f"hT{hb}", [f"hT{hb}"])
        for g2 in range(2):
            pb = g2
            for dc in range(8):
                P.pe(lambda e: e.matmul(ps[pb][0:2, :], lhsT=w[:, dc, 768 + 2 * g2:770 + 2 * g2], rhs=hT[hb][:, dc, :],
                                        start=(dc == 0), stop=(dc == 7)),
                     r=["w", f"hT{hb}"], w=[f"ps{pb}"])
        P.act(lambda e: e.activation(out=gi[:, tsl(t)], in_=ps[0][0:2, :], func=AF.Identity, bias=consts[0:2, 12:13]),
              r=["ps0", "consts"], w=["gi"])
        P.act(lambda e: e.activation(out=gf[:, tsl(t)], in_=ps[1][0:2, :], func=AF.Exp, bias=consts[0:2, 13:14], scale=-1.0),
              r=["ps1", "consts"], w=["gf"])
    P.act(lambda e: e.activation(out=gf, in_=gf, func=AF.Ln, bias=1.0, scale=1.0), r=["gf"], w=["gf"])
    P.dve(lambda e: e.tensor_scalar(out=gf, in0=gf, scalar1=-0.5, scalar2=None, op0=ALU.mult), r=["gf"], w=["gf"])
    P.dve(lambda e: e.tensor_tensor_scan(out=gf, data0=gf, data1=gf, initial=0.0, op0=ALU.add, op1=ALU.add),
          r=["gf"], w=["gf"])
    P.dve(lambda e: e.tensor_tensor(out=gi, in0=gi, in1=gf, op=ALU.subtract), r=["gi", "gf"], w=["gi"])
    P.dve(lambda e: e.tensor_tensor_scan(out=mu, data0=gi, data1=gi, initial=0.0, op0=ALU.max, op1=ALU.max),
          r=["gi"], w=["mu"])
    P.dve(lambda e: e.tensor_tensor(out=gf, in0=gf, in1=mu, op=ALU.add), r=["gf", "mu"], w=["gf"])
    fm = gf
    id2 = sel[:, 384:386]
    for c in range(NCM):
        P.pe(lambda e: e.matmul(ps[2][:, 2 * c:2 * c + 2], lhsT=gi[:, c * LM:(c + 1) * LM], rhs=id2, start=True, stop=True),
             r=["gi", "sel"], w=["ps2"])
    P.act(lambda e: e.copy(out=Gcol.rearrange("p c h -> p (c h)"), in_=ps[2][:, 0:2 * NCM]), r=["ps2"], w=["Gcol"])

    pcnt = [0]

    def pbank():
        pcnt[0] += 1
        return pcnt[0] % 2

    for t in range(NT):
        hb = t % 2
        hsrc(hT[hb], t, f"hT{hb}", [f"hT{hb}"])
        for hh in range(2):
            P.pe(lambda e: e.matmul(ps[2], lhsT=sel[:, hh * 128:(hh + 1) * 128], rhs=mu[:, tsl(t)], start=True, stop=True),
                 r=["sel", "mu"], w=["ps2"])
            P.act(lambda e: e.copy(out=mub[hh], in_=ps[2]), r=["ps2"], w=[f"mub{hh}"])
            P.act(lambda e: e.activation(out=nmu[hh][:, 4 * t + 1:4 * t + 5], in_=ps[2][:, LM - 1::LM], func=AF.Copy, scale=-1.0),
                  r=["ps2"], w=[f"nmu{hh}"])
            P.pe(lambda e: e.matmul(ps[2], lhsT=sel[:, hh * 128:(hh + 1) * 128], rhs=fm[:, tsl(t)], start=True, stop=True),
                 r=["sel", "gf"], w=["ps2"])
            P.act(lambda e: e.activation(out=emt[hh], in_=ps[2], func=AF.Exp, scale=-1.0), r=["ps2"], w=[f"emt{hh}"])
        P.pe(lambda e: e.matmul(ps[2], lhsT=sel[:, 256:384], rhs=mu[:, tsl(t)], start=True, stop=True),
             r=["sel", "mu"], w=["ps2"])
        P.act(lambda e: e.copy(out=muq, in_=ps[2]), r=["ps2"], w=["muq"])
        P.act(lambda e: e.copy(out=pmuq[:, 4 * t + 1:4 * t + 5], in_=ps[2][:, LM - 1::LM]), r=["ps2"], w=["pmuq"])
        for which, xbuf, dst in ((0, xq, qT), (1, xk, kT)):
            pb = pbank()
            xn = "xq" if which == 0 else "xk"
            for dc in range(8):
                P.pe(lambda e: e.matmul(ps[pb], lhsT=w[:, dc, which * 128:(which + 1) * 128], rhs=hT[hb][:, dc, :],
                                        start=(dc == 0), stop=(dc == 7)),
                     r=["w", f"hT{hb}"], w=[f"ps{pb}"])
            P.act(lambda e: e.copy(out=xbuf[:, 3:3 + TT], in_=ps[pb]), r=[f"ps{pb}"], w=[xn])
            cw = 4 * which
            P.dve(lambda e: e.tensor_scalar(out=cacc, in0=xbuf[:, 0:TT], scalar1=consts[:, cw:cw + 1],
                                            scalar2=consts[:, 8 + which:9 + which], op0=ALU.mult, op1=ALU.add),
                  r=[xn, "consts"], w=["cacc"])
            for j in range(1, 4):
                P.dve(lambda e: e.scalar_tensor_tensor(out=cacc, in0=xbuf[:, j:j + TT], scalar=consts[:, cw + j:cw + j + 1],
                                                       in1=cacc, op0=ALU.mult, op1=ALU.add),
                      r=[xn, "consts", "cacc"], w=["cacc"])
            P.pool(lambda e: e.tensor_copy(out=xbuf[:, 0:3], in_=xbuf[:, TT:TT + 3]), r=[xn, "cacc"], w=[xn])
            P.act(lambda e: e.activation(out=csil, in_=cacc, func=AF.Silu), r=["cacc"], w=["csil"])
            if which == 0:
                P.dve(lambda e: e.tensor_copy(out=dst, in_=csil), r=["csil"], w=["qT"])
            else:
                P.dve(lambda e: e.tensor_scalar(out=dst, in0=csil, scalar1=0.125, scalar2=None, op0=ALU.mult),
                      r=["csil"], w=["kT"])
        for hh in range(2):
            pb = pbank()
            for dc in range(8):
                P.pe(lambda e: e.matmul(ps[pb], lhsT=w[:, dc, 512 + hh * 128:640 + hh * 128], rhs=hT[hb][:, dc, :],
                                        start=(dc == 0), stop=(dc == 7)),
                     r=["w", f"hT{hb}"], w=[f"ps{pb}"])
            P.act(lambda e: e.activation(out=sigo[hh], in_=ps[pb], func=AF.Sigmoid), r=[f"ps{pb}"], w=[f"sigo{hh}"])
        for hh in range(2):
            pb = pbank()
            for j in range(4):
                for dc in range(8):
                    P.pe(lambda e: e.matmul(ps[pb][:, j * 128:(j + 1) * 128], lhsT=hT[hb][:, dc, j * 128:(j + 1) * 128],
                                            rhs=w[:, dc, 256 + hh * 128:384 + hh * 128], start=(dc == 0), stop=(dc == 7)),
                         r=["w", f"hT{hb}"], w=[f"ps{pb}"])
            P.act(lambda e: e.copy(out=vtok[hh].rearrange("p j d -> p (j d)"), in_=ps[pb]), r=[f"ps{pb}"], w=[f"vtok{hh}"])
        pb = pbank()
        for j in range(4):
            P.pe(lambda e: e.matmul(ps[pb][:, j * 128:(j + 1) * 128], lhsT=kT[:, j * 128:(j + 1) * 128], rhs=ident,
                                    start=True, stop=True),
                 r=["kT", "cm"], w=[f"ps{pb}"])
        P.act(lambda e: e.copy(out=ktok.rearrange("p j d -> p (j d)"), in_=ps[pb]), r=[f"ps{pb}"], w=["ktok"])
        for j in range(4):
            c = 4 * t + j
            P.act(lambda e: e.activation(out=rb[:, j * LM:(j + 1) * LM], in_=muq[:, j * LM:(j + 1) * LM], func=AF.Exp,
                                         bias=pmuq[:, c:c + 1], scale=-1.0),
                  r=["muq", "pmuq"], w=["rb"])
        P.dve(lambda e: e.tensor_tensor(out=qsT, in0=qT, in1=rb, op=ALU.mult), r=["qT", "rb"], w=["qsT"])
        for j in range(4):
            c = 4 * t + j
            cs = slice(j * LM, (j + 1) * LM)
            for hh in range(2):
                hp = slice(hh * 64, (hh + 1) * 64)
                P.pe(lambda e: e.matmul(ps[3][:, 0:128], lhsT=kT[hp, cs], rhs=qT[hp, cs], start=True, stop=True),
                     r=["kT", "qT"], w=["ps3"])
                P.act(lambda e: e.activation(out=Wt, in_=mub[hh][:, cs], func=AF.Exp, bias=Gcol[:, c, hh:hh + 1], scale=-1.0),
                      r=[f"mub{hh}", "Gcol"], w=["Wt"])
                P.pool(lambda e: e.tensor_tensor(out=Wm, in0=Wt, in1=tril, op=ALU.mult), r=["Wt", "cm"], w=["Wm"])
                P.dve(lambda e: e.tensor_tensor(out=pT, in0=ps[3][:, 0:128], in1=Wm, op=ALU.mult), r=["ps3", "Wm"], w=["pT"])
                P.pe(lambda e: e.matmul(ps[4][:, 0:128], lhsT=vtok[hh][:, j, :], rhs=pT, start=True, stop=False),
                     r=[f"vtok{hh}", "pT"], w=["ps4"])
                P.pe(lambda e: e.matmul(ps[4][:, 0:128], lhsT=Cb[hp, :], rhs=qsT[hp, cs], start=False, stop=True),
                     r=["Cb", "qsT"], w=["ps4"])
                P.pe(lambda e: e.matmul(ps[5][:, 0:128], lhsT=ones, rhs=pT, start=True, stop=False),
                     r=["ones", "pT"], w=["ps5"])
                P.pe(lambda e: e.matmul(ps[5][:, 0:128], lhsT=Nbb[hp, :], rhs=qsT[hp, cs], start=False, stop=True),
                     r=["Nbb", "qsT"], w=["ps5"])
                P.act(lambda e: e.activation(out=dn, in_=ps[5][:, 0:128], func=AF.Abs), r=["ps5"], w=["dn"])
                P.dve(lambda e: e.tensor_tensor(out=dn, in0=dn, in1=emt[hh][:, cs], op=ALU.max),
                      r=["dn", f"emt{hh}"], w=["dn"])
                P.dve(lambda e: e.reciprocal(out=dn, in_=dn), r=["dn"], w=["dn"])
                P.dve(lambda e: e.tensor_tensor(out=hid[hh][:, cs], in0=ps[4][:, 0:128], in1=dn, op=ALU.mult),
                      r=["ps4", "dn"], w=[f"hid{hh}"])
                P.act(lambda e: e.activation(out=wcol[:, hh:hh + 1], in_=Gcol[:, c, hh:hh + 1], func=AF.Exp,
                                             bias=nmu[hh][:, c + 1:c + 2], scale=1.0),
                      r=["Gcol", f"nmu{hh}"], w=["wcol"])
                P.pool(lambda e: e.tensor_scalar(out=kw[:, hp], in0=ktok[:, j, hp], scalar1=wcol[:, hh:hh + 1], scalar2=None,
                                                 op0=ALU.mult),
                       r=["ktok", "wcol"], w=["kw"])
            P.act(lambda e: e.activation(out=carry, in_=pmuq[:, c + 1:c + 2], func=AF.Exp, bias=pmuq[:, c:c + 1], scale=-1.0),
                  r=["pmuq"], w=["carry"])
            for hh in range(2):
                hp = slice(hh * 64, (hh + 1) * 64)
                P.pe(lambda e: e.matmul(ps[6][hp, 0:128], lhsT=kw[:, hp], rhs=vtok[hh][:, j, :], start=True, stop=True),
                     r=["kw", f"vtok{hh}"], w=["ps6"])
            P.pe(lambda e: e.matmul(ps[7][:, 0:128], lhsT=kw, rhs=ones, start=True, stop=True), r=["kw", "ones"], w=["ps7"])
            P.dve(lambda e: e.scalar_tensor_tensor(out=C, in0=C, scalar=carry, in1=ps[6][:, 0:128], op0=ALU.mult, op1=ALU.add),
                  r=["C", "carry", "ps6"], w=["C"])
            P.pool(lambda e: e.tensor_copy(out=Cb, in_=C), r=["C"], w=["Cb"])
            P.dve(lambda e: e.scalar_tensor_tensor(out=Nb, in0=Nb, scalar=carry, in1=ps[7][:, 0:128], op0=ALU.mult, op1=ALU.add),
                  r=["Nb", "carry", "ps7"], w=["Nb"])
            P.pool(lambda e: e.tensor_copy(out=Nbb, in_=Nb), r=["Nb"], w=["Nbb"])
        for hh in range(2):
            P.act(lambda e: e.activation(out=hsq, in_=hid[hh], func=AF.Square), r=[f"hid{hh}"], w=["hsq"])
            pb = pbank()
            P.pe(lambda e: e.matmul(ps[pb], lhsT=ones, rhs=hsq, start=True, stop=True), r=["ones", "hsq"], w=[f"ps{pb}"])
            P.act(lambda e: e.activation(out=sd, in_=ps[pb], func=AF.Sqrt, bias=EPS, scale=1.0 / 128), r=[f"ps{pb}"], w=["sd"])
            P.dve(lambda e: e.reciprocal(out=rstd, in_=sd), r=["sd"], w=["rstd"])
            P.dve(lambda e: e.scalar_tensor_tensor(out=rstd, in0=rstd, scalar=consts[:, 10 + hh:11 + hh], in1=sigo[hh],
                                                   op0=ALU.mult, op1=ALU.mult),
                  r=["rstd", "consts", f"sigo{hh}"], w=["rstd"])
            P.dve(lambda e: e.tensor_tensor(out=ho[hh], in0=hid[hh], in1=rstd, op=ALU.mult), r=[f"hid{hh}", "rstd"], w=[f"ho{hh}"])
            P.dma("sp", o_out[hh * 128:(hh + 1) * 128, tsl(t)], ho[hh], "oout", r=[f"ho{hh}"])

import math

D = 1024
S = 8192
TT = 512
NT = S // TT
LR = 64
NJ = TT // LR
NCR = S // LR
GN_EPS = 64e-5
DEC = math.exp(-0.5)


def emit_rwk(CX, io, nt=NT):
    nc = CX.nc
    P = CX.P
    hsrc = io["hsrc"]
    wbig_in = io["wbig"]
    w2c_in = io["w2c"]
    g2c_in = io["g2c"]
    mu_in = io["mu"]
    cvec_in = io["cvec"]
    lnb_in = io["lnwb"]
    cf_in = io["cf32"]
    o_out = io["o_out"]

    sb = CX.sb
    stage = sb("stage", [128, 4096], F32).ap()
    wbig = sb("wbig_sb", [128, 8, 1024], BF16).ap()
    w2c = sb("w2c_sb", [64, 512], BF16).ap()
    g2c = sb("g2c_sb", [128, 256], BF16).ap()
    mu = sb("mu_sb", [128, 48], F32).ap()
    cvec = sb("cvec_sb", [128, 16], F32).ap()
    lnwb = sb("lnwb_sb", [128, 256], F32).ap()
    cf = sb("cf_sb", [128, 1025], F32).ap()
    mask320 = cf[:, 0:320]
    identS = cf[:, 320:384]
    onescol = cf[:, 384:385]
    blockones = cf[:, 385:513]
    keep = cf[:, 513:1025]
    hT = [sb(f"hT{i}", [128, 8, 1 + TT], BF16).ap() for i in range(2)]
    xx = sb("xx", [128, 8, TT], F32).ap()
    xm = [sb(f"xm{i}", [128, 8, TT], BF16).ap() for i in range(2)]
    lora = sb("lora", [128, TT], BF16).ap()
    sg = sb("sg", [128, TT], BF16).ap()
    names = ["r", "k", "v", "lg", "ag", "kk", "km", "lp", "tmp", "tmp2", "bt", "kt", "rkr", "ssq"]
    A_ = [{n: sb(f"{n}{p}", [128, TT], F32).ap() for n in names} for p in range(2)]
    ar = [sb(f"ar{p}", [128, NJ, 128], F32).ap() for p in range(2)]
    S0T = [sb(f"S0T{p}", [128, 64], F32).ap() for p in range(2)]
    AM = sb("AM", [128, 320], F32).ap()
    bkP = sb("bkP", [128, 128], F32).ap()
    tok = sb("tok", [128, 192], F32).ap()
    Y = sb("Y", [128, 64], F32).ap()
    An = [sb(f"An{i}", [128, 128], F32).ap() for i in range(2)]
    st6 = sb("st6", [128, 6], F32).ap()
    mv = sb("mv", [128, 2], F32).ap()
    rs = sb("rs", [128, 1], F32).ap()
    bsc = sb("bsc", [128, 1], F32).ap()
    yn = sb("yn", [128, 64], F32).ap()
    yo = sb("yo", [128, 64], F32).ap()
    ofm = [sb(f"ofm{p}", [128, TT], BF16).ap() for p in range(2)]
    ps = CX.ps

    def tsl(t):
        return slice(t * TT, (t + 1) * TT)

    for p in range(2):
        P.pool(lambda e: e.memset(S0T[p], 0.0), w=[f"S0T{p}"])
    P.pool(lambda e: e.memset(hT[1][:, :, 0:1], 0.0), w=["hT1"])
    P.dma("sp", mu, mu_in, "mu", w=["mu"])
    P.dma("sp", cvec, cvec_in, "cvec", w=["cvec"])
    P.dma("sp", lnwb, lnb_in, "lnwb", w=["lnwb"])
    P.dma("sp", cf, cf_in, "cf", w=["cf"])
    for hf in range(2):
        P.dma("sp", stage.rearrange("p (c f) -> p c f", c=4), wbig_in[:, 4 * hf:4 * hf + 4, :], "stage", w=["stage"])
        P.pool(lambda e: e.tensor_copy(out=wbig[:, 4 * hf:4 * hf + 4, :].rearrange("p c f -> p (c f)"), in_=stage),
               r=["stage"], w=["wbig"])
    P.dma("sp", stage[0:64, 0:512], w2c_in, "stage", w=["stage"])
    P.pool(lambda e: e.tensor_copy(out=w2c, in_=stage[0:64, 0:512]), r=["stage"], w=["w2c"])
    P.dma("sp", stage[:, 0:256], g2c_in, "stage", w=["stage"])
    P.pool(lambda e: e.tensor_copy(out=g2c, in_=stage[:, 0:256]), r=["stage"], w=["g2c"])

    pcnt = [0]

    def pbank():
        pcnt[0] += 1
        return pcnt[0] % 2

    xcnt = [0]

    def mix(j, hb):
        xcnt[0] += 1
        b = xcnt[0] % 2
        for dc in range(8):
            P.dve(lambda e: e.scalar_tensor_tensor(out=xm[b][:, dc, :], in0=xx[:, dc, :], scalar=mu[:, j * 8 + dc:j * 8 + dc + 1],
                                                   in1=hT[hb][:, dc, 1:1 + TT], op0=ALU.mult, op1=ALU.add),
                  r=["xx", "mu", f"hT{hb}"], w=[f"xm{b}"])
        return b

    def proj(b, c0, m, hb):
        pb = pbank()
        for dc in range(8):
            P.pe(lambda e: e.matmul(ps[pb][0:m, :], lhsT=wbig[:, dc, c0:c0 + m], rhs=xm[b][:, dc, :],
                                    start=(dc == 0), stop=(dc == 7)),
                 r=["wbig", f"xm{b}"], w=[f"ps{pb}"])
        return pb

    for t in range(nt):
        hb = t % 2
        ob = 1 - hb
        hsrc(hT[hb][:, :, 1:1 + TT], t, f"hT{hb}", [f"hT{hb}"])
        if t > 0:
            P.pool(lambda e: e.tensor_copy(out=hT[hb][:, :, 0:1], in_=hT[ob][:, :, TT:TT + 1]), r=[f"hT{ob}"], w=[f"hT{hb}"])
        else:
            P.pool(lambda e: e.memset(hT[hb][:, :, 0:1], 0.0), w=[f"hT{hb}"])
        P.dve(lambda e: e.tensor_tensor(out=xx, in0=hT[hb][:, :, 0:TT], in1=hT[hb][:, :, 1:1 + TT], op=ALU.subtract),
              r=[f"hT{hb}"], w=["xx"])
        for j, nm, c0 in ((0, "r", 0), (2, "k", 256), (3, "v", 512)):
            b = mix(j, hb)
            for p in range(2):
                pb = proj(b, c0 + 128 * p, 128, hb)
                P.act(lambda e: e.copy(out=A_[p][nm], in_=ps[pb]), r=[f"ps{pb}"], w=[f"{nm}{p}"])
        b = mix(1, hb)
        pb = proj(b, 768, 64, hb)
        P.act(lambda e: e.activation(out=lora[0:64, :], in_=ps[pb][0:64, :], func=AF.Tanh), r=[f"ps{pb}"], w=["lora"])
        for p in range(2):
            pb = pbank()
            P.pe(lambda e: e.matmul(ps[pb], lhsT=w2c[:, 128 * p:128 * p + 128], rhs=lora[0:64, :], start=True, stop=True),
                 r=["w2c", "lora"], w=[f"ps{pb}"])
            P.act(lambda e: e.activation(out=A_[p]["lg"], in_=ps[pb], func=AF.Sigmoid, bias=cvec[:, 8 * p:8 * p + 1]),
                  r=[f"ps{pb}", "cvec"], w=[f"lg{p}"])
            P.pool(lambda e: e.tensor_scalar(out=A_[p]["lg"], in0=A_[p]["lg"], scalar1=-DEC, scalar2=None, op0=ALU.mult),
                   r=[f"lg{p}"], w=[f"lg{p}"])
        b = mix(4, hb)
        pb = proj(b, 832, 64, hb)
        P.act(lambda e: e.copy(out=lora[0:64, :], in_=ps[pb][0:64, :]), r=[f"ps{pb}"], w=["lora"])
        for p in range(2):
            pb = pbank()
            P.pe(lambda e: e.matmul(ps[pb], lhsT=w2c[:, 256 + 128 * p:256 + 128 * p + 128], rhs=lora[0:64, :], start=True, stop=True),
                 r=["w2c", "lora"], w=[f"ps{pb}"])
            P.act(lambda e: e.activation(out=A_[p]["ag"], in_=ps[pb], func=AF.Sigmoid, bias=cvec[:, 8 * p + 1:8 * p + 2]),
                  r=[f"ps{pb}", "cvec"], w=[f"ag{p}"])
        b = mix(5, hb)
        pb = proj(b, 896, 128, hb)
        P.act(lambda e: e.activation(out=sg, in_=ps[pb], func=AF.Sigmoid), r=[f"ps{pb}"], w=["sg"])

        for p in range(2):
            a = A_[p]
            cv = lambda i: cvec[:, 8 * p + i:8 * p + i + 1]
            R = lambda *n: [f"{x}{p}" for x in n]
            P.pool(lambda e: e.tensor_scalar(out=a["kk"], in0=a["k"], scalar1=cv(2), scalar2=None, op0=ALU.mult),
                   r=R("k") + ["cvec"], w=R("kk"))
            P.pool(lambda e: e.tensor_tensor(out=a["tmp"], in0=a["kk"], in1=a["kk"], op=ALU.mult), r=R("kk"), w=R("tmp"))
            pb = pbank()
            P.pe(lambda e: e.matmul(ps[pb], lhsT=blockones, rhs=a["tmp"], start=True, stop=True), r=["cf"] + R("tmp"), w=[f"ps{pb}"])
            P.act(lambda e: e.activation(out=a["ssq"], in_=ps[pb], func=AF.Sqrt), r=[f"ps{pb}"], w=R("ssq"))
            P.dve(lambda e: e.tensor_scalar_max(out=a["ssq"], in0=a["ssq"], scalar1=1e-12), r=R("ssq"), w=R("ssq"))
            P.dve(lambda e: e.reciprocal(out=a["ssq"], in_=a["ssq"]), r=R("ssq"), w=R("ssq"))
            P.dve(lambda e: e.tensor_tensor(out=a["kk"], in0=a["kk"], in1=a["ssq"], op=ALU.mult), r=R("kk", "ssq"), w=R("kk"))
            P.dve(lambda e: e.tensor_scalar(out=a["km"], in0=a["ag"], scalar1=cv(3), scalar2=cv(4), op0=ALU.mult, op1=ALU.add),
                  r=R("ag") + ["cvec"], w=R("km"))
            P.dve(lambda e: e.tensor_tensor(out=a["km"], in0=a["km"], in1=a["k"], op=ALU.mult), r=R("km", "k"), w=R("km"))
            P.dve(lambda e: e.scalar_tensor_tensor(out=a["rkr"], in0=a["r"], scalar=cv(5), in1=a["km"], op0=ALU.mult, op1=ALU.mult),
                  r=R("r", "km") + ["cvec"], w=R("rkr"))
            P.dve(lambda e: e.tensor_tensor_scan(out=a["lp"], data0=keep, data1=a["lg"], initial=0.0, op0=ALU.mult, op1=ALU.add),
                  r=["cf"] + R("lg"), w=R("lp"))
            arv = ar[p]
            v3 = lambda x: x.rearrange("p (j l) -> p j l", l=LR)
            P.act(lambda e: e.activation(out=a["tmp"], in_=a["lp"], func=AF.Exp), r=R("lp"), w=R("tmp"))
            P.dve(lambda e: e.tensor_tensor(out=arv[:, :, 64:128], in0=v3(a["r"]), in1=v3(a["tmp"]), op=ALU.mult),
                  r=R("r", "tmp"), w=R("ar"))
            P.pool(lambda e: e.tensor_tensor(out=a["tmp2"], in0=a["lp"], in1=a["lg"], op=ALU.subtract), r=R("lp", "lg"), w=R("tmp2"))
            P.act(lambda e: e.activation(out=a["tmp2"], in_=a["tmp2"], func=AF.Exp), r=R("tmp2"), w=R("tmp2"))
            P.dve(lambda e: e.scalar_tensor_tensor(out=arv[:, :, 0:64], in0=v3(a["kk"]), scalar=-1.0, in1=v3(a["tmp2"]),
                                                   op0=ALU.mult, op1=ALU.mult),
                  r=R("kk", "tmp2"), w=R("ar"))
            P.act(lambda e: e.activation(out=a["tmp2"], in_=a["lp"], func=AF.Exp, scale=-1.0), r=R("lp"), w=R("tmp2"))
            P.pool(lambda e: e.tensor_tensor(out=a["bt"], in0=a["kk"], in1=a["ag"], op=ALU.mult), r=R("kk", "ag"), w=R("bt"))
            P.pool(lambda e: e.tensor_tensor(out=a["bt"], in0=a["bt"], in1=a["tmp2"], op=ALU.mult), r=R("bt", "tmp2"), w=R("bt"))
            P.dve(lambda e: e.tensor_tensor(out=a["kt"], in0=a["km"], in1=a["tmp2"], op=ALU.mult), r=R("km", "tmp2"), w=R("kt"))

            for j in range(NJ):
                c = NJ * t + j
                cs = slice(j * LR, (j + 1) * LR)
                PL = a["tmp"][:, j * LR + LR - 1:j * LR + LR]
                H = [slice(0, 64), slice(64, 128)]
                for hp in H:
                    P.pe(lambda e: e.matmul(ps[2][hp, 0:128], lhsT=a["bt"][hp, cs], rhs=arv[hp, j, :], start=True, stop=True),
                         r=R("bt", "ar"), w=["ps2"])
                    P.pe(lambda e: e.matmul(ps[2][hp, 128:256], lhsT=a["kt"][hp, cs], rhs=arv[hp, j, :], start=True, stop=True),
                         r=R("kt", "ar"), w=["ps2"])
                    P.pe(lambda e: e.matmul(ps[2][hp, 256:320], lhsT=arv[hp, j, 0:64], rhs=a["bt"][hp, cs], start=True, stop=True),
                         r=R("bt", "ar"), w=["ps2"])
                P.dve(lambda e: e.tensor_tensor(out=AM, in0=ps[2][:, 0:320], in1=mask320, op=ALU.mult), r=["ps2", "cf"], w=["AM"])
                P.pool(lambda e: e.tensor_scalar(out=bkP[:, 0:64], in0=a["bt"][:, cs], scalar1=PL, scalar2=None, op0=ALU.mult),
                       r=R("bt", "tmp"), w=["bkP"])
                P.pool(lambda e: e.tensor_scalar(out=bkP[:, 64:128], in0=a["kt"][:, cs], scalar1=PL, scalar2=None, op0=ALU.mult),
                       r=R("kt", "tmp"), w=["bkP"])
                for hp in H:
                    P.pe(lambda e: e.matmul(ps[3][hp, 0:64], lhsT=a["v"][hp, cs], rhs=identS[hp, :], start=True, stop=True),
                         r=R("v") + ["cf"], w=["ps3"])
                    P.pe(lambda e: e.matmul(ps[3][hp, 64:128], lhsT=bkP[hp, 0:64], rhs=identS[hp, :], start=True, stop=True),
                         r=["bkP", "cf"], w=["ps3"])
                    P.pe(lambda e: e.matmul(ps[3][hp, 128:192], lhsT=bkP[hp, 64:128], rhs=identS[hp, :], start=True, stop=True),
                         r=["bkP", "cf"], w=["ps3"])
                P.act(lambda e: e.copy(out=tok, in_=ps[3][:, 0:192]), r=["ps3"], w=["tok"])
                vtok = tok[:, 0:64]
                btPtok = tok[:, 64:128]
                ktPtok = tok[:, 128:192]
                for hp in H:
                    P.pe(lambda e: e.matmul(ps[4][hp, 0:64], lhsT=arv[hp, j, 0:64], rhs=S0T[p][hp, :], start=True, stop=False),
                         r=R("ar", "S0T"), w=["ps4"])
                    P.pe(lambda e: e.matmul(ps[4][hp, 0:64], lhsT=AM[hp, 128:192], rhs=vtok[hp, :], start=False, stop=True),
                         r=["AM", "tok"], w=["ps4"])
                P.act(lambda e: e.copy(out=Y, in_=ps[4][:, 0:64]), r=["ps4"], w=["Y"])
                Acur = AM[:, 0:64]
                ATcur = AM[:, 256:320]
                an, atn = "AM", "AM"
                for kq in range(6):
                    for hp in H:
                        P.pe(lambda e: e.matmul(ps[4][hp, 0:64], lhsT=Acur[hp, :], rhs=Y[hp, :], start=True, stop=True),
                             r=[an, "Y"], w=["ps4"])
                    P.dve(lambda e: e.tensor_tensor(out=Y, in0=ps[4][:, 0:64], in1=Y, op=ALU.add), r=["ps4", "Y"], w=["Y"])
                    if kq < 5:
                        for hp in H:
                            P.pe(lambda e: e.matmul(ps[5][hp, 0:64], lhsT=ATcur[hp, :], rhs=Acur[hp, :], start=True, stop=True),
                                 r=[an], w=["ps5"])
                            P.pe(lambda e: e.matmul(ps[5][hp, 64:128], lhsT=Acur[hp, :], rhs=ATcur[hp, :], start=True, stop=True),
                                 r=[an], w=["ps5"])
                        nb = kq % 2
                        P.act(lambda e: e.copy(out=An[nb], in_=ps[5][:, 0:128]), r=["ps5"], w=[f"An{nb}"])
                        Acur = An[nb][:, 0:64]
                        ATcur = An[nb][:, 64:128]
                        an = f"An{nb}"
                for hi, hp in enumerate(H):
                    P.pe(lambda e: e.matmul(ps[6][hp, 0:64], lhsT=arv[hp, j, 64:128], rhs=S0T[p][hp, :], start=True, stop=False),
                         r=R("ar", "S0T"), w=["ps6"])
                    P.pe(lambda e: e.matmul(ps[6][hp, 0:64], lhsT=AM[hp, 64:128], rhs=Y[hp, :], start=False, stop=False),
                         r=["AM", "Y"], w=["ps6"])
                    P.pe(lambda e: e.matmul(ps[6][hp, 0:64], lhsT=AM[hp, 192:256], rhs=vtok[hp, :], start=False, stop=True),
                         r=["AM", "tok"], w=["ps6"])
                    P.pe(lambda e: e.matmul(ps[6][hp, 64:65], lhsT=a["rkr"][hp, cs], rhs=onescol[hp, :], start=True, stop=True),
                         r=R("rkr") + ["cf"], w=["ps6"])
                    P.pe(lambda e: e.matmul(ps[6][hp, 128:192], lhsT=sg[:, cs], rhs=g2c[:, 128 * p + 64 * hi:128 * p + 64 * hi + 64],
                                            start=True, stop=True),
                         r=["sg", "g2c"], w=["ps6"])
                for hp in H:
                    P.pe(lambda e: e.matmul(ps[7][hp, 0:64], lhsT=btPtok[hp, :], rhs=Y[hp, :], start=True, stop=False),
                         r=["tok", "Y"], w=["ps7"])
                    P.pe(lambda e: e.matmul(ps[7][hp, 0:64], lhsT=ktPtok[hp, :], rhs=vtok[hp, :], start=False, stop=True),
                         r=["tok"], w=["ps7"])
                P.dve(lambda e: e.scalar_tensor_tensor(out=S0T[p], in0=S0T[p], scalar=PL, in1=ps[7][:, 0:64], op0=ALU.mult, op1=ALU.add),
                      r=R("S0T", "tmp") + ["ps7"], w=R("S0T"))
                P.dve(lambda e: e.bn_stats(out=st6, in_=ps[6][:, 0:64]), r=["ps6"], w=["st6"])
                P.dve(lambda e: e.bn_aggr(out=mv, in_=st6), r=["st6"], w=["mv"])
                P.act(lambda e: e.activation(out=rs, in_=mv[:, 1:2], func=AF.Sqrt, bias=GN_EPS), r=["mv"], w=["rs"])
                P.dve(lambda e: e.reciprocal(out=rs, in_=rs), r=["rs"], w=["rs"])
                P.dve(lambda e: e.tensor_scalar(out=yn, in0=ps[6][:, 0:64], scalar1=mv[:, 0:1], scalar2=rs, op0=ALU.subtract, op1=ALU.mult),
                      r=["ps6", "mv", "rs"], w=["yn"])
                P.pool(lambda e: e.tensor_tensor(out=yn, in0=yn, in1=lnwb[:, 128 * p:128 * p + 64], op=ALU.mult), r=["yn", "lnwb"], w=["yn"])
                P.pool(lambda e: e.tensor_tensor(out=yn, in0=yn, in1=lnwb[:, 128 * p + 64:128 * p + 128], op=ALU.add), r=["yn", "lnwb"], w=["yn"])
                P.act(lambda e: e.copy(out=bsc, in_=ps[6][:, 64:65]), r=["ps6"], w=["bsc"])
                P.dve(lambda e: e.scalar_tensor_tensor(out=yn, in0=vtok, scalar=bsc, in1=yn, op0=ALU.mult, op1=ALU.add),
                      r=["tok", "bsc", "yn"], w=["yn"])
                P.dve(lambda e: e.tensor_tensor(out=yo, in0=ps[6][:, 128:192], in1=yn, op=ALU.mult),
                      r=["ps6", "yn"], w=["yo"])
                for hp in H:
                    P.pe(lambda e: e.matmul(ps[3][hp, 256:320], lhsT=yo[hp, :], rhs=identS[hp, :], start=True, stop=True),
                         r=["yo", "cf"], w=["ps3"])
                P.act(lambda e: e.copy(out=ofm[p][:, cs], in_=ps[3][:, 256:320]), r=["ps3"], w=[f"ofm{p}"])
            P.dma("sp", o_out[128 * p:128 * p + 128, tsl(t)], ofm[p], "oout", r=[f"ofm{p}"])

import ml_dtypes as _mld

_PROGS = {}


def _prog(key, fn):
    if key not in _PROGS:
        _PROGS[key] = fn()
    return _PROGS[key]


def _gl(g):
    return np.ascontiguousarray(np.asarray(g, np.float32).reshape(8, 128).T)


def _mla_inputs(z, j, hT_b, pos_b, c):
    hp = c % 4
    wa = z['mla_w_a'][j]
    sw = np.concatenate([np.arange(32, 64), np.arange(0, 32)])
    wa_ext = np.concatenate([wa, wa[:, 768:832][:, sw]], axis=1)
    wqb = z['mla_w_qb'][j]
    wkvb = z['mla_w_kvb'][j]
    cols = []
    for hh in range(2):
        hd = 2 * hp + hh
        blk = wqb[:, hd * 192:(hd + 1) * 192]
        cols.append(np.concatenate([blk[:, :128], blk[:, 128:192], blk[:, 128:192][:, sw]], axis=1))
    wqb_c = np.concatenate(cols, axis=1)
    wkvb_c = np.concatenate([wkvb[:, (2 * hp + hh) * 256:(2 * hp + hh + 1) * 256] for hh in range(2)], axis=1)
    consts = np.zeros((128, 8), np.float32)
    consts[:, 0:4] = z['mla_q_norm'][j].reshape(4, 128).T
    consts[:, 4:6] = z['mla_kv_norm'][j].reshape(2, 128).T
    inv = 1.0 / (10000.0 ** (np.arange(0, 64, 2, dtype=np.float32) / 64))
    consts[0:64, 6] = np.concatenate([inv, inv]) / (2 * math.pi)
    consts[0:64, 7] = np.concatenate([-np.ones(32), np.ones(32)])
    k = np.arange(128)[:, None]
    q = np.arange(512)[None, :]
    masks = np.stack([((jj * 128 + k) <= q) for jj in range(4)], axis=1).astype(_mld.bfloat16)
    return {'h_in': hT_b, 'pos': np.ascontiguousarray(pos_b.reshape(1, -1).astype(np.int32)),
            'wa': np.ascontiguousarray(wa_ext), 'wqb': np.ascontiguousarray(wqb_c),
            'wkvb': np.ascontiguousarray(wkvb_c), 'consts': consts, 'masks': np.ascontiguousarray(masks)}


def _mls_inputs(z, j, hT_b, c):
    hp = c % 4
    hA, hB = 2 * hp, 2 * hp + 1
    W = z['ml_w_in'][j]
    cols = np.concatenate([np.arange(hA * 64, hA * 64 + 128), 512 + np.arange(hA * 64, hA * 64 + 128),
                           1024 + np.arange(hA * 128, hA * 128 + 256), 2048 + np.arange(hA * 128, hA * 128 + 256),
                           np.array([3072 + hA, 3072 + hB, 3080 + hA, 3080 + hB])])
    w_c = np.ascontiguousarray(W[:, cols])
    consts = np.zeros((128, 16), np.float32)
    cw = z['ml_conv_w'][j]
    cb = z['ml_conv_b'][j]
    consts[:, 0:4] = cw[:, hA * 64:hA * 64 + 128].T
    consts[:, 4:8] = cw[:, 512 + hA * 64:512 + hA * 64 + 128].T
    consts[:, 8] = cb[hA * 64:hA * 64 + 128]
    consts[:, 9] = cb[512 + hA * 64:512 + hA * 64 + 128]
    on = z['ml_out_norm'][j]
    consts[:, 10] = on[hA * 128:(hA + 1) * 128]
    consts[:, 11] = on[hB * 128:(hB + 1) * 128]
    bif = z['ml_b_if'][j]
    consts[0:2, 12] = bif[[hA, hB]]
    consts[0:2, 13] = -bif[[8 + hA, 8 + hB]]
    cm = np.zeros((128, 256), np.float32)
    cm[:, 0:128] = np.eye(128)
    s = np.arange(128)[:, None]
    t = np.arange(128)[None, :]
    cm[:, 128:256] = (s <= t)
    sel = np.zeros((2, 386), np.float32)
    sel[0, 0:128] = 1
    sel[1, 128:256] = 1
    sel[0, 256:320] = 1
    sel[1, 320:384] = 1
    sel[0, 384] = 1
    sel[1, 385] = 1
    return {'h_in': hT_b, 'w': w_c, 'consts': consts, 'cm': cm.astype(_mld.bfloat16), 'sel': sel}


def _rwk_consts():
    cf = np.zeros((128, 1025), np.float32)
    s = np.arange(64)[:, None]
    t = np.arange(64)[None, :]
    su = (s < t).astype(np.float32)
    ui = (s <= t).astype(np.float32)
    m = np.concatenate([su, ui, su, ui, su.T], axis=1)
    cf[:, 0:320] = np.concatenate([m, m], axis=0)
    cf[:, 320:384] = np.concatenate([np.eye(64), np.eye(64)], axis=0)
    cf[:, 384] = 1.0
    bo = np.zeros((128, 128), np.float32)
    bo[:64, :64] = 1
    bo[64:, 64:] = 1
    cf[:, 385:513] = bo
    keep = np.ones(512, np.float32)
    keep[::64] = 0
    cf[:, 513:1025] = keep[None, :]
    return cf


def _rwk_inputs(z, j, hT_b, c):
    q = c % 4
    ch = slice(q * 256, (q + 1) * 256)
    wbig = np.concatenate([z['rw_w_r'][j][:, ch], z['rw_w_k'][j][:, ch], z['rw_w_v'][j][:, ch],
                           z['rw_w1'][j], z['rw_a1'][j], z['rw_g1'][j]], axis=1)
    w2c = np.concatenate([z['rw_w2'][j][:, ch], z['rw_a2'][j][:, ch]], axis=1)
    g2c = z['rw_g2'][j][:, ch]
    mu = z['rw_mu'][j]
    mu_l = np.ascontiguousarray(mu.reshape(6, 8, 128).transpose(2, 0, 1).reshape(128, 48))
    cvec = np.zeros((128, 16), np.float32)
    rk = z['rw_r_k'][j].reshape(-1)
    one = np.ones(128, np.float32)
    for p in range(2):
        cc = slice(q * 256 + p * 128, q * 256 + (p + 1) * 128)
        cvec[:, 8 * p + 0] = z['rw_w0'][j][cc]
        cvec[:, 8 * p + 1] = z['rw_a0'][j][cc]
        cvec[:, 8 * p + 2] = z['rw_k_k'][j][cc]
        cvec[:, 8 * p + 3] = z['rw_k_a'][j][cc]
        cvec[:, 8 * p + 4] = 1.0 - z['rw_k_a'][j][cc]
        cvec[:, 8 * p + 5] = rk[cc]
    lnwb = np.zeros((128, 256), np.float32)
    for p in range(2):
        for h in range(2):
            cc = slice(q * 256 + p * 128 + h * 64, q * 256 + p * 128 + (h + 1) * 64)
            lnwb[h * 64:(h + 1) * 64, p * 128:p * 128 + 64] = z['rw_ln_w'][j][cc][None, :]
            lnwb[h * 64:(h + 1) * 64, p * 128 + 64:p * 128 + 128] = z['rw_ln_b'][j][cc][None, :]
    return {'h_in': hT_b, 'wbig': np.ascontiguousarray(wbig), 'w2c': np.ascontiguousarray(w2c),
            'g2c': np.ascontiguousarray(g2c), 'mu': mu_l, 'cvec': cvec, 'lnwb': lnwb, 'cf32': _rwk_consts()}


def _build_fused(nl=4):
    nc = bass.Bass("TRN2", target_bir_lowering=False)
    C = Ctx(nc)
    P = C.P
    dt = nc.dram_tensor
    RG = [[0, 1, 2, 3], [4, 5, 6, 7]]
    U32 = mybir.dt.uint32
    fm = lambda ap: ap.rearrange("(c p) t -> p c t", p=128)
    x_ext = fm(dt("x_in", [1024, 2048], F32, kind="ExternalInput").ap())
    y_ext = fm(dt("y_out", [1024, 2048], F32, kind="ExternalOutput").ap())
    idx_in = dt("idx", [128, 8], U32, kind="ExternalInput").ap()
    xbuf = fm(dt("xbuf", [1024, 2048], F32).ap())
    hbuf_t = dt("hbuf", [1024, 2048], BF16).ap()
    hall_t = dt("hall", [4096, 2048], BF16).ap()
    opart_t = dt("opart", [256, 8192], BF16).ap()
    oall_t = dt("oall", [1024, 8192], BF16).ap()
    hall_v = hall_t.rearrange("(k q c p) f -> q p k c f", k=4, q=4, p=128)
    oall_rows = oall_t.rearrange("r (q f) -> (r q) f", q=4)

    def hsrc(dst, t, key, wn):
        for k in range(4):
            P.dma("sp", dst[:, 2 * k:2 * k + 2, :], hall_v[t // 4][:, k, :, (t % 4) * 512:(t % 4 + 1) * 512], key, w=wn)

    def ffw_decl(l, i):
        wv = lambda ap: ap.rearrange("(c p) f -> p c f", p=128)
        return (wv(dt(f"wg_{l}_{i}", [1024, 2816], F32, kind="ExternalInput").ap()),
                wv(dt(f"wu_{l}_{i}", [1024, 2816], F32, kind="ExternalInput").ap()),
                wv(dt(f"wd_{l}_{i}", [2816, 1024], F32, kind="ExternalInput").ap()))

    def gather_h():
        P.barrier()
        for k in range(4):
            P.op("pool", lambda e: e.collective_compute("AllGather", ALU.bypass, replica_groups=RG,
                                                         ins=[hbuf_t[k * 256:(k + 1) * 256, :].opt()],
                                                         outs=[hall_t[k * 1024:(k + 1) * 1024, :].opt()]),
                 r=[], w=[f"hall{k}"], dma="cc", inc=1)
        P.barrier()
        C.reset()

    def gather_o():
        P.barrier()
        for k in range(4):
            P.op("pool", lambda e: e.collective_compute("AllGather", ALU.bypass, replica_groups=RG,
                                                         ins=[opart_t[k * 64:(k + 1) * 64, :].opt()],
                                                         outs=[oall_t[k * 256:(k + 1) * 256, :].opt()]),
                 r=[], w=[f"oall{k}"], dma="cc", inc=1)
        P.barrier()
        C.reset()

    g0 = dt("gains_a", [128, 16], F32, kind="ExternalInput").ap()
    emit_tp(C, {"x_in": x_ext, "ffw": [ffw_decl(0, 0)], "gains": g0, "x_out": xbuf, "h_out": fm(hbuf_t)}, 1, False, True, False)
    gather_h()
    for l in range(nl):
        kind = l % 3
        wv = lambda ap: ap.rearrange("(c p) f -> p c f", p=128)
        if kind == 0:
            io = {"hsrc": hsrc, "pos": dt(f"m{l}_pos", [1, 8192], I32, kind="ExternalInput").ap(),
                  "wa": wv(dt(f"m{l}_wa", [1024, 896], F32, kind="ExternalInput").ap()),
                  "wqb": wv(dt(f"m{l}_wqb", [512, 512], F32, kind="ExternalInput").ap()),
                  "wkvb": wv(dt(f"m{l}_wkvb", [256, 512], F32, kind="ExternalInput").ap()),
                  "consts": dt(f"m{l}_consts", [128, 8], F32, kind="ExternalInput").ap(),
                  "masks": dt(f"m{l}_masks", [128, 4, 512], BF16, kind="ExternalInput").ap(),
                  "o_out": opart_t}
            emit_mla(C, io)
        elif kind == 1:
            io = {"hsrc": hsrc, "w": wv(dt(f"m{l}_w", [1024, 772], F32, kind="ExternalInput").ap()),
                  "consts": dt(f"m{l}_consts", [128, 16], F32, kind="ExternalInput").ap(),
                  "cm": dt(f"m{l}_cm", [128, 256], BF16, kind="ExternalInput").ap(),
                  "sel": dt(f"m{l}_sel", [2, 386], F32, kind="ExternalInput").ap(),
                  "o_out": opart_t}
            emit_mls(C, io)
        else:
            io = {"hsrc": hsrc, "wbig": wv(dt(f"m{l}_wbig", [1024, 1024], F32, kind="ExternalInput").ap()),
                  "w2c": dt(f"m{l}_w2c", [64, 512], F32, kind="ExternalInput").ap(),
                  "g2c": dt(f"m{l}_g2c", [128, 256], F32, kind="ExternalInput").ap(),
                  "mu": dt(f"m{l}_mu", [128, 48], F32, kind="ExternalInput").ap(),
                  "cvec": dt(f"m{l}_cvec", [128, 16], F32, kind="ExternalInput").ap(),
                  "lnwb": dt(f"m{l}_lnwb", [128, 256], F32, kind="ExternalInput").ap(),
                  "cf32": dt(f"m{l}_cf32", [128, 1025], F32, kind="ExternalInput").ap(),
                  "o_out": opart_t}
            emit_rwk(C, io)
        gather_o()
        wo = wv(dt(f"wo_{l}", [1024, 1024], F32, kind="ExternalInput").ap())
        if l < nl - 1:
            gk = dt(f"gains_{l}", [128, 24], F32, kind="ExternalInput").ap()
            emit_tp(C, {"x_in": xbuf, "oall": oall_rows, "wo": wo, "idx": idx_in, "ffw": [ffw_decl(l, 1), ffw_decl(l + 1, 0)],
                        "gains": gk, "x_out": xbuf, "h_out": fm(hbuf_t)}, 2, True, True, False)
            gather_h()
        else:
            gk = dt(f"gains_{l}", [128, 16], F32, kind="ExternalInput").ap()
            emit_tp(C, {"x_in": xbuf, "oall": oall_rows, "wo": wo, "idx": idx_in, "ffw": [ffw_decl(l, 1)],
                        "gains": gk, "y_out": y_ext}, 1, True, False, True)
    P.emit()
    return nc


def kernel(nl=4, **z):
    z = {k: np.asarray(v) for k, v in z.items()}
    cores = list(range(8))
    x = z['x'].astype(np.float32)
    nc = _prog(('fused', nl), lambda: _build_fused(nl))
    ims = []
    pp = np.arange(128)[:, None]
    cc = np.arange(8)[None, :]
    for c in cores:
        b, q = c // 4, c % 4
        m = {'x_in': np.ascontiguousarray(x[b, q * 2048:(q + 1) * 2048, :].T),
             'idx': (((((cc * 128 + pp) % 256) // 64) * 256 + ((cc * 128 + pp) // 256) * 64 + (cc * 128 + pp) % 64) * 4 + q).astype(np.uint32)}
        for l in range(nl):
            for i in range(2):
                if l == nl - 1 and i == 1 and False:
                    continue
                m[f'wg_{l}_{i}'] = z['ffn_w_gate'][l, i]
                m[f'wu_{l}_{i}'] = z['ffn_w_up'][l, i]
                m[f'wd_{l}_{i}'] = z['ffn_w_down'][l, i]
        m['gains_a'] = np.concatenate([_gl(z['ffn_norm'][0, 0]), _gl(z['mix_norm'][0])], axis=1)
        dummy_h = None
        for l in range(nl):
            kind, j = l % 3, l // 3
            if kind == 0:
                d = _mla_inputs(z, j, dummy_h, z['positions'][b], c)
                wo = z['mla_w_o'][j]
            elif kind == 1:
                d = _mls_inputs(z, j, dummy_h, c)
                wo = z['ml_w_o'][j]
            else:
                d = _rwk_inputs(z, j, dummy_h, c)
                wo = z['rw_w_o'][j]
            for k, v in d.items():
                if k != 'h_in':
                    m[f'm{l}_{k}'] = v
            m[f'wo_{l}'] = np.ascontiguousarray(wo)
            if l < nl - 1:
                m[f'gains_{l}'] = np.concatenate([_gl(z['ffn_norm'][l, 1]), _gl(z['ffn_norm'][l + 1, 0]),
                                                  _gl(z['mix_norm'][l + 1])], axis=1)
            else:
                m[f'gains_{l}'] = np.concatenate([_gl(z['ffn_norm'][l, 1]), _gl(z['final_norm'])], axis=1)
        ims.append(m)
    res = run_bass_kernel_spmd(nc, ims, core_ids=cores)
    out = np.zeros((2, 8192, 1024), np.float32)
    for c in cores:
        out[c // 4, (c % 4) * 2048:(c % 4 + 1) * 2048, :] = res.results[c]['y_out'].T
    return out
```

```python
import math
from concourse.bass_utils import run_bass_kernel_spmd
import numpy as np
import concourse.bass as bass
import concourse.mybir as mybir

F32 = mybir.dt.float32
BF16 = mybir.dt.bfloat16
I32 = mybir.dt.int32
ALU = mybir.AluOpType
AF = mybir.ActivationFunctionType
AX = mybir.AxisListType

SAME_ENG_SYNC = True


class _Op:
    __slots__ = ("eng", "fn", "deps", "dma", "marked", "val", "sem", "inc", "phase")


class _Rec:
    def __init__(self):
        self.calls = []

    def __getattr__(self, name):
        def f(*a, **k):
            self.calls.append((name, a, k))
            return self
        return f


def _replay(calls):
    def fn(eng):
        ins = None
        for name, a, k in calls:
            ins = getattr(eng, name)(*a, **k)
        return ins
    return fn


class Prog:
    ENG = ("pe", "act", "dve", "pool", "sp")

    def __init__(self, nc):
        self.nc = nc
        self.ops = {e: [] for e in self.ENG}
        self.res_w = {}
        self.res_r = {}
        self.out_dmas = []
        self.all = []
        self.phase = 0
        self.bar_deps = []
        self.passed = set()
        self.last_compute = {}
        self.last_dma = {}

    def op(self, eng, fn, r=(), w=(), dma=None, out=False, inc=None):
        o = _Op()
        o.eng = eng
        rec = _Rec()
        fn(rec)
        o.fn = _replay(rec.calls)
        o.dma = dma
        o.inc = inc if inc is not None else (16 if dma is not None else 1)
        o.marked = dma is not None
        o.val = 0
        o.sem = None
        deps = []
        for x in r:
            d = self.res_w.get(x)
            if d is not None:
                deps.append((d, 0))
        for x in w:
            d = self.res_w.get(x)
            if d is not None:
                deps.append((d, 0))
            for d in self.res_r.get(x, ()):
                deps.append((d, 1))
        for x in w:
            self.res_w[x] = o
            self.res_r[x] = []
        for x in r:
            if x not in w:
                self.res_r.setdefault(x, []).append(o)
        if eng not in self.passed:
            self.passed.add(eng)
            deps.extend((d, 0) for d in self.bar_deps)
        o.deps = deps
        o.phase = self.phase
        if dma is not None:
            self.last_dma[dma] = o
        else:
            self.last_compute[eng] = o
        self.ops[eng].append(o)
        self.all.append(o)
        if out:
            self.out_dmas.append(o)
        return o

    def barrier(self):
        self.bar_deps = list(self.last_compute.values()) + list(self.last_dma.values())
        self.passed = set()
        self.res_w = {}
        self.res_r = {}
        self.phase += 1

    def pe(self, fn, r=(), w=()):
        return self.op("pe", fn, r, w)

    def act(self, fn, r=(), w=()):
        return self.op("act", fn, r, w)

    def dve(self, fn, r=(), w=()):
        return self.op("dve", fn, r, w)

    def pool(self, fn, r=(), w=()):
        return self.op("pool", fn, r, w)

    def dma(self, q, out_ap, in_ap, key, r=(), w=(), out=False, **kw):
        return self.op(q, lambda e: e.dma_start(out=out_ap, in_=in_ap, **kw), r, w, dma=key, out=out)

    def _need(self, o, d, kind):
        if d is o:
            return False
        if d.dma is not None:
            return True
        if d.eng != o.eng:
            return True
        if o.eng == "pe":
            return False
        if kind == 1:
            return False
        return SAME_ENG_SYNC

    def emit(self):
        nc = self.nc
        fin = _Op()
        fin.eng = "sp"; fin.fn = None; fin.dma = None; fin.marked = False; fin.val = 0; fin.sem = None; fin.inc = 1
        fin.deps = [(d, 0) for d in self.out_dmas] + [(d, 0) for d in self.last_dma.values()]
        fin.phase = self.phase
        self.ops["sp"].append(fin)
        for e in self.ENG:
            for o in self.ops[e]:
                o.deps = [(d, k) for (d, k) in o.deps if self._need(o, d, k)]
                for d, k in o.deps:
                    d.marked = True
        import contextlib
        stack = contextlib.ExitStack()
        esem = {}
        dsem = {}
        dcnt = {}
        cnt = {}
        for e in ("all",):
            for o in self.all:
                e = o.eng
                if o.dma is not None:
                    if o.dma not in dsem:
                        dsem[o.dma] = stack.enter_context(nc.semaphore("d_" + o.dma))
                        dcnt[o.dma] = 0
                    dcnt[o.dma] += o.inc
                    o.sem = dsem[o.dma]
                    o.val = dcnt[o.dma]
                elif o.marked:
                    ek = (e, o.phase // 4)
                    if ek not in esem:
                        esem[ek] = stack.enter_context(nc.semaphore("s_%s_%d" % ek))
                        cnt[ek] = 0
                    cnt[ek] += 1
                    o.sem = esem[ek]
                    o.val = cnt[ek]
        self.nsem = len(dsem) + len(esem)
        engobj = {"pe": "tensor", "act": "scalar", "dve": "vector", "pool": "gpsimd", "sp": "sync"}
        ops = self.ops

        def run(ename, eng):
            seen = {}
            for o in ops[ename]:
                waits = {}
                for d, k in o.deps:
                    key = d.sem.name
                    if seen.get(key, 0) < d.val:
                        seen[key] = d.val
                        waits[key] = (d.sem, d.val)
                for key, (s, v) in waits.items():
                    eng.wait_ge(s, v)
                if o.fn is None:
                    continue
                ins = o.fn(eng)
                if o.marked:
                    ins.then_inc(o.sem, o.inc)

        with nc.Block() as block:
            @block.tensor
            def _(e):
                run("pe", e)

            @block.scalar
            def _(e):
                run("act", e)

            @block.vector
            def _(e):
                run("dve", e)

            @block.gpsimd
            def _(e):
                run("pool", e)

            @block.sync
            def _(e):
                run("sp", e)
        stack.close()


class _T:
    def __init__(self, ap):
        self._ap = ap

    def ap(self):
        return self._ap


class Ctx:
    def __init__(self, nc, arena_f32=51000):
        self.nc = nc
        self.P = Prog(nc)
        self.big = nc.alloc_sbuf_tensor("arena", [128, arena_f32], F32).ap()
        self.cap = arena_f32
        self.off = 0
        self.ps = [nc.alloc_psum_tensor("ps%d" % i, [128, 512], F32).ap() for i in range(8)]

    def reset(self):
        self.off = 0

    def sb(self, name, shape, dtype):
        p = shape[0]
        n = 1
        for d in shape[1:]:
            n *= d
        esz = 4 if dtype in (F32, I32, mybir.dt.uint32) else 2
        n4 = (n * esz + 3) // 4
        n4 = (n4 + 7) // 8 * 8
        assert self.off + n4 <= self.cap, ("SBUF arena overflow", name, self.off, n4, self.cap)
        ap = self.big[0:p, self.off:self.off + n4]
        self.off += n4
        if dtype != F32:
            ap = ap.bitcast(dtype)
        ap = ap[:, 0:n]
        if len(shape) == 3:
            ap = ap.rearrange("p (a b) -> p a b", a=shape[1])
        elif len(shape) != 2:
            raise ValueError(shape)
        return _T(ap)


D = 1024
FF = 2816
T = 2048
TT = 512
NTT = T // TT
FG = 256
NFG = FF // FG
EPS = 1e-6


def emit_tp(CX, io, n_ffn, do_wo, do_mix, do_final):
    nc = CX.nc
    P = CX.P
    x_in = io["x_in"]
    if do_wo:
        oall = io["oall"]
        wo_in = io["wo"]
        idx_in = io["idx"]
    ffw = io["ffw"]
    n_gain = n_ffn + (1 if (do_mix or do_final) else 0)
    g_in = io["gains"]
    if do_final:
        y_out = io["y_out"]
    else:
        x_out = io["x_out"]
    if do_mix:
        h_out = io["h_out"]

    sb = CX.sb
    x = sb("x", [128, 8, T], F32).ap()
    h = sb("h", [128, 8, T], BF16).ap()
    gains = sb("gains_sb", [128, max(n_gain, 1) * 8], F32).ap()
    ones = sb("ones", [128, 128], BF16).ap()
    sq = sb("sq", [128, 8, TT], BF16).ap()
    sd = sb("sd", [128, TT], F32).ap()
    rstd = sb("rstd", [128, TT], F32).ap()
    wst = [[sb(f"wst{i}_{j}", [128, 8 * FG], F32).ap() for j in range(3)] for i in range(2)]
    wbf = [[sb(f"wbf{i}_{j}", [128, 8 * FG], BF16).ap() for j in range(3)] for i in range(2)]
    a_sb = [sb(f"a{i}", [128, TT], F32).ap() for i in range(2)]
    z = [sb(f"z{i}", [128, 2, TT], BF16).ap() for i in range(2)]
    ps = CX.ps
    idx = sb("idx", [128, 8], mybir.dt.uint32).ap()

    P.pool(lambda e: e.memset(ones, 1.0), w=["ones"])
    P.dma("sp", gains, g_in, "gains", w=["gains"])
    for tt in range(NTT):
        P.dma("sp", x[:, :, tt * TT:(tt + 1) * TT], x_in[:, :, tt * TT:(tt + 1) * TT], f"x{tt}", w=[f"x{tt}"])

    def tsl(tt):
        return slice(tt * TT, (tt + 1) * TT)

    def norm_stats(tt):
        P.act(lambda e: e.activation(out=sq, in_=x[:, :, tsl(tt)], func=AF.Square), r=[f"x{tt}"], w=["sq"])
        for dc in range(8):
            P.pe(lambda e, dc=dc: e.matmul(ps[6], lhsT=ones, rhs=sq[:, dc, :], start=(dc == 0), stop=(dc == 7)),
                 r=["ones", "sq"], w=["ps6"])
        P.act(lambda e: e.activation(out=sd, in_=ps[6], func=AF.Sqrt, bias=EPS, scale=1.0 / D), r=["ps6"], w=["sd"])
        P.dve(lambda e: e.reciprocal(out=rstd, in_=sd), r=["sd"], w=["rstd"])

    def norm_to_h(tt, gi):
        norm_stats(tt)
        for dc in range(8):
            P.dve(lambda e, dc=dc: e.scalar_tensor_tensor(
                out=h[:, dc, tsl(tt)], in0=x[:, dc, tsl(tt)], scalar=gains[:, gi * 8 + dc:gi * 8 + dc + 1],
                in1=rstd, op0=ALU.mult, op1=ALU.mult),
                r=[f"x{tt}", "gains", "rstd"], w=[f"h{tt}"])

    if do_wo:
        P.dma("sp", idx, idx_in, "idx", w=["idx"])
        for c in range(8):
            P.op("pool", lambda e: e.indirect_dma_start(out=h[:, c, :], out_offset=None, in_=oall,
                                                         in_offset=bass.IndirectOffsetOnAxis(ap=idx[:, c:c + 1], axis=0)),
                 r=["idx"], w=[f"h{tt}" for tt in range(NTT)], dma="ogat")
        for g in range(4):
            b = g % 2
            P.dma("sp", wst[b][0].rearrange("p (c f) -> p c f", c=8), wo_in[:, :, g * FG:(g + 1) * FG],
                  f"wst{b}0", w=[f"wst{b}0"])
            P.pool(lambda e, b=b: e.tensor_copy(out=wbf[b][0], in_=wst[b][0]), r=[f"wst{b}0"], w=[f"wbf{b}0"])
            wv = wbf[b][0].rearrange("p (c f) -> p c f", c=8)
            for tt in range(NTT):
                for j in range(2):
                    dc_out = g * 2 + j
                    pb = (tt * 2 + j) % 2
                    for kc in range(8):
                        P.pe(lambda e, kc=kc, j=j, tt=tt, pb=pb, wv=wv: e.matmul(
                            ps[4 + pb], lhsT=wv[:, kc, j * 128:(j + 1) * 128], rhs=h[:, kc, tsl(tt)],
                            start=(kc == 0), stop=(kc == 7)),
                            r=[f"wbf{b}0", f"h{tt}"], w=[f"ps{4 + pb}"])
                    P.dve(lambda e, dc_out=dc_out, tt=tt, pb=pb: e.tensor_tensor(
                        out=x[:, dc_out, tsl(tt)], in0=ps[4 + pb], in1=x[:, dc_out, tsl(tt)], op=ALU.add),
                        r=[f"ps{4 + pb}", f"x{tt}"], w=[f"x{tt}"])

    pend = [None]
    for s in range(n_ffn):
        wg, wu, wd = ffw[s]
        for tt in range(NTT):
            norm_to_h(tt, s)
        for g in range(NFG):
            b = g % 2
            fs = slice(g * FG, (g + 1) * FG)
            P.dma("sp", wst[b][0].rearrange("p (c f) -> p c f", c=8), wg[:, :, fs], f"wst{b}0", w=[f"wst{b}0"])
            P.dma("sp", wst[b][1].rearrange("p (c f) -> p c f", c=8), wu[:, :, fs], f"wst{b}1", w=[f"wst{b}1"])
            P.dma("sp", wst[b][2].rearrange("p (c f) -> p c f", c=2), wd[:, 2 * g:2 * g + 2, :], f"wst{b}2",
                  w=[f"wst{b}2"])
            for j in range(3):
                P.pool(lambda e, b=b, j=j: e.tensor_copy(out=wbf[b][j], in_=wst[b][j]),
                       r=[f"wst{b}{j}"], w=[f"wbf{b}{j}"])
            wgv = wbf[b][0].rearrange("p (c f) -> p c f", c=8)
            wuv = wbf[b][1].rearrange("p (c f) -> p c f", c=8)
            wdv = wbf[b][2].rearrange("p (c f) -> p c f", c=2)
            for tt in range(NTT):
                zb = tt % 2
                for fc in range(2):
                    pg = 2 * fc
                    pu = 2 * fc + 1
                    for dc in range(8):
                        P.pe(lambda e, dc=dc, fc=fc, tt=tt, pg=pg, wgv=wgv: e.matmul(
                            ps[pg], lhsT=wgv[:, dc, fc * 128:(fc + 1) * 128], rhs=h[:, dc, tsl(tt)],
                            start=(dc == 0), stop=(dc == 7)),
                            r=[f"wbf{b}0", f"h{tt}"], w=[f"ps{pg}"])
                    for dc in range(8):
                        P.pe(lambda e, dc=dc, fc=fc, tt=tt, pu=pu, wuv=wuv: e.matmul(
                            ps[pu], lhsT=wuv[:, dc, fc * 128:(fc + 1) * 128], rhs=h[:, dc, tsl(tt)],
                            start=(dc == 0), stop=(dc == 7)),
                            r=[f"wbf{b}1", f"h{tt}"], w=[f"ps{pu}"])
                    P.act(lambda e, fc=fc, pg=pg: e.activation(out=a_sb[fc], in_=ps[pg], func=AF.Silu),
                          r=[f"ps{pg}"], w=[f"a{fc}"])
                    P.dve(lambda e, fc=fc, pu=pu, zb=zb: e.tensor_tensor(
                        out=z[zb][:, fc, :], in0=a_sb[fc], in1=ps[pu], op=ALU.mult),
                        r=[f"a{fc}", f"ps{pu}"], w=[f"z{zb}"])
                if pend[0] is not None:
                    pend[0]()

                def down(tt=tt, zb=zb, wdv=wdv, b=b):
                    for dc in range(8):
                        pb = 4 + dc % 2
                        for fc in range(2):
                            P.pe(lambda e: e.matmul(
                                ps[pb], lhsT=wdv[:, fc, dc * 128:(dc + 1) * 128], rhs=z[zb][:, fc, :],
                                start=(fc == 0), stop=(fc == 1)),
                                r=[f"wbf{b}2", f"z{zb}"], w=[f"ps{pb}"])
                        P.dve(lambda e: e.scalar_tensor_tensor(
                            out=x[:, dc, tsl(tt)], in0=ps[pb], scalar=0.5, in1=x[:, dc, tsl(tt)],
                            op0=ALU.mult, op1=ALU.add),
                            r=[f"ps{pb}", f"x{tt}"], w=[f"x{tt}"])
                pend[0] = down
        pend[0]()
        pend[0] = None

    if do_mix:
        for tt in range(NTT):
            norm_to_h(tt, n_ffn)
            P.dma("sp", h_out[:, :, tsl(tt)], h[:, :, tsl(tt)], "hout", r=[f"h{tt}"])
    if do_final:
        for tt in range(NTT):
            norm_stats(tt)
            for dc in range(8):
                P.dve(lambda e, dc=dc, tt=tt: e.scalar_tensor_tensor(
                    out=x[:, dc, tsl(tt)], in0=x[:, dc, tsl(tt)],
                    scalar=gains[:, n_ffn * 8 + dc:n_ffn * 8 + dc + 1],
                    in1=rstd, op0=ALU.mult, op1=ALU.mult),
                    r=[f"x{tt}", "gains", "rstd"], w=[f"x{tt}"])
            P.dma("sp", y_out[:, :, tsl(tt)], x[:, :, tsl(tt)], "xout", r=[f"x{tt}"], out=True)
    else:
        for tt in range(NTT):
            P.dma("sp", x_out[:, :, tsl(tt)], x[:, :, tsl(tt)], "xout", r=[f"x{tt}"])

import math

D = 1024
S = 8192
TT = 512
NT = S // TT
QR, KVR, ROPE, NOPE, DV = 512, 256, 64, 128, 128
LATC = 896
EPS = 1e-6
SCALE = (NOPE + ROPE) ** -0.5


def emit_mla(CX, io):
    nc = CX.nc
    P = CX.P
    hsrc = io["hsrc"]
    pos_in = io["pos"]
    wa_in = io["wa"]
    wqb_in = io["wqb"]
    wkvb_in = io["wkvb"]
    c_in = io["consts"]
    masks_in = io["masks"]
    o_out = io["o_out"]

    sb = CX.sb
    stage = sb("stage", [128, 4 * LATC], F32).ap()
    wa = sb("wa_sb", [128, 8, LATC], BF16).ap()
    wqb = sb("wqb_sb", [128, 4, 512], BF16).ap()
    wkvb = sb("wkvb_sb", [128, 2, 512], BF16).ap()
    consts = sb("consts_sb", [128, 8], F32).ap()
    masks = sb("masks_sb", [128, 4, 512], BF16).ap()
    ones = sb("ones", [128, 128], BF16).ap()
    hT = [sb(f"hT{i}", [128, 8, TT], BF16).ap() for i in range(2)]
    lat = sb("lat", [128, 6, TT], F32).ap()
    sq = sb("sq", [128, 6, TT], BF16).ap()
    sd = sb("sd", [128, TT], F32).ap()
    rq = sb("rq", [128, TT], F32).ap()
    rkv = sb("rkv", [128, TT], F32).ap()
    cqn = sb("cqn", [128, 4, TT], BF16).ap()
    ckvn = sb("ckvn", [128, 2, TT], BF16).ap()
    posi = sb("posi", [64, TT], I32).ap()
    ui = sb("ui", [64, TT], I32).ap()
    u = sb("u", [64, TT], F32).ap()
    uc = sb("uc", [64, TT], F32).ap()
    wr = sb("wr", [64, TT], F32).ap()
    cos2 = sb("cos2", [64, TT], F32).ap()
    sin2 = sb("sin2", [64, TT], F32).ap()
    t1 = sb("t1", [64, TT], F32).ap()
    t2 = sb("t2", [64, TT], F32).ap()
    Kn = [sb(f"Kn{i}", [128, S], BF16).ap() for i in range(2)]
    Kr = sb("Kr", [64, S], BF16).ap()
    V = [sb(f"V{i}", [128, S // 128, DV], BF16).ap() for i in range(2)]
    qn = [sb(f"qn{i}", [128, TT], BF16).ap() for i in range(2)]
    qr = [sb(f"qr{i}", [64, TT], BF16).ap() for i in range(2)]
    pT = [sb(f"pT{i}", [128, TT], BF16).ap() for i in range(4)]
    rden = sb("rden", [128, TT], F32).ap()
    osb = [sb(f"osb{i}", [128, TT], BF16).ap() for i in range(2)]
    ps = CX.ps

    P.pool(lambda e: e.memset(ones, 1.0), w=["ones"])
    P.dma("sp", consts, c_in, "consts", w=["consts"])
    P.dma("sp", masks, masks_in, "masks", w=["masks"])
    for hf in range(2):
        P.dma("sp", stage.rearrange("p (c f) -> p c f", c=4), wa_in[:, 4 * hf:4 * hf + 4, :], "stage", w=["stage"])
        P.pool(lambda e, hf=hf: e.tensor_copy(out=wa[:, 4 * hf:4 * hf + 4, :].rearrange("p c f -> p (c f)"), in_=stage),
               r=["stage"], w=["wa"])
    P.dma("sp", stage[:, 0:2048].rearrange("p (c f) -> p c f", c=4), wqb_in, "stage", w=["stage"])
    P.pool(lambda e: e.tensor_copy(out=wqb.rearrange("p c f -> p (c f)"), in_=stage[:, 0:2048]), r=["stage"], w=["wqb"])
    P.dma("sp", stage[:, 0:1024].rearrange("p (c f) -> p c f", c=2), wkvb_in, "stage", w=["stage"])
    P.pool(lambda e: e.tensor_copy(out=wkvb.rearrange("p c f -> p (c f)"), in_=stage[:, 0:1024]), r=["stage"], w=["wkvb"])

    pcount = [0]

    def pbank():
        pcount[0] += 1
        return pcount[0] % 2

    def tsl(t):
        return slice(t * TT, (t + 1) * TT)

    def rope(src_ps, src_sw_ps, dst, rd, wt):
        P.dve(lambda e: e.tensor_tensor(out=t1, in0=src_ps, in1=cos2, op=ALU.mult), r=rd + ["cos2"], w=["t1"])
        P.dve(lambda e: e.tensor_tensor(out=t2, in0=src_sw_ps, in1=sin2, op=ALU.mult), r=rd + ["sin2"], w=["t2"])
        P.dve(lambda e: e.tensor_tensor(out=dst, in0=t1, in1=t2, op=ALU.add), r=["t1", "t2"], w=wt)

    for t in range(NT):
        hb = t % 2
        hsrc(hT[hb], t, f"hT{hb}", [f"hT{hb}"])
        P.dma("sp", posi, pos_in[:, tsl(t)].partition_broadcast(64), "posi", w=["posi"])
        P.dve(lambda e: e.tensor_copy(out=t1, in_=posi), r=["posi"], w=["t1"])
        P.dve(lambda e: e.tensor_scalar(out=u, in0=t1, scalar1=consts[0:64, 6:7], scalar2=None, op0=ALU.mult),
              r=["t1", "consts"], w=["u"])
        P.dve(lambda e: e.tensor_copy(out=ui, in_=u), r=["u"], w=["ui"])
        P.dve(lambda e: e.tensor_copy(out=t2, in_=ui), r=["ui"], w=["t2"])
        P.dve(lambda e: e.tensor_tensor(out=u, in0=u, in1=t2, op=ALU.subtract), r=["u", "t2"], w=["u"])
        P.dve(lambda e: e.tensor_single_scalar(out=wr, in_=u, scalar=0.5, op=ALU.is_gt), r=["u"], w=["wr"])
        P.dve(lambda e: e.tensor_tensor(out=u, in0=u, in1=wr, op=ALU.subtract), r=["u", "wr"], w=["u"])
        P.dve(lambda e: e.tensor_single_scalar(out=wr, in_=u, scalar=-0.5, op=ALU.is_lt), r=["u"], w=["wr"])
        P.dve(lambda e: e.tensor_tensor(out=u, in0=u, in1=wr, op=ALU.add), r=["u", "wr"], w=["u"])
        P.dve(lambda e: e.tensor_scalar_add(out=uc, in0=u, scalar1=0.25), r=["u"], w=["uc"])
        P.dve(lambda e: e.tensor_single_scalar(out=wr, in_=uc, scalar=0.5, op=ALU.is_gt), r=["uc"], w=["wr"])
        P.dve(lambda e: e.tensor_tensor(out=uc, in0=uc, in1=wr, op=ALU.subtract), r=["uc", "wr"], w=["uc"])
        P.act(lambda e: e.activation(out=sin2, in_=u, func=AF.Sin, scale=2 * math.pi), r=["u"], w=["sin2"])
        P.act(lambda e: e.activation(out=cos2, in_=uc, func=AF.Sin, scale=2 * math.pi), r=["uc"], w=["cos2"])
        P.dve(lambda e: e.tensor_scalar(out=sin2, in0=sin2, scalar1=consts[0:64, 7:8], scalar2=None, op0=ALU.mult),
              r=["sin2", "consts"], w=["sin2"])
        for c in range(6):
            pb = pbank()
            for dc in range(8):
                P.pe(lambda e, c=c, dc=dc, pb=pb: e.matmul(ps[pb], lhsT=wa[:, dc, c * 128:(c + 1) * 128],
                                                            rhs=hT[hb][:, dc, :], start=(dc == 0), stop=(dc == 7)),
                     r=["wa", f"hT{hb}"], w=[f"ps{pb}"])
            P.act(lambda e, c=c, pb=pb: e.copy(out=lat[:, c, :], in_=ps[pb]), r=[f"ps{pb}"], w=[f"lat{c}"])
        pbs = []
        for c2 in range(2):
            pb = pbank()
            pbs.append(pb)
            for dc in range(8):
                P.pe(lambda e, c2=c2, dc=dc, pb=pb: e.matmul(ps[pb][0:64, :], lhsT=wa[:, dc, 768 + 64 * c2:832 + 64 * c2],
                                                              rhs=hT[hb][:, dc, :], start=(dc == 0), stop=(dc == 7)),
                     r=["wa", f"hT{hb}"], w=[f"ps{pb}"])
        rope(ps[pbs[0]][0:64, :], ps[pbs[1]][0:64, :], Kr[:, tsl(t)], [f"ps{pbs[0]}", f"ps{pbs[1]}"], [f"Kr{t}"])
        P.act(lambda e: e.activation(out=sq, in_=lat[:, 0:6, :], func=AF.Square),
              r=[f"lat{c}" for c in range(6)], w=["sq"])
        pb = pbank()
        for c in range(4):
            P.pe(lambda e, c=c, pb=pb: e.matmul(ps[pb], lhsT=ones, rhs=sq[:, c, :], start=(c == 0), stop=(c == 3)),
                 r=["ones", "sq"], w=[f"ps{pb}"])
        P.act(lambda e, pb=pb: e.activation(out=sd, in_=ps[pb], func=AF.Sqrt, bias=EPS, scale=1.0 / QR),
              r=[f"ps{pb}"], w=["sd"])
        P.dve(lambda e: e.reciprocal(out=rq, in_=sd), r=["sd"], w=["rq"])
        pb = pbank()
        for c in range(2):
            P.pe(lambda e, c=c, pb=pb: e.matmul(ps[pb], lhsT=ones, rhs=sq[:, 4 + c, :], start=(c == 0), stop=(c == 1)),
                 r=["ones", "sq"], w=[f"ps{pb}"])
        P.act(lambda e, pb=pb: e.activation(out=sd, in_=ps[pb], func=AF.Sqrt, bias=EPS, scale=1.0 / KVR),
              r=[f"ps{pb}"], w=["sd"])
        P.dve(lambda e: e.reciprocal(out=rkv, in_=sd), r=["sd"], w=["rkv"])
        for c in range(4):
            P.dve(lambda e, c=c: e.scalar_tensor_tensor(out=cqn[:, c, :], in0=lat[:, c, :], scalar=consts[:, c:c + 1],
                                                        in1=rq, op0=ALU.mult, op1=ALU.mult),
                  r=[f"lat{c}", "rq", "consts"], w=["cqn"])
        for c in range(2):
            P.dve(lambda e, c=c: e.scalar_tensor_tensor(out=ckvn[:, c, :], in0=lat[:, 4 + c, :],
                                                        scalar=consts[:, 4 + c:5 + c],
                                                        in1=rkv, op0=ALU.mult, op1=ALU.mult),
                  r=[f"lat{4 + c}", "rkv", "consts"], w=["ckvn"])
        for hh in range(2):
            qo = hh * 256
            pb = pbank()
            for c in range(4):
                P.pe(lambda e, c=c, pb=pb, qo=qo: e.matmul(ps[pb], lhsT=wqb[:, c, qo:qo + 128], rhs=cqn[:, c, :],
                                                            start=(c == 0), stop=(c == 3)),
                     r=["wqb", "cqn"], w=[f"ps{pb}"])
            P.act(lambda e, pb=pb, hh=hh: e.copy(out=qn[hh], in_=ps[pb]), r=[f"ps{pb}"], w=[f"qn{hh}"])
            pbs = []
            for c2 in range(2):
                pb = pbank()
                pbs.append(pb)
                for c in range(4):
                    P.pe(lambda e, c=c, c2=c2, pb=pb, qo=qo: e.matmul(
                        ps[pb][0:64, :], lhsT=wqb[:, c, qo + 128 + 64 * c2:qo + 192 + 64 * c2], rhs=cqn[:, c, :],
                        start=(c == 0), stop=(c == 3)),
                        r=["wqb", "cqn"], w=[f"ps{pb}"])
            rope(ps[pbs[0]][0:64, :], ps[pbs[1]][0:64, :], qr[hh], [f"ps{pbs[0]}", f"ps{pbs[1]}"], [f"qr{hh}"])
            ko = hh * 256
            pb = pbank()
            for c in range(2):
                P.pe(lambda e, c=c, pb=pb, ko=ko: e.matmul(ps[pb], lhsT=wkvb[:, c, ko:ko + 128], rhs=ckvn[:, c, :],
                                                            start=(c == 0), stop=(c == 1)),
                     r=["wkvb", "ckvn"], w=[f"ps{pb}"])
            P.act(lambda e, pb=pb, hh=hh, t=t: e.copy(out=Kn[hh][:, tsl(t)], in_=ps[pb]), r=[f"ps{pb}"],
                  w=[f"Kn{hh}_{t}"])
            pb = pbank()
            for j in range(4):
                for c in range(2):
                    P.pe(lambda e, c=c, j=j, pb=pb, ko=ko: e.matmul(
                        ps[pb][:, j * 128:(j + 1) * 128], lhsT=ckvn[:, c, j * 128:(j + 1) * 128],
                        rhs=wkvb[:, c, ko + 128:ko + 256], start=(c == 0), stop=(c == 1)),
                        r=["wkvb", "ckvn"], w=[f"ps{pb}"])
            P.act(lambda e, pb=pb, hh=hh, t=t: e.copy(
                out=V[hh][:, 4 * t:4 * t + 4, :], in_=ps[pb].rearrange("p (j d) -> p j d", j=4)),
                r=[f"ps{pb}"], w=[f"V{hh}_{t}"])
        SB = [2, 3, 6, 7]
        LA = 2
        for hh in range(2):
            ob = 4
            nkb = 4 * t + 4

            def qk(kb):
                sbk = SB[kb % 4]
                j = kb - 4 * t
                q0 = max(j, 0) * 128
                tk = kb // 4
                ksl = slice(kb * 128, (kb + 1) * 128)
                P.pe(lambda e: e.matmul(ps[sbk][:, q0:], lhsT=Kn[hh][:, ksl], rhs=qn[hh][:, q0:], start=True, stop=False),
                     r=[f"Kn{hh}_{tk}", f"qn{hh}"], w=[f"ps{sbk}"])
                P.pe(lambda e: e.matmul(ps[sbk][:, q0:], lhsT=Kr[:, ksl], rhs=qr[hh][:, q0:], start=False, stop=True),
                     r=[f"Kr{tk}", f"qr{hh}"], w=[f"ps{sbk}"])
                pi = kb % 4
                P.act(lambda e: e.activation(out=pT[pi][:, q0:], in_=ps[sbk][:, q0:], func=AF.Exp, scale=SCALE),
                      r=[f"ps{sbk}"], w=[f"pT{pi}"])
                if j >= 0:
                    P.pool(lambda e: e.tensor_tensor(out=pT[pi][:, q0:], in0=pT[pi][:, q0:], in1=masks[:, j, q0:], op=ALU.mult),
                           r=[f"pT{pi}", "masks"], w=[f"pT{pi}"])

            def pv(kb):
                j = kb - 4 * t
                q0 = max(j, 0) * 128
                tk = kb // 4
                pi = kb % 4
                P.pe(lambda e: e.matmul(ps[ob][:, q0:], lhsT=V[hh][:, kb, :], rhs=pT[pi][:, q0:], start=(kb == 0), stop=(kb == nkb - 1)),
                     r=[f"V{hh}_{tk}", f"pT{pi}"], w=[f"ps{ob}"])
                P.pe(lambda e: e.matmul(ps[ob + 1][:, q0:], lhsT=ones, rhs=pT[pi][:, q0:], start=(kb == 0), stop=(kb == nkb - 1)),
                     r=["ones", f"pT{pi}"], w=[f"ps{ob + 1}"])

            for i in range(nkb + LA):
                if i < nkb:
                    qk(i)
                if i - LA >= 0:
                    pv(i - LA)
            P.dve(lambda e: e.reciprocal(out=rden, in_=ps[ob + 1]), r=[f"ps{ob + 1}"], w=["rden"])
            P.dve(lambda e: e.tensor_tensor(out=osb[hh], in0=ps[ob], in1=rden, op=ALU.mult),
                  r=[f"ps{ob}", "rden"], w=[f"osb{hh}"])
            P.dma("sp", o_out[hh * DV:(hh + 1) * DV, tsl(t)], osb[hh], "oout", r=[f"osb{hh}"])


D = 1024
S = 8192
TT = 512
NT = S // TT
LM = 128
NCM = S // LM
EPS = 1e-6
WC = 772


def emit_mls(CX, io):
    nc = CX.nc
    P = CX.P
    hsrc = io["hsrc"]
    w_in = io["w"]
    c_in = io["consts"]
    cm_in = io["cm"]
    sel_in = io["sel"]
    o_out = io["o_out"]

    sb = CX.sb
    stage = sb("stage", [128, 4 * WC], F32).ap()
    w = sb("w_sb", [128, 8, WC], BF16).ap()
    consts = sb("consts_sb", [128, 16], F32).ap()
    cm = sb("cm_sb", [128, 256], BF16).ap()
    ident = cm[:, 0:128]
    tril = cm[:, 128:256]
    sel = sb("sel_sb", [2, 386], F32).ap()
    ones = sb("ones", [128, 128], BF16).ap()
    hT = [sb(f"hT{i}", [128, 8, TT], BF16).ap() for i in range(2)]
    gi = sb("gi", [2, S], F32).ap()
    gf = sb("gf", [2, S], F32).ap()
    mu = sb("mu", [2, S], F32).ap()
    Gcol = sb("Gcol", [128, NCM, 2], F32).ap()
    nmu = [sb(f"nmu{i}", [128, NCM + 1], F32).ap() for i in range(2)]
    pmuq = sb("pmuq", [128, NCM + 1], F32).ap()
    mub = [sb(f"mub{i}", [128, TT], F32).ap() for i in range(2)]
    muq = sb("muq", [128, TT], F32).ap()
    emt = [sb(f"emt{i}", [128, TT], F32).ap() for i in range(2)]
    xq = sb("xq", [128, 3 + TT], F32).ap()
    xk = sb("xk", [128, 3 + TT], F32).ap()
    cacc = sb("cacc", [128, TT], F32).ap()
    csil = sb("csil", [128, TT], F32).ap()
    qT = sb("qT", [128, TT], BF16).ap()
    qsT = sb("qsT", [128, TT], BF16).ap()
    kT = sb("kT", [128, TT], BF16).ap()
    ktok = sb("ktok", [128, 4, 128], F32).ap()
    kw = sb("kw", [128, 128], BF16).ap()
    vtok = [sb(f"vtok{i}", [128, 4, 128], BF16).ap() for i in range(2)]
    sigo = [sb(f"sigo{i}", [128, TT], F32).ap() for i in range(2)]
    Wt = sb("Wt", [128, 128], F32).ap()
    Wm = sb("Wm", [128, 128], F32).ap()
    pT = sb("pT", [128, 128], BF16).ap()
    rb = sb("rb", [128, TT], F32).ap()
    wcol = sb("wcol", [128, 2], F32).ap()
    carry = sb("carry", [128, 1], F32).ap()
    C = sb("C", [128, 128], F32).ap()
    Cb = sb("Cb", [128, 128], BF16).ap()
    Nb = sb("Nb", [128, 128], F32).ap()
    Nbb = sb("Nbb", [128, 128], BF16).ap()
    dn = sb("dn", [128, 128], F32).ap()
    hid = [sb(f"hid{i}", [128, TT], F32).ap() for i in range(2)]
    hsq = sb("hsq", [128, TT], BF16).ap()
    sd = sb("sd", [128, TT], F32).ap()
    rstd = sb("rstd", [128, TT], F32).ap()
    ho = [sb(f"ho{i}", [128, TT], BF16).ap() for i in range(2)]
    ps = CX.ps

    def tsl(t):
        return slice(t * TT, (t + 1) * TT)

    P.pool(lambda e: e.memset(ones, 1.0), w=["ones"])
    P.pool(lambda e: e.memset(C, 0.0), w=["C"])
    P.pool(lambda e: e.memset(Cb, 0.0), w=["Cb"])
    P.pool(lambda e: e.memset(Nb, 0.0), w=["Nb"])
    P.pool(lambda e: e.memset(Nbb, 0.0), w=["Nbb"])
    P.pool(lambda e: e.memset(xq[:, 0:3], 0.0), w=["xq"])
    P.pool(lambda e: e.memset(xk[:, 0:3], 0.0), w=["xk"])
    for i in range(2):
        P.pool(lambda e, i=i: e.memset(nmu[i][:, 0:1], 0.0), w=[f"nmu{i}"])
    P.pool(lambda e: e.memset(pmuq[:, 0:1], 0.0), w=["pmuq"])
    P.dma("sp", consts, c_in, "consts", w=["consts"])
    P.dma("sp", cm, cm_in, "cm", w=["cm"])
    P.dma("sp", sel, sel_in, "sel", w=["sel"])
    for hf in range(2):
        P.dma("sp", stage.rearrange("p (c f) -> p c f", c=4), w_in[:, 4 * hf:4 * hf + 4, :], "stage", w=["stage"])
        P.pool(lambda e: e.tensor_copy(out=w[:, 4 * hf:4 * hf + 4, :].rearrange("p c f -> p (c f)"), in_=stage),
               r=["stage"], w=["w"])

    for t in range(NT):
        hb = t % 2
        hsrc(hT[hb], t, f"hT{hb}", [f"hT{hb}"])
        for g2 in range(2):
            pb = g2
            for dc in range(8):
                P.pe(lambda e: e.matmul(ps[pb][0:2, :], lhsT=w[:, dc, 768 + 2 * g2:770 + 2 * g2], rhs=hT[hb][:, dc, :],
                                        start=(dc == 0), stop=(dc == 7)),
                     r=["w", f"hT{hb}"], w=[f"ps{pb}"])
        P.act(lambda e: e.activation(out=gi[:, tsl(t)], in_=ps[0][0:2, :], func=AF.Identity, bias=consts[0:2, 12:13]),
              r=["ps0", "consts"], w=["gi"])
        P.act(lambda e: e.activation(out=gf[:, tsl(t)], in_=ps[1][0:2, :], func=AF.Exp, bias=consts[0:2, 13:14], scale=-1.0),
              r=["ps1", "consts"], w=["gf"])
    P.act(lambda e: e.activation(out=gf, in_=gf, func=AF.Ln, bias=1.0, scale=1.0), r=["gf"], w=["gf"])
    P.dve(lambda e: e.tensor_scalar(out=gf, in0=gf, scalar1=-0.5, scalar2=None, op0=ALU.mult), r=["gf"], w=["gf"])
    P.dve(lambda e: e.tensor_tensor_scan(out=gf, data0=gf, data1=gf, initial=0.0, op0=ALU.add, op1=ALU.add),
          r=["gf"], w=["gf"])
    P.dve(lambda e: e.tensor_tensor(out=gi, in0=gi, in1=gf, op=ALU.subtract), r=["gi", "gf"], w=["gi"])
    P.dve(lambda e: e.tensor_tensor_scan(out=mu, data0=gi, data1=gi, initial=0.0, op0=ALU.max, op1=ALU.max),
          r=["gi"], w=["mu"])
    P.dve(lambda e: e.tensor_tensor(out=gf, in0=gf, in1=mu, op=ALU.add), r=["gf", "mu"], w=["gf"])
    fm = gf
    id2 = sel[:, 384:386]
    for c in range(NCM):
        P.pe(lambda e: e.matmul(ps[2][:, 2 * c:2 * c + 2], lhsT=gi[:, c * LM:(c + 1) * LM], rhs=id2, start=True, stop=True),
             r=["gi", "sel"], w=["ps2"])
    P.act(lambda e: e.copy(out=Gcol.rearrange("p c h -> p (c h)"), in_=ps[2][:, 0:2 * NCM]), r=["ps2"], w=["Gcol"])

    pcnt = [0]

    def pbank():
        pcnt[0] += 1
        return pcnt[0] % 2

    for t in range(NT):
        hb = t % 2
        hsrc(hT[hb], t, f"hT{hb}", [f"hT{hb}"])
        for hh in range(2):
            P.pe(lambda e: e.matmul(ps[2], lhsT=sel[:, hh * 128:(hh + 1) * 128], rhs=mu[:, tsl(t)], start=True, stop=True),
                 r=["sel", "mu"], w=["ps2"])
            P.act(lambda e: e.copy(out=mub[hh], in_=ps[2]), r=["ps2"], w=[f"mub{hh}"])
            P.act(lambda e: e.activation(out=nmu[hh][:, 4 * t + 1:4 * t + 5], in_=ps[2][:, LM - 1::LM], func=AF.Copy, scale=-1.0),
                  r=["ps2"], w=[f"nmu{hh}"])
            P.pe(lambda e: e.matmul(ps[2], lhsT=sel[:, hh * 128:(hh + 1) * 128], rhs=fm[:, tsl(t)], start=True, stop=True),
                 r=["sel", "gf"], w=["ps2"])
            P.act(lambda e: e.activation(out=emt[hh], in_=ps[2], func=AF.Exp, scale=-1.0), r=["ps2"], w=[f"emt{hh}"])
        P.pe(lambda e: e.matmul(ps[2], lhsT=sel[:, 256:384], rhs=mu[:, tsl(t)], start=True, stop=True),
             r=["sel", "mu"], w=["ps2"])
        P.act(lambda e: e.copy(out=muq, in_=ps[2]), r=["ps2"], w=["muq"])
        P.act(lambda e: e.copy(out=pmuq[:, 4 * t + 1:4 * t + 5], in_=ps[2][:, LM - 1::LM]), r=["ps2"], w=["pmuq"])
        for which, xbuf, dst in ((0, xq, qT), (1, xk, kT)):
            pb = pbank()
            xn = "xq" if which == 0 else "xk"
            for dc in range(8):
                P.pe(lambda e: e.matmul(ps[pb], lhsT=w[:, dc, which * 128:(which + 1) * 128], rhs=hT[hb][:, dc, :],
                                        start=(dc == 0), stop=(dc == 7)),
                     r=["w", f"hT{hb}"], w=[f"ps{pb}"])
            P.act(lambda e: e.copy(out=xbuf[:, 3:3 + TT], in_=ps[pb]), r=[f"ps{pb}"], w=[xn])
            cw = 4 * which
            P.dve(lambda e: e.tensor_scalar(out=cacc, in0=xbuf[:, 0:TT], scalar1=consts[:, cw:cw + 1],
                                            scalar2=consts[:, 8 + which:9 + which], op0=ALU.mult, op1=ALU.add),
                  r=[xn, "consts"], w=["cacc"])
            for j in range(1, 4):
                P.dve(lambda e: e.scalar_tensor_tensor(out=cacc, in0=xbuf[:, j:j + TT], scalar=consts[:, cw + j:cw + j + 1],
                                                       in1=cacc, op0=ALU.mult, op1=ALU.add),
                      r=[xn, "consts", "cacc"], w=["cacc"])
            P.pool(lambda e: e.tensor_copy(out=xbuf[:, 0:3], in_=xbuf[:, TT:TT + 3]), r=[xn, "cacc"], w=[xn])
            P.act(lambda e: e.activation(out=csil, in_=cacc, func=AF.Silu), r=["cacc"], w=["csil"])
            if which == 0:
                P.dve(lambda e: e.tensor_copy(out=dst, in_=csil), r=["csil"], w=["qT"])
            else:
                P.dve(lambda e: e.tensor_scalar(out=dst, in0=csil, scalar1=0.125, scalar2=None, op0=ALU.mult),
                      r=["csil"], w=["kT"])
        for hh in range(2):
            pb = pbank()
            for dc in range(8):
                P.pe(lambda e: e.matmul(ps[pb], lhsT=w[:, dc, 512 + hh * 128:640 + hh * 128], rhs=hT[hb][:, dc, :],
                                        start=(dc == 0), stop=(dc == 7)),
                     r=["w", f"hT{hb}"], w=[f"ps{pb}"])
            P.act(lambda e: e.activation(out=sigo[hh], in_=ps[pb], func=AF.Sigmoid), r=[f"ps{pb}"], w=[f"sigo{hh}"])
        for hh in range(2):
            pb = pbank()
            for j in range(4):
                for dc in range(8):
                    P.pe(lambda e: e.matmul(ps[pb][:, j * 128:(j + 1) * 128], lhsT=hT[hb][:, dc, j * 128:(j + 1) * 128],
                                            rhs=w[:, dc, 256 + hh * 128:384 + hh * 128], start=(dc == 0), stop=(dc == 7)),
                         r=["w", f"hT{hb}"], w=[f"ps{pb}"])
            P.act(lambda e: e.copy(out=vtok[hh].rearrange("p j d -> p (j d)"), in_=ps[pb]), r=[f"ps{pb}"], w=[f"vtok{hh}"])
        pb = pbank()
        for j in range(4):
            P.pe(lambda e: e.matmul(ps[pb][:, j * 128:(j + 1) * 128], lhsT=kT[:, j * 128:(j + 1) * 128], rhs=ident,
                                    start=True, stop=True),
                 r=["kT", "cm"], w=[f"ps{pb}"])
        P.act(lambda e: e.copy(out=ktok.rearrange("p j d -> p (j d)"), in_=ps[pb]), r=[f"ps{pb}"], w=["ktok"])
        for j in range(4):
            c = 4 * t + j
            P.act(lambda e: e.activation(out=rb[:, j * LM:(j + 1) * LM], in_=muq[:, j * LM:(j + 1) * LM], func=AF.Exp,
                                         bias=pmuq[:, c:c + 1], scale=-1.0),
                  r=["muq", "pmuq"], w=["rb"])
        P.dve(lambda e: e.tensor_tensor(out=qsT, in0=qT, in1=rb, op=ALU.mult), r=["qT", "rb"], w=["qsT"])
        for j in range(4):
            c = 4 * t + j
            cs = slice(j * LM, (j + 1) * LM)
            for hh in range(2):
                hp = slice(hh * 64, (hh + 1) * 64)
                P.pe(lambda e: e.matmul(ps[3][:, 0:128], lhsT=kT[hp, cs], rhs=qT[hp, cs], start=True, stop=True),
                     r=["kT", "qT"], w=["ps3"])
                P.act(lambda e: e.activation(out=Wt, in_=mub[hh][:, cs], func=AF.Exp, bias=Gcol[:, c, hh:hh + 1], scale=-1.0),
                      r=[f"mub{hh}", "Gcol"], w=["Wt"])
                P.pool(lambda e: e.tensor_tensor(out=Wm, in0=Wt, in1=tril, op=ALU.mult), r=["Wt", "cm"], w=["Wm"])
                P.dve(lambda e: e.tensor_tensor(out=pT, in0=ps[3][:, 0:128], in1=Wm, op=ALU.mult), r=["ps3", "Wm"], w=["pT"])
                P.pe(lambda e: e.matmul(ps[4][:, 0:128], lhsT=vtok[hh][:, j, :], rhs=pT, start=True, stop=False),
                     r=[f"vtok{hh}", "pT"], w=["ps4"])
                P.pe(lambda e: e.matmul(ps[4][:, 0:128], lhsT=Cb[hp, :], rhs=qsT[hp, cs], start=False, stop=True),
                     r=["Cb", "qsT"], w=["ps4"])
                P.pe(lambda e: e.matmul(ps[5][:, 0:128], lhsT=ones, rhs=pT, start=True, stop=False),
                     r=["ones", "pT"], w=["ps5"])
                P.pe(lambda e: e.matmul(ps[5][:, 0:128], lhsT=Nbb[hp, :], rhs=qsT[hp, cs], start=False, stop=True),
                     r=["Nbb", "qsT"], w=["ps5"])
                P.act(lambda e: e.activation(out=dn, in_=ps[5][:, 0:128], func=AF.Abs), r=["ps5"], w=["dn"])
                P.dve(lambda e: e.tensor_tensor(out=dn, in0=dn, in1=emt[hh][:, cs], op=ALU.max),
                      r=["dn", f"emt{hh}"], w=["dn"])
                P.dve(lambda e: e.reciprocal(out=dn, in_=dn), r=["dn"], w=["dn"])
                P.dve(lambda e: e.tensor_tensor(out=hid[hh][:, cs], in0=ps[4][:, 0:128], in1=dn, op=ALU.mult),
                      r=["ps4", "dn"], w=[f"hid{hh}"])
                P.act(lambda e: e.activation(out=wcol[:, hh:hh + 1], in_=Gcol[:, c, hh:hh + 1], func=AF.Exp,
                                             bias=nmu[hh][:, c + 1:c + 2], scale=1.0),
                      r=["Gcol", f"nmu{hh}"], w=["wcol"])
                P.pool(lambda e: e.tensor_scalar(out=kw[:, hp], in0=ktok[:, j, hp], scalar1=wcol[:, hh:hh + 1], scalar2=None,
                                                 op0=ALU.mult),
                       r=["ktok", "wcol"], w=["kw"])
            P.act(lambda e: e.activation(out=carry, in_=pmuq[:, c + 1:c + 2], func=AF.Exp, bias=pmuq[:, c:c + 1], scale=-1.0),
                  r=["pmuq"], w=["carry"])
            for hh in range(2):
                hp = slice(hh * 64, (hh + 1) * 64)
                P.pe(lambda e: e.matmul(ps[6][hp, 0:128], lhsT=kw[:, hp], rhs=vtok[hh][:, j, :], start=True, stop=True),
                     r=["kw", f"vtok{hh}"], w=["ps6"])
            P.pe(lambda e: e.matmul(ps[7][:, 0:128], lhsT=kw, rhs=ones, start=True, stop=True), r=["kw", "ones"], w=["ps7"])
            P.dve(lambda e: e.scalar_tensor_tensor(out=C, in0=C, scalar=carry, in1=ps[6][:, 0:128], op0=ALU.mult, op1=ALU.add),
                  r=["C", "carry", "ps6"], w=["C"])
            P.pool(lambda e: e.tensor_copy(out=Cb, in_=C), r=["C"], w=["Cb"])
            P.dve(lambda e: e.scalar_tensor_tensor(out=Nb, in0=Nb, scalar=carry, in1=ps[7][:, 0:128], op0=ALU.mult, op1=ALU.add),
                  r=["Nb", "carry", "ps7"], w=["Nb"])
            P.pool(lambda e: e.tensor_copy(out=Nbb, in_=Nb), r=["Nb"], w=["Nbb"])
        for hh in range(2):
            P.act(lambda e: e.activation(out=hsq, in_=hid[hh], func=AF.Square), r=[f"hid{hh}"], w=["hsq"])
            pb = pbank()
            P.pe(lambda e: e.matmul(ps[pb], lhsT=ones, rhs=hsq, start=True, stop=True), r=["ones", "hsq"], w=[f"ps{pb}"])
            P.act(lambda e: e.activation(out=sd, in_=ps[pb], func=AF.Sqrt, bias=EPS, scale=1.0 / 128), r=[f"ps{pb}"], w=["sd"])
            P.dve(lambda e: e.reciprocal(out=rstd, in_=sd), r=["sd"], w=["rstd"])
            P.dve(lambda e: e.scalar_tensor_tensor(out=rstd, in0=rstd, scalar=consts[:, 10 + hh:11 + hh], in1=sigo[hh],
                                                   op0=ALU.mult, op1=ALU.mult),
                  r=["rstd", "consts", f"sigo{hh}"], w=["rstd"])
            P.dve(lambda e: e.tensor_tensor(out=ho[hh], in0=hid[hh], in1=rstd, op=ALU.mult), r=[f"hid{hh}", "rstd"], w=[f"ho{hh}"])
            P.dma("sp", o_out[hh * 128:(hh + 1) * 128, tsl(t)], ho[hh], "oout", r=[f"ho{hh}"])

import math

D = 1024
S = 8192
TT = 512
NT = S // TT
LR = 64
NJ = TT // LR
NCR = S // LR
GN_EPS = 64e-5
DEC = math.exp(-0.5)


def emit_rwk(CX, io, nt=NT):
    nc = CX.nc
    P = CX.P
    hsrc = io["hsrc"]
    wbig_in = io["wbig"]
    w2c_in = io["w2c"]
    g2c_in = io["g2c"]
    mu_in = io["mu"]
    cvec_in = io["cvec"]
    lnb_in = io["lnwb"]
    cf_in = io["cf32"]
    o_out = io["o_out"]

    sb = CX.sb
    stage = sb("stage", [128, 4096], F32).ap()
    wbig = sb("wbig_sb", [128, 8, 1024], BF16).ap()
    w2c = sb("w2c_sb", [64, 512], BF16).ap()
    g2c = sb("g2c_sb", [128, 256], BF16).ap()
    mu = sb("mu_sb", [128, 48], F32).ap()
    cvec = sb("cvec_sb", [128, 16], F32).ap()
    lnwb = sb("lnwb_sb", [128, 256], F32).ap()
    cf = sb("cf_sb", [128, 1025], F32).ap()
    mask320 = cf[:, 0:320]
    identS = cf[:, 320:384]
    onescol = cf[:, 384:385]
    blockones = cf[:, 385:513]
    keep = cf[:, 513:1025]
    hT = [sb(f"hT{i}", [128, 8, 1 + TT], BF16).ap() for i in range(2)]
    xx = sb("xx", [128, 8, TT], F32).ap()
    xm = [sb(f"xm{i}", [128, 8, TT], BF16).ap() for i in range(2)]
    lora = sb("lora", [128, TT], BF16).ap()
    sg = sb("sg", [128, TT], BF16).ap()
    names = ["r", "k", "v", "lg", "ag", "kk", "km", "lp", "tmp", "tmp2", "bt", "kt", "rkr", "ssq"]
    A_ = [{n: sb(f"{n}{p}", [128, TT], F32).ap() for n in names} for p in range(2)]
    ar = [sb(f"ar{p}", [128, NJ, 128], F32).ap() for p in range(2)]
    S0T = [sb(f"S0T{p}", [128, 64], F32).ap() for p in range(2)]
    AMs = [sb(f"AM{p}", [128, 320], F32).ap() for p in range(2)]
    bkPs = [sb(f"bkP{p}", [128, 128], F32).ap() for p in range(2)]
    toks = [sb(f"tok{p}", [128, 192], F32).ap() for p in range(2)]
    Ys = [sb(f"Y{p}", [128, 64], F32).ap() for p in range(2)]
    Ans = [[sb(f"An{p}_{i}", [128, 128], F32).ap() for i in range(2)] for p in range(2)]
    st6s = [sb(f"st6{p}", [128, 6], F32).ap() for p in range(2)]
    mvs = [sb(f"mv{p}", [128, 2], F32).ap() for p in range(2)]
    rss = [sb(f"rs{p}", [128, 1], F32).ap() for p in range(2)]
    bscs = [sb(f"bsc{p}", [128, 1], F32).ap() for p in range(2)]
    yns = [sb(f"yn{p}", [128, 64], F32).ap() for p in range(2)]
    yos = [sb(f"yo{p}", [128, 64], F32).ap() for p in range(2)]
    ofm = [sb(f"ofm{p}", [128, TT], BF16).ap() for p in range(2)]
    ps = CX.ps

    def tsl(t):
        return slice(t * TT, (t + 1) * TT)

    for p in range(2):
        P.pool(lambda e: e.memset(S0T[p], 0.0), w=[f"S0T{p}"])
    P.pool(lambda e: e.memset(hT[1][:, :, 0:1], 0.0), w=["hT1"])
    P.dma("sp", mu, mu_in, "mu", w=["mu"])
    P.dma("sp", cvec, cvec_in, "cvec", w=["cvec"])
    P.dma("sp", lnwb, lnb_in, "lnwb", w=["lnwb"])
    P.dma("sp", cf, cf_in, "cf", w=["cf"])
    for hf in range(2):
        P.dma("sp", stage.rearrange("p (c f) -> p c f", c=4), wbig_in[:, 4 * hf:4 * hf + 4, :], "stage", w=["stage"])
        P.pool(lambda e: e.tensor_copy(out=wbig[:, 4 * hf:4 * hf + 4, :].rearrange("p c f -> p (c f)"), in_=stage),
               r=["stage"], w=["wbig"])
    P.dma("sp", stage[0:64, 0:512], w2c_in, "stage", w=["stage"])
    P.pool(lambda e: e.tensor_copy(out=w2c, in_=stage[0:64, 0:512]), r=["stage"], w=["w2c"])
    P.dma("sp", stage[:, 0:256], g2c_in, "stage", w=["stage"])
    P.pool(lambda e: e.tensor_copy(out=g2c, in_=stage[:, 0:256]), r=["stage"], w=["g2c"])

    pcnt = [0]

    def pbank():
        pcnt[0] += 1
        return pcnt[0] % 2

    xcnt = [0]

    def mix(j, hb):
        xcnt[0] += 1
        b = xcnt[0] % 2
        for dc in range(8):
            P.dve(lambda e: e.scalar_tensor_tensor(out=xm[b][:, dc, :], in0=xx[:, dc, :], scalar=mu[:, j * 8 + dc:j * 8 + dc + 1],
                                                   in1=hT[hb][:, dc, 1:1 + TT], op0=ALU.mult, op1=ALU.add),
                  r=["xx", "mu", f"hT{hb}"], w=[f"xm{b}"])
        return b

    def proj(b, c0, m, hb):
        pb = pbank()
        for dc in range(8):
            P.pe(lambda e: e.matmul(ps[pb][0:m, :], lhsT=wbig[:, dc, c0:c0 + m], rhs=xm[b][:, dc, :],
                                    start=(dc == 0), stop=(dc == 7)),
                 r=["wbig", f"xm{b}"], w=[f"ps{pb}"])
        return pb

    for t in range(nt):
        hb = t % 2
        ob = 1 - hb
        hsrc(hT[hb][:, :, 1:1 + TT], t, f"hT{hb}", [f"hT{hb}"])
        if t > 0:
            P.pool(lambda e: e.tensor_copy(out=hT[hb][:, :, 0:1], in_=hT[ob][:, :, TT:TT + 1]), r=[f"hT{ob}"], w=[f"hT{hb}"])
        else:
            P.pool(lambda e: e.memset(hT[hb][:, :, 0:1], 0.0), w=[f"hT{hb}"])
        P.dve(lambda e: e.tensor_tensor(out=xx, in0=hT[hb][:, :, 0:TT], in1=hT[hb][:, :, 1:1 + TT], op=ALU.subtract),
              r=[f"hT{hb}"], w=["xx"])
        for j, nm, c0 in ((0, "r", 0), (2, "k", 256), (3, "v", 512)):
            b = mix(j, hb)
            for p in range(2):
                pb = proj(b, c0 + 128 * p, 128, hb)
                P.act(lambda e: e.copy(out=A_[p][nm], in_=ps[pb]), r=[f"ps{pb}"], w=[f"{nm}{p}"])
        b = mix(1, hb)
        pb = proj(b, 768, 64, hb)
        P.act(lambda e: e.activation(out=lora[0:64, :], in_=ps[pb][0:64, :], func=AF.Tanh), r=[f"ps{pb}"], w=["lora"])
        for p in range(2):
            pb = pbank()
            P.pe(lambda e: e.matmul(ps[pb], lhsT=w2c[:, 128 * p:128 * p + 128], rhs=lora[0:64, :], start=True, stop=True),
                 r=["w2c", "lora"], w=[f"ps{pb}"])
            P.act(lambda e: e.activation(out=A_[p]["lg"], in_=ps[pb], func=AF.Sigmoid, bias=cvec[:, 8 * p:8 * p + 1]),
                  r=[f"ps{pb}", "cvec"], w=[f"lg{p}"])
            P.pool(lambda e: e.tensor_scalar(out=A_[p]["lg"], in0=A_[p]["lg"], scalar1=-DEC, scalar2=None, op0=ALU.mult),
                   r=[f"lg{p}"], w=[f"lg{p}"])
        b = mix(4, hb)
        pb = proj(b, 832, 64, hb)
        P.act(lambda e: e.copy(out=lora[0:64, :], in_=ps[pb][0:64, :]), r=[f"ps{pb}"], w=["lora"])
        for p in range(2):
            pb = pbank()
            P.pe(lambda e: e.matmul(ps[pb], lhsT=w2c[:, 256 + 128 * p:256 + 128 * p + 128], rhs=lora[0:64, :], start=True, stop=True),
                 r=["w2c", "lora"], w=[f"ps{pb}"])
            P.act(lambda e: e.activation(out=A_[p]["ag"], in_=ps[pb], func=AF.Sigmoid, bias=cvec[:, 8 * p + 1:8 * p + 2]),
                  r=[f"ps{pb}", "cvec"], w=[f"ag{p}"])
        b = mix(5, hb)
        pb = proj(b, 896, 128, hb)
        P.act(lambda e: e.activation(out=sg, in_=ps[pb], func=AF.Sigmoid), r=[f"ps{pb}"], w=["sg"])

        for p in range(2):
            a = A_[p]
            cv = lambda i: cvec[:, 8 * p + i:8 * p + i + 1]
            R = lambda *n: [f"{x}{p}" for x in n]
            P.pool(lambda e: e.tensor_scalar(out=a["kk"], in0=a["k"], scalar1=cv(2), scalar2=None, op0=ALU.mult),
                   r=R("k") + ["cvec"], w=R("kk"))
            P.pool(lambda e: e.tensor_tensor(out=a["tmp"], in0=a["kk"], in1=a["kk"], op=ALU.mult), r=R("kk"), w=R("tmp"))
            pb = pbank()
            P.pe(lambda e: e.matmul(ps[pb], lhsT=blockones, rhs=a["tmp"], start=True, stop=True), r=["cf"] + R("tmp"), w=[f"ps{pb}"])
            P.act(lambda e: e.activation(out=a["ssq"], in_=ps[pb], func=AF.Sqrt), r=[f"ps{pb}"], w=R("ssq"))
            P.dve(lambda e: e.tensor_scalar_max(out=a["ssq"], in0=a["ssq"], scalar1=1e-12), r=R("ssq"), w=R("ssq"))
            P.dve(lambda e: e.reciprocal(out=a["ssq"], in_=a["ssq"]), r=R("ssq"), w=R("ssq"))
            P.dve(lambda e: e.tensor_tensor(out=a["kk"], in0=a["kk"], in1=a["ssq"], op=ALU.mult), r=R("kk", "ssq"), w=R("kk"))
            P.dve(lambda e: e.tensor_scalar(out=a["km"], in0=a["ag"], scalar1=cv(3), scalar2=cv(4), op0=ALU.mult, op1=ALU.add),
                  r=R("ag") + ["cvec"], w=R("km"))
            P.dve(lambda e: e.tensor_tensor(out=a["km"], in0=a["km"], in1=a["k"], op=ALU.mult), r=R("km", "k"), w=R("km"))
            P.dve(lambda e: e.scalar_tensor_tensor(out=a["rkr"], in0=a["r"], scalar=cv(5), in1=a["km"], op0=ALU.mult, op1=ALU.mult),
                  r=R("r", "km") + ["cvec"], w=R("rkr"))
            P.dve(lambda e: e.tensor_tensor_scan(out=a["lp"], data0=keep, data1=a["lg"], initial=0.0, op0=ALU.mult, op1=ALU.add),
                  r=["cf"] + R("lg"), w=R("lp"))
            arv = ar[p]
            v3 = lambda x: x.rearrange("p (j l) -> p j l", l=LR)
            P.act(lambda e: e.activation(out=a["tmp"], in_=a["lp"], func=AF.Exp), r=R("lp"), w=R("tmp"))
            P.dve(lambda e: e.tensor_tensor(out=arv[:, :, 64:128], in0=v3(a["r"]), in1=v3(a["tmp"]), op=ALU.mult),
                  r=R("r", "tmp"), w=R("ar"))
            P.pool(lambda e: e.tensor_tensor(out=a["tmp2"], in0=a["lp"], in1=a["lg"], op=ALU.subtract), r=R("lp", "lg"), w=R("tmp2"))
            P.act(lambda e: e.activation(out=a["tmp2"], in_=a["tmp2"], func=AF.Exp), r=R("tmp2"), w=R("tmp2"))
            P.dve(lambda e: e.scalar_tensor_tensor(out=arv[:, :, 0:64], in0=v3(a["kk"]), scalar=-1.0, in1=v3(a["tmp2"]),
                                                   op0=ALU.mult, op1=ALU.mult),
                  r=R("kk", "tmp2"), w=R("ar"))
            P.act(lambda e: e.activation(out=a["tmp2"], in_=a["lp"], func=AF.Exp, scale=-1.0), r=R("lp"), w=R("tmp2"))
            P.pool(lambda e: e.tensor_tensor(out=a["bt"], in0=a["kk"], in1=a["ag"], op=ALU.mult), r=R("kk", "ag"), w=R("bt"))
            P.pool(lambda e: e.tensor_tensor(out=a["bt"], in0=a["bt"], in1=a["tmp2"], op=ALU.mult), r=R("bt", "tmp2"), w=R("bt"))
            P.dve(lambda e: e.tensor_tensor(out=a["kt"], in0=a["km"], in1=a["tmp2"], op=ALU.mult), r=R("km", "tmp2"), w=R("kt"))

        if True:
            def chunk(p, j):
                CH = 4 if p == 0 else 0
                SQ = 5 if p == 0 else 1
                a = A_[p]
                cv = lambda i: cvec[:, 8 * p + i:8 * p + i + 1]
                R = lambda *n: [f"{x}{p}" for x in n]
                arv = ar[p]
                AM = AMs[p]
                bkP = bkPs[p]
                tok = toks[p]
                Y = Ys[p]
                An = Ans[p]
                st6 = st6s[p]
                mv = mvs[p]
                rs = rss[p]
                bsc = bscs[p]
                yn = yns[p]
                yo = yos[p]
                c = NJ * t + j
                cs = slice(j * LR, (j + 1) * LR)
                PL = a["tmp"][:, j * LR + LR - 1:j * LR + LR]
                H = [slice(0, 64), slice(64, 128)]
                for hp in H:
                    P.pe(lambda e: e.matmul(ps[2][hp, 0:128], lhsT=a["bt"][hp, cs], rhs=arv[hp, j, :], start=True, stop=True),
                         r=R("bt", "ar"), w=["ps2"])
                    P.pe(lambda e: e.matmul(ps[2][hp, 128:256], lhsT=a["kt"][hp, cs], rhs=arv[hp, j, :], start=True, stop=True),
                         r=R("kt", "ar"), w=["ps2"])
                    P.pe(lambda e: e.matmul(ps[2][hp, 256:320], lhsT=arv[hp, j, 0:64], rhs=a["bt"][hp, cs], start=True, stop=True),
                         r=R("bt", "ar"), w=["ps2"])
                P.dve(lambda e: e.tensor_tensor(out=AM, in0=ps[2][:, 0:320], in1=mask320, op=ALU.mult), r=["ps2", "cf"], w=[f"AM{p}"])
                yield
                P.pool(lambda e: e.tensor_scalar(out=bkP[:, 0:64], in0=a["bt"][:, cs], scalar1=PL, scalar2=None, op0=ALU.mult),
                       r=R("bt", "tmp"), w=[f"bkP{p}"])
                P.pool(lambda e: e.tensor_scalar(out=bkP[:, 64:128], in0=a["kt"][:, cs], scalar1=PL, scalar2=None, op0=ALU.mult),
                       r=R("kt", "tmp"), w=[f"bkP{p}"])
                for hp in H:
                    P.pe(lambda e: e.matmul(ps[3][hp, 0:64], lhsT=a["v"][hp, cs], rhs=identS[hp, :], start=True, stop=True),
                         r=R("v") + ["cf"], w=["ps3"])
                    P.pe(lambda e: e.matmul(ps[3][hp, 64:128], lhsT=bkP[hp, 0:64], rhs=identS[hp, :], start=True, stop=True),
                         r=[f"bkP{p}", "cf"], w=["ps3"])
                    P.pe(lambda e: e.matmul(ps[3][hp, 128:192], lhsT=bkP[hp, 64:128], rhs=identS[hp, :], start=True, stop=True),
                         r=[f"bkP{p}", "cf"], w=["ps3"])
                P.act(lambda e: e.copy(out=tok, in_=ps[3][:, 0:192]), r=["ps3"], w=[f"tok{p}"])
                yield
                vtok = tok[:, 0:64]
                btPtok = tok[:, 64:128]
                ktPtok = tok[:, 128:192]
                for hp in H:
                    P.pe(lambda e: e.matmul(ps[CH][hp, 0:64], lhsT=arv[hp, j, 0:64], rhs=S0T[p][hp, :], start=True, stop=False),
                         r=R("ar", "S0T"), w=[f"ps{CH}"])
                    P.pe(lambda e: e.matmul(ps[CH][hp, 0:64], lhsT=AM[hp, 128:192], rhs=vtok[hp, :], start=False, stop=True),
                         r=[f"AM{p}", f"tok{p}"], w=[f"ps{CH}"])
                P.act(lambda e: e.copy(out=Y, in_=ps[CH][:, 0:64]), r=[f"ps{CH}"], w=[f"Y{p}"])
                yield
                Acur = AM[:, 0:64]
                ATcur = AM[:, 256:320]
                an, atn = f"AM{p}", f"AM{p}"
                for kq in range(6):
                    for hp in H:
                        P.pe(lambda e: e.matmul(ps[CH][hp, 0:64], lhsT=Acur[hp, :], rhs=Y[hp, :], start=True, stop=True),
                             r=[an, f"Y{p}"], w=[f"ps{CH}"])
                    P.dve(lambda e: e.tensor_tensor(out=Y, in0=ps[CH][:, 0:64], in1=Y, op=ALU.add), r=[f"ps{CH}", f"Y{p}"], w=[f"Y{p}"])
                    yield
                    if kq < 5:
                        for hp in H:
                            P.pe(lambda e: e.matmul(ps[SQ][hp, 0:64], lhsT=ATcur[hp, :], rhs=Acur[hp, :], start=True, stop=True),
                                 r=[an], w=[f"ps{SQ}"])
                            P.pe(lambda e: e.matmul(ps[SQ][hp, 64:128], lhsT=Acur[hp, :], rhs=ATcur[hp, :], start=True, stop=True),
                                 r=[an], w=[f"ps{SQ}"])
                        nb = kq % 2
                        P.act(lambda e: e.copy(out=An[nb], in_=ps[SQ][:, 0:128]), r=[f"ps{SQ}"], w=[f"An{p}_{nb}"])
                        yield
                        Acur = An[nb][:, 0:64]
                        ATcur = An[nb][:, 64:128]
                        an = f"An{p}_{nb}"
                for hi, hp in enumerate(H):
                    P.pe(lambda e: e.matmul(ps[6][hp, 0:64], lhsT=arv[hp, j, 64:128], rhs=S0T[p][hp, :], start=True, stop=False),
                         r=R("ar", "S0T"), w=["ps6"])
                    P.pe(lambda e: e.matmul(ps[6][hp, 0:64], lhsT=AM[hp, 64:128], rhs=Y[hp, :], start=False, stop=False),
                         r=[f"AM{p}", f"Y{p}"], w=["ps6"])
                    P.pe(lambda e: e.matmul(ps[6][hp, 0:64], lhsT=AM[hp, 192:256], rhs=vtok[hp, :], start=False, stop=True),
                         r=[f"AM{p}", f"tok{p}"], w=["ps6"])
                    P.pe(lambda e: e.matmul(ps[6][hp, 64:65], lhsT=a["rkr"][hp, cs], rhs=onescol[hp, :], start=True, stop=True),
                         r=R("rkr") + ["cf"], w=["ps6"])
                    P.pe(lambda e: e.matmul(ps[6][hp, 128:192], lhsT=sg[:, cs], rhs=g2c[:, 128 * p + 64 * hi:128 * p + 64 * hi + 64],
                                            start=True, stop=True),
                         r=["sg", "g2c"], w=["ps6"])
                for hp in H:
                    P.pe(lambda e: e.matmul(ps[7][hp, 0:64], lhsT=btPtok[hp, :], rhs=Y[hp, :], start=True, stop=False),
                         r=[f"tok{p}", f"Y{p}"], w=["ps7"])
                    P.pe(lambda e: e.matmul(ps[7][hp, 0:64], lhsT=ktPtok[hp, :], rhs=vtok[hp, :], start=False, stop=True),
                         r=[f"tok{p}"], w=["ps7"])
                P.dve(lambda e: e.scalar_tensor_tensor(out=S0T[p], in0=S0T[p], scalar=PL, in1=ps[7][:, 0:64], op0=ALU.mult, op1=ALU.add),
                      r=R("S0T", "tmp") + ["ps7"], w=R("S0T"))
                P.dve(lambda e: e.bn_stats(out=st6, in_=ps[6][:, 0:64]), r=["ps6"], w=[f"st6{p}"])
                P.dve(lambda e: e.bn_aggr(out=mv, in_=st6), r=[f"st6{p}"], w=[f"mv{p}"])
                P.act(lambda e: e.activation(out=rs, in_=mv[:, 1:2], func=AF.Sqrt, bias=GN_EPS), r=[f"mv{p}"], w=[f"rs{p}"])
                P.dve(lambda e: e.reciprocal(out=rs, in_=rs), r=[f"rs{p}"], w=[f"rs{p}"])
                P.dve(lambda e: e.tensor_scalar(out=yn, in0=ps[6][:, 0:64], scalar1=mv[:, 0:1], scalar2=rs, op0=ALU.subtract, op1=ALU.mult),
                      r=["ps6", f"mv{p}", f"rs{p}"], w=[f"yn{p}"])
                P.pool(lambda e: e.tensor_tensor(out=yn, in0=yn, in1=lnwb[:, 128 * p:128 * p + 64], op=ALU.mult), r=[f"yn{p}", "lnwb"], w=[f"yn{p}"])
                P.pool(lambda e: e.tensor_tensor(out=yn, in0=yn, in1=lnwb[:, 128 * p + 64:128 * p + 128], op=ALU.add), r=[f"yn{p}", "lnwb"], w=[f"yn{p}"])
                P.act(lambda e: e.copy(out=bsc, in_=ps[6][:, 64:65]), r=["ps6"], w=[f"bsc{p}"])
                P.dve(lambda e: e.scalar_tensor_tensor(out=yn, in0=vtok, scalar=bsc, in1=yn, op0=ALU.mult, op1=ALU.add),
                      r=[f"tok{p}", f"bsc{p}", f"yn{p}"], w=[f"yn{p}"])
                P.dve(lambda e: e.tensor_tensor(out=yo, in0=ps[6][:, 128:192], in1=yn, op=ALU.mult),
                      r=["ps6", f"yn{p}"], w=[f"yo{p}"])
                for hp in H:
                    P.pe(lambda e: e.matmul(ps[3][hp, 256:320], lhsT=yo[hp, :], rhs=identS[hp, :], start=True, stop=True),
                         r=[f"yo{p}", "cf"], w=["ps3"])
                P.act(lambda e: e.copy(out=ofm[p][:, cs], in_=ps[3][:, 256:320]), r=["ps3"], w=[f"ofm{p}"])
        for j in range(NJ):
            gens = [chunk(0, j), chunk(1, j)]
            while gens:
                for g in list(gens):
                    try:
                        next(g)
                    except StopIteration:
                        gens.remove(g)
        for p in range(2):
            P.dma("sp", o_out[128 * p:128 * p + 128, tsl(t)], ofm[p], "oout", r=[f"ofm{p}"])

import ml_dtypes as _mld

_PROGS = {}


def _prog(key, fn):
    if key not in _PROGS:
        _PROGS[key] = fn()
    return _PROGS[key]


def _gl(g):
    return np.ascontiguousarray(np.asarray(g, np.float32).reshape(8, 128).T)


def _mla_inputs(z, j, hT_b, pos_b, c):
    hp = c % 4
    wa = z['mla_w_a'][j]
    sw = np.concatenate([np.arange(32, 64), np.arange(0, 32)])
    wa_ext = np.concatenate([wa, wa[:, 768:832][:, sw]], axis=1)
    wqb = z['mla_w_qb'][j]
    wkvb = z['mla_w_kvb'][j]
    cols = []
    for hh in range(2):
        hd = 2 * hp + hh
        blk = wqb[:, hd * 192:(hd + 1) * 192]
        cols.append(np.concatenate([blk[:, :128], blk[:, 128:192], blk[:, 128:192][:, sw]], axis=1))
    wqb_c = np.concatenate(cols, axis=1)
    wkvb_c = np.concatenate([wkvb[:, (2 * hp + hh) * 256:(2 * hp + hh + 1) * 256] for hh in range(2)], axis=1)
    consts = np.zeros((128, 8), np.float32)
    consts[:, 0:4] = z['mla_q_norm'][j].reshape(4, 128).T
    consts[:, 4:6] = z['mla_kv_norm'][j].reshape(2, 128).T
    inv = 1.0 / (10000.0 ** (np.arange(0, 64, 2, dtype=np.float32) / 64))
    consts[0:64, 6] = np.concatenate([inv, inv]) / (2 * math.pi)
    consts[0:64, 7] = np.concatenate([-np.ones(32), np.ones(32)])
    k = np.arange(128)[:, None]
    q = np.arange(512)[None, :]
    masks = np.stack([((jj * 128 + k) <= q) for jj in range(4)], axis=1).astype(_mld.bfloat16)
    return {'h_in': hT_b, 'pos': np.ascontiguousarray(pos_b.reshape(1, -1).astype(np.int32)),
            'wa': np.ascontiguousarray(wa_ext), 'wqb': np.ascontiguousarray(wqb_c),
            'wkvb': np.ascontiguousarray(wkvb_c), 'consts': consts, 'masks': np.ascontiguousarray(masks)}


def _mls_inputs(z, j, hT_b, c):
    hp = c % 4
    hA, hB = 2 * hp, 2 * hp + 1
    W = z['ml_w_in'][j]
    cols = np.concatenate([np.arange(hA * 64, hA * 64 + 128), 512 + np.arange(hA * 64, hA * 64 + 128),
                           1024 + np.arange(hA * 128, hA * 128 + 256), 2048 + np.arange(hA * 128, hA * 128 + 256),
                           np.array([3072 + hA, 3072 + hB, 3080 + hA, 3080 + hB])])
    w_c = np.ascontiguousarray(W[:, cols])
    consts = np.zeros((128, 16), np.float32)
    cw = z['ml_conv_w'][j]
    cb = z['ml_conv_b'][j]
    consts[:, 0:4] = cw[:, hA * 64:hA * 64 + 128].T
    consts[:, 4:8] = cw[:, 512 + hA * 64:512 + hA * 64 + 128].T
    consts[:, 8] = cb[hA * 64:hA * 64 + 128]
    consts[:, 9] = cb[512 + hA * 64:512 + hA * 64 + 128]
    on = z['ml_out_norm'][j]
    consts[:, 10] = on[hA * 128:(hA + 1) * 128]
    consts[:, 11] = on[hB * 128:(hB + 1) * 128]
    bif = z['ml_b_if'][j]
    consts[0:2, 12] = bif[[hA, hB]]
    consts[0:2, 13] = -bif[[8 + hA, 8 + hB]]
    cm = np.zeros((128, 256), np.float32)
    cm[:, 0:128] = np.eye(128)
    s = np.arange(128)[:, None]
    t = np.arange(128)[None, :]
    cm[:, 128:256] = (s <= t)
    sel = np.zeros((2, 386), np.float32)
    sel[0, 0:128] = 1
    sel[1, 128:256] = 1
    sel[0, 256:320] = 1
    sel[1, 320:384] = 1
    sel[0, 384] = 1
    sel[1, 385] = 1
    return {'h_in': hT_b, 'w': w_c, 'consts': consts, 'cm': cm.astype(_mld.bfloat16), 'sel': sel}


def _rwk_consts():
    cf = np.zeros((128, 1025), np.float32)
    s = np.arange(64)[:, None]
    t = np.arange(64)[None, :]
    su = (s < t).astype(np.float32)
    ui = (s <= t).astype(np.float32)
    m = np.concatenate([su, ui, su, ui, su.T], axis=1)
    cf[:, 0:320] = np.concatenate([m, m], axis=0)
    cf[:, 320:384] = np.concatenate([np.eye(64), np.eye(64)], axis=0)
    cf[:, 384] = 1.0
    bo = np.zeros((128, 128), np.float32)
    bo[:64, :64] = 1
    bo[64:, 64:] = 1
    cf[:, 385:513] = bo
    keep = np.ones(512, np.float32)
    keep[::64] = 0
    cf[:, 513:1025] = keep[None, :]
    return cf


def _rwk_inputs(z, j, hT_b, c):
    q = c % 4
    ch = slice(q * 256, (q + 1) * 256)
    wbig = np.concatenate([z['rw_w_r'][j][:, ch], z['rw_w_k'][j][:, ch], z['rw_w_v'][j][:, ch],
                           z['rw_w1'][j], z['rw_a1'][j], z['rw_g1'][j]], axis=1)
    w2c = np.concatenate([z['rw_w2'][j][:, ch], z['rw_a2'][j][:, ch]], axis=1)
    g2c = z['rw_g2'][j][:, ch]
    mu = z['rw_mu'][j]
    mu_l = np.ascontiguousarray(mu.reshape(6, 8, 128).transpose(2, 0, 1).reshape(128, 48))
    cvec = np.zeros((128, 16), np.float32)
    rk = z['rw_r_k'][j].reshape(-1)
    one = np.ones(128, np.float32)
    for p in range(2):
        cc = slice(q * 256 + p * 128, q * 256 + (p + 1) * 128)
        cvec[:, 8 * p + 0] = z['rw_w0'][j][cc]
        cvec[:, 8 * p + 1] = z['rw_a0'][j][cc]
        cvec[:, 8 * p + 2] = z['rw_k_k'][j][cc]
        cvec[:, 8 * p + 3] = z['rw_k_a'][j][cc]
        cvec[:, 8 * p + 4] = 1.0 - z['rw_k_a'][j][cc]
        cvec[:, 8 * p + 5] = rk[cc]
    lnwb = np.zeros((128, 256), np.float32)
    for p in range(2):
        for h in range(2):
            cc = slice(q * 256 + p * 128 + h * 64, q * 256 + p * 128 + (h + 1) * 64)
            lnwb[h * 64:(h + 1) * 64, p * 128:p * 128 + 64] = z['rw_ln_w'][j][cc][None, :]
            lnwb[h * 64:(h + 1) * 64, p * 128 + 64:p * 128 + 128] = z['rw_ln_b'][j][cc][None, :]
    return {'h_in': hT_b, 'wbig': np.ascontiguousarray(wbig), 'w2c': np.ascontiguousarray(w2c),
            'g2c': np.ascontiguousarray(g2c), 'mu': mu_l, 'cvec': cvec, 'lnwb': lnwb, 'cf32': _rwk_consts()}


def _build_fused(nl=4):
    nc = bass.Bass("TRN2", target_bir_lowering=False)
    C = Ctx(nc)
    P = C.P
    dt = nc.dram_tensor
    RG = [[0, 1, 2, 3], [4, 5, 6, 7]]
    U32 = mybir.dt.uint32
    fm = lambda ap: ap.rearrange("(c p) t -> p c t", p=128)
    x_ext = fm(dt("x_in", [1024, 2048], F32, kind="ExternalInput").ap())
    y_ext = fm(dt("y_out", [1024, 2048], F32, kind="ExternalOutput").ap())
    idx_in = dt("idx", [128, 8], U32, kind="ExternalInput").ap()
    xbuf = fm(dt("xbuf", [1024, 2048], F32).ap())
    hbuf_t = dt("hbuf", [1024, 2048], BF16).ap()
    hall_t = dt("hall", [4096, 2048], BF16).ap()
    opart_t = dt("opart", [256, 8192], BF16).ap()
    oall_t = dt("oall", [1024, 8192], BF16).ap()
    hall_v = hall_t.rearrange("(k q c p) f -> q p k c f", k=4, q=4, p=128)
    oall_rows = oall_t.rearrange("r (q f) -> (r q) f", q=4)

    def hsrc(dst, t, key, wn):
        for k in range(4):
            P.dma("sp", dst[:, 2 * k:2 * k + 2, :], hall_v[t // 4][:, k, :, (t % 4) * 512:(t % 4 + 1) * 512], key, w=wn)

    def ffw_decl(l, i):
        wv = lambda ap: ap.rearrange("(c p) f -> p c f", p=128)
        return (wv(dt(f"wg_{l}_{i}", [1024, 2816], F32, kind="ExternalInput").ap()),
                wv(dt(f"wu_{l}_{i}", [1024, 2816], F32, kind="ExternalInput").ap()),
                wv(dt(f"wd_{l}_{i}", [2816, 1024], F32, kind="ExternalInput").ap()))

    def gather_h():
        P.barrier()
        for k in range(4):
            P.op("pool", lambda e: e.collective_compute("AllGather", ALU.bypass, replica_groups=RG,
                                                         ins=[hbuf_t[k * 256:(k + 1) * 256, :].opt()],
                                                         outs=[hall_t[k * 1024:(k + 1) * 1024, :].opt()]),
                 r=[], w=[f"hall{k}"], dma="cc", inc=1)
        P.barrier()
        C.reset()

    def gather_o():
        P.barrier()
        for k in range(4):
            P.op("pool", lambda e: e.collective_compute("AllGather", ALU.bypass, replica_groups=RG,
                                                         ins=[opart_t[k * 64:(k + 1) * 64, :].opt()],
                                                         outs=[oall_t[k * 256:(k + 1) * 256, :].opt()]),
                 r=[], w=[f"oall{k}"], dma="cc", inc=1)
        P.barrier()
        C.reset()

    g0 = dt("gains_a", [128, 16], F32, kind="ExternalInput").ap()
    emit_tp(C, {"x_in": x_ext, "ffw": [ffw_decl(0, 0)], "gains": g0, "x_out": xbuf, "h_out": fm(hbuf_t)}, 1, False, True, False)
    gather_h()
    for l in range(nl):
        kind = l % 3
        wv = lambda ap: ap.rearrange("(c p) f -> p c f", p=128)
        if kind == 0:
            io = {"hsrc": hsrc, "pos": dt(f"m{l}_pos", [1, 8192], I32, kind="ExternalInput").ap(),
                  "wa": wv(dt(f"m{l}_wa", [1024, 896], F32, kind="ExternalInput").ap()),
                  "wqb": wv(dt(f"m{l}_wqb", [512, 512], F32, kind="ExternalInput").ap()),
                  "wkvb": wv(dt(f"m{l}_wkvb", [256, 512], F32, kind="ExternalInput").ap()),
                  "consts": dt(f"m{l}_consts", [128, 8], F32, kind="ExternalInput").ap(),
                  "masks": dt(f"m{l}_masks", [128, 4, 512], BF16, kind="ExternalInput").ap(),
                  "o_out": opart_t}
            emit_mla(C, io)
        elif kind == 1:
            io = {"hsrc": hsrc, "w": wv(dt(f"m{l}_w", [1024, 772], F32, kind="ExternalInput").ap()),
                  "consts": dt(f"m{l}_consts", [128, 16], F32, kind="ExternalInput").ap(),
                  "cm": dt(f"m{l}_cm", [128, 256], BF16, kind="ExternalInput").ap(),
                  "sel": dt(f"m{l}_sel", [2, 386], F32, kind="ExternalInput").ap(),
                  "o_out": opart_t}
            emit_mls(C, io)
        else:
            io = {"hsrc": hsrc, "wbig": wv(dt(f"m{l}_wbig", [1024, 1024], F32, kind="ExternalInput").ap()),
                  "w2c": dt(f"m{l}_w2c", [64, 512], F32, kind="ExternalInput").ap(),
                  "g2c": dt(f"m{l}_g2c", [128, 256], F32, kind="ExternalInput").ap(),
                  "mu": dt(f"m{l}_mu", [128, 48], F32, kind="ExternalInput").ap(),
                  "cvec": dt(f"m{l}_cvec", [128, 16], F32, kind="ExternalInput").ap(),
                  "lnwb": dt(f"m{l}_lnwb", [128, 256], F32, kind="ExternalInput").ap(),
                  "cf32": dt(f"m{l}_cf32", [128, 1025], F32, kind="ExternalInput").ap(),
                  "o_out": opart_t}
            emit_rwk(C, io)
        gather_o()
        wo = wv(dt(f"wo_{l}", [1024, 1024], F32, kind="ExternalInput").ap())
        if l < nl - 1:
            gk = dt(f"gains_{l}", [128, 24], F32, kind="ExternalInput").ap()
            emit_tp(C, {"x_in": xbuf, "oall": oall_rows, "wo": wo, "idx": idx_in, "ffw": [ffw_decl(l, 1), ffw_decl(l + 1, 0)],
                        "gains": gk, "x_out": xbuf, "h_out": fm(hbuf_t)}, 2, True, True, False)
            gather_h()
        else:
            gk = dt(f"gains_{l}", [128, 16], F32, kind="ExternalInput").ap()
            emit_tp(C, {"x_in": xbuf, "oall": oall_rows, "wo": wo, "idx": idx_in, "ffw": [ffw_decl(l, 1)],
                        "gains": gk, "y_out": y_ext}, 1, True, False, True)
    P.emit()
    return nc


def kernel(nl=4, **z):
    z = {k: np.asarray(v) for k, v in z.items()}
    cores = list(range(8))
    x = z['x'].astype(np.float32)
    nc = _prog(('fused', nl), lambda: _build_fused(nl))
    ims = []
    pp = np.arange(128)[:, None]
    cc = np.arange(8)[None, :]
    for c in cores:
        b, q = c // 4, c % 4
        m = {'x_in': np.ascontiguousarray(x[b, q * 2048:(q + 1) * 2048, :].T),
             'idx': (((((cc * 128 + pp) % 256) // 64) * 256 + ((cc * 128 + pp) // 256) * 64 + (cc * 128 + pp) % 64) * 4 + q).astype(np.uint32)}
        for l in range(nl):
            for i in range(2):
                if l == nl - 1 and i == 1 and False:
                    continue
                m[f'wg_{l}_{i}'] = z['ffn_w_gate'][l, i]
                m[f'wu_{l}_{i}'] = z['ffn_w_up'][l, i]
                m[f'wd_{l}_{i}'] = z['ffn_w_down'][l, i]
        m['gains_a'] = np.concatenate([_gl(z['ffn_norm'][0, 0]), _gl(z['mix_norm'][0])], axis=1)
        dummy_h = None
        for l in range(nl):
            kind, j = l % 3, l // 3
            if kind == 0:
                d = _mla_inputs(z, j, dummy_h, z['positions'][b], c)
                wo = z['mla_w_o'][j]
            elif kind == 1:
                d = _mls_inputs(z, j, dummy_h, c)
                wo = z['ml_w_o'][j]
            else:
                d = _rwk_inputs(z, j, dummy_h, c)
                wo = z['rw_w_o'][j]
            for k, v in d.items():
                if k != 'h_in':
                    m[f'm{l}_{k}'] = v
            m[f'wo_{l}'] = np.ascontiguousarray(wo)
            if l < nl - 1:
                m[f'gains_{l}'] = np.concatenate([_gl(z['ffn_norm'][l, 1]), _gl(z['ffn_norm'][l + 1, 0]),
                                                  _gl(z['mix_norm'][l + 1])], axis=1)
            else:
                m[f'gains_{l}'] = np.concatenate([_gl(z['ffn_norm'][l, 1]), _gl(z['final_norm'])], axis=1)
        ims.append(m)
    res = run_bass_kernel_spmd(nc, ims, core_ids=cores)
    out = np.zeros((2, 8192, 1024), np.float32)
    for c in cores:
        out[c // 4, (c % 4) * 2048:(c % 4 + 1) * 2048, :] = res.results[c]['y_out'].T
    return out
```

```python
import math
from concourse.bass_utils import run_bass_kernel_spmd
import numpy as np
import concourse.bass as bass
import concourse.mybir as mybir

F32 = mybir.dt.float32
BF16 = mybir.dt.bfloat16
I32 = mybir.dt.int32
ALU = mybir.AluOpType
AF = mybir.ActivationFunctionType
AX = mybir.AxisListType

SAME_ENG_SYNC = True


class _Op:
    __slots__ = ("eng", "fn", "deps", "dma", "marked", "val", "sem", "inc", "phase")


class _Rec:
    def __init__(self):
        self.calls = []

    def __getattr__(self, name):
        def f(*a, **k):
            self.calls.append((name, a, k))
            return self
        return f


def _replay(calls):
    def fn(eng):
        ins = None
        for name, a, k in calls:
            ins = getattr(eng, name)(*a, **k)
        return ins
    return fn


class Prog:
    ENG = ("pe", "act", "dve", "pool", "sp")

    def __init__(self, nc):
        self.nc = nc
        self.ops = {e: [] for e in self.ENG}
        self.res_w = {}
        self.res_r = {}
        self.out_dmas = []
        self.all = []
        self.phase = 0
        self.bar_deps = []
        self.passed = set()
        self.last_compute = {}
        self.last_dma = {}

    def op(self, eng, fn, r=(), w=(), dma=None, out=False, inc=None):
        o = _Op()
        o.eng = eng
        rec = _Rec()
        fn(rec)
        o.fn = _replay(rec.calls)
        o.dma = dma
        o.inc = inc if inc is not None else (16 if dma is not None else 1)
        o.marked = dma is not None
        o.val = 0
        o.sem = None
        deps = []
        for x in r:
            d = self.res_w.get(x)
            if d is not None:
                deps.append((d, 0))
        for x in w:
            d = self.res_w.get(x)
            if d is not None:
                deps.append((d, 0))
            for d in self.res_r.get(x, ()):
                deps.append((d, 1))
        for x in w:
            self.res_w[x] = o
            self.res_r[x] = []
        for x in r:
            if x not in w:
                self.res_r.setdefault(x, []).append(o)
        if eng not in self.passed:
            self.passed.add(eng)
            deps.extend((d, 0) for d in self.bar_deps)
        o.deps = deps
        o.phase = self.phase
        if dma is not None:
            self.last_dma[dma] = o
        else:
            self.last_compute[eng] = o
        self.ops[eng].append(o)
        self.all.append(o)
        if out:
            self.out_dmas.append(o)
        return o

    def barrier(self):
        self.bar_deps = list(self.last_compute.values()) + list(self.last_dma.values())
        self.passed = set()
        self.res_w = {}
        self.res_r = {}
        self.phase += 1

    def pe(self, fn, r=(), w=()):
        return self.op("pe", fn, r, w)

    def act(self, fn, r=(), w=()):
        return self.op("act", fn, r, w)

    def dve(self, fn, r=(), w=()):
        return self.op("dve", fn, r, w)

    def pool(self, fn, r=(), w=()):
        return self.op("pool", fn, r, w)

    def dma(self, q, out_ap, in_ap, key, r=(), w=(), out=False, **kw):
        return self.op(q, lambda e: e.dma_start(out=out_ap, in_=in_ap, **kw), r, w, dma=key, out=out)

    def _need(self, o, d, kind):
        if d is o:
            return False
        if d.dma is not None:
            return True
        if d.eng != o.eng:
            return True
        if o.eng == "pe":
            return False
        if kind == 1:
            return False
        return SAME_ENG_SYNC

    def emit(self):
        nc = self.nc
        fin = _Op()
        fin.eng = "sp"; fin.fn = None; fin.dma = None; fin.marked = False; fin.val = 0; fin.sem = None; fin.inc = 1
        fin.deps = [(d, 0) for d in self.out_dmas] + [(d, 0) for d in self.last_dma.values()]
        fin.phase = self.phase
        self.ops["sp"].append(fin)
        for e in self.ENG:
            for o in self.ops[e]:
                o.deps = [(d, k) for (d, k) in o.deps if self._need(o, d, k)]
                for d, k in o.deps:
                    d.marked = True
        import contextlib
        stack = contextlib.ExitStack()
        esem = {}
        dsem = {}
        dcnt = {}
        cnt = {}
        for e in ("all",):
            for o in self.all:
                e = o.eng
                if o.dma is not None:
                    if o.dma not in dsem:
                        dsem[o.dma] = stack.enter_context(nc.semaphore("d_" + o.dma))
                        dcnt[o.dma] = 0
                    dcnt[o.dma] += o.inc
                    o.sem = dsem[o.dma]
                    o.val = dcnt[o.dma]
                elif o.marked:
                    ek = (e, o.phase // 4)
                    if ek not in esem:
                        esem[ek] = stack.enter_context(nc.semaphore("s_%s_%d" % ek))
                        cnt[ek] = 0
                    cnt[ek] += 1
                    o.sem = esem[ek]
                    o.val = cnt[ek]
        self.nsem = len(dsem) + len(esem)
        engobj = {"pe": "tensor", "act": "scalar", "dve": "vector", "pool": "gpsimd", "sp": "sync"}
        ops = self.ops

        def run(ename, eng):
            seen = {}
            for o in ops[ename]:
                waits = {}
                for d, k in o.deps:
                    key = d.sem.name
                    if seen.get(key, 0) < d.val:
                        seen[key] = d.val
                        waits[key] = (d.sem, d.val)
                for key, (s, v) in waits.items():
                    eng.wait_ge(s, v)
                if o.fn is None:
                    continue
                ins = o.fn(eng)
                if o.marked:
                    ins.then_inc(o.sem, o.inc)

        with nc.Block() as block:
            @block.tensor
            def _(e):
                run("pe", e)

            @block.scalar
            def _(e):
                run("act", e)

            @block.vector
            def _(e):
                run("dve", e)

            @block.gpsimd
            def _(e):
                run("pool", e)

            @block.sync
            def _(e):
                run("sp", e)
        stack.close()


class _T:
    def __init__(self, ap):
        self._ap = ap

    def ap(self):
        return self._ap


class Ctx:
    def __init__(self, nc, arena_f32=51000):
        self.nc = nc
        self.P = Prog(nc)
        self.big = nc.alloc_sbuf_tensor("arena", [128, arena_f32], F32).ap()
        self.cap = arena_f32
        self.off = 0
        self.ps = [nc.alloc_psum_tensor("ps%d" % i, [128, 512], F32).ap() for i in range(8)]

    def reset(self):
        self.off = 0

    def sb(self, name, shape, dtype):
        p = shape[0]
        n = 1
        for d in shape[1:]:
            n *= d
        esz = 4 if dtype in (F32, I32, mybir.dt.uint32) else 2
        n4 = (n * esz + 3) // 4
        n4 = (n4 + 7) // 8 * 8
        assert self.off + n4 <= self.cap, ("SBUF arena overflow", name, self.off, n4, self.cap)
        ap = self.big[0:p, self.off:self.off + n4]
        self.off += n4
        if dtype != F32:
            ap = ap.bitcast(dtype)
        ap = ap[:, 0:n]
        if len(shape) == 3:
            ap = ap.rearrange("p (a b) -> p a b", a=shape[1])
        elif len(shape) != 2:
            raise ValueError(shape)
        return _T(ap)


D = 1024
FF = 2816
T = 2048
TT = 512
NTT = T // TT
FG = 256
NFG = FF // FG
EPS = 1e-6


def emit_tp(CX, io, n_ffn, do_wo, do_mix, do_final):
    nc = CX.nc
    P = CX.P
    x_in = io["x_in"]
    if do_wo:
        oall = io["oall"]
        wo_in = io["wo"]
        idx_in = io["idx"]
    ffw = io["ffw"]
    n_gain = n_ffn + (1 if (do_mix or do_final) else 0)
    g_in = io["gains"]
    if do_final:
        y_out = io["y_out"]
    else:
        x_out = io["x_out"]
    if do_mix:
        h_out = io["h_out"]

    sb = CX.sb
    x = sb("x", [128, 8, T], F32).ap()
    h = sb("h", [128, 8, T], BF16).ap()
    gains = sb("gains_sb", [128, max(n_gain, 1) * 8], F32).ap()
    ones = sb("ones", [128, 128], BF16).ap()
    sq = sb("sq", [128, 8, TT], BF16).ap()
    sd = sb("sd", [128, TT], F32).ap()
    rstd = sb("rstd", [128, TT], F32).ap()
    wst = [[sb(f"wst{i}_{j}", [128, 8 * FG], F32).ap() for j in range(3)] for i in range(2)]
    wbf = [[sb(f"wbf{i}_{j}", [128, 8 * FG], BF16).ap() for j in range(3)] for i in range(2)]
    a_sb = [sb(f"a{i}", [128, TT], F32).ap() for i in range(2)]
    z = [sb(f"z{i}", [128, 2, TT], BF16).ap() for i in range(2)]
    ps = CX.ps
    idx = sb("idx", [128, 8], mybir.dt.uint32).ap()

    def XR(tt):
        return [f"x{tt}_{dc}" for dc in range(8)]

    P.pool(lambda e: e.memset(ones, 1.0), w=["ones"])
    P.dma("sp", gains, g_in, "gains", w=["gains"])
    for tt in range(NTT):
        P.dma("sp", x[:, :, tt * TT:(tt + 1) * TT], x_in[:, :, tt * TT:(tt + 1) * TT], f"x{tt}", w=XR(tt))

    def tsl(tt):
        return slice(tt * TT, (tt + 1) * TT)

    def norm_stats(tt):
        P.act(lambda e: e.activation(out=sq, in_=x[:, :, tsl(tt)], func=AF.Square), r=XR(tt), w=["sq"])
        for dc in range(8):
            P.pe(lambda e, dc=dc: e.matmul(ps[6], lhsT=ones, rhs=sq[:, dc, :], start=(dc == 0), stop=(dc == 7)),
                 r=["ones", "sq"], w=["ps6"])
        P.act(lambda e: e.activation(out=sd, in_=ps[6], func=AF.Sqrt, bias=EPS, scale=1.0 / D), r=["ps6"], w=["sd"])
        P.dve(lambda e: e.reciprocal(out=rstd, in_=sd), r=["sd"], w=["rstd"])

    def norm_to_h(tt, gi):
        norm_stats(tt)
        for dc in range(8):
            P.dve(lambda e, dc=dc: e.scalar_tensor_tensor(
                out=h[:, dc, tsl(tt)], in0=x[:, dc, tsl(tt)], scalar=gains[:, gi * 8 + dc:gi * 8 + dc + 1],
                in1=rstd, op0=ALU.mult, op1=ALU.mult),
                r=[f"x{tt}_{dc}", "gains", "rstd"], w=[f"h{tt}"])

    if do_wo:
        P.dma("sp", idx, idx_in, "idx", w=["idx"])
        for c in range(8):
            P.op("pool", lambda e: e.indirect_dma_start(out=h[:, c, :], out_offset=None, in_=oall,
                                                         in_offset=bass.IndirectOffsetOnAxis(ap=idx[:, c:c + 1], axis=0)),
                 r=["idx"], w=[f"h{tt}" for tt in range(NTT)], dma="ogat")
        for g in range(4):
            b = g % 2
            P.dma("sp", wst[b][0].rearrange("p (c f) -> p c f", c=8), wo_in[:, :, g * FG:(g + 1) * FG],
                  f"wst{b}0", w=[f"wst{b}0"])
            P.pool(lambda e, b=b: e.tensor_copy(out=wbf[b][0], in_=wst[b][0]), r=[f"wst{b}0"], w=[f"wbf{b}0"])
            wv = wbf[b][0].rearrange("p (c f) -> p c f", c=8)
            for tt in range(NTT):
                for j in range(2):
                    dc_out = g * 2 + j
                    pb = (tt * 2 + j) % 2
                    for kc in range(8):
                        P.pe(lambda e, kc=kc, j=j, tt=tt, pb=pb, wv=wv: e.matmul(
                            ps[4 + pb], lhsT=wv[:, kc, j * 128:(j + 1) * 128], rhs=h[:, kc, tsl(tt)],
                            start=(kc == 0), stop=(kc == 7)),
                            r=[f"wbf{b}0", f"h{tt}"], w=[f"ps{4 + pb}"])
                    P.dve(lambda e, dc_out=dc_out, tt=tt, pb=pb: e.tensor_tensor(
                        out=x[:, dc_out, tsl(tt)], in0=ps[4 + pb], in1=x[:, dc_out, tsl(tt)], op=ALU.add),
                        r=[f"ps{4 + pb}", f"x{tt}_{dc_out}"], w=[f"x{tt}_{dc_out}"])

    pend = [None]
    for s in range(n_ffn):
        wg, wu, wd = ffw[s]
        for tt in range(NTT):
            norm_to_h(tt, s)
        for g in range(NFG):
            b = g % 2
            fs = slice(g * FG, (g + 1) * FG)
            P.dma("sp", wst[b][0].rearrange("p (c f) -> p c f", c=8), wg[:, :, fs], f"wst{b}0", w=[f"wst{b}0"])
            P.dma("sp", wst[b][1].rearrange("p (c f) -> p c f", c=8), wu[:, :, fs], f"wst{b}1", w=[f"wst{b}1"])
            P.dma("sp", wst[b][2].rearrange("p (c f) -> p c f", c=2), wd[:, 2 * g:2 * g + 2, :], f"wst{b}2",
                  w=[f"wst{b}2"])
            for j in range(3):
                P.pool(lambda e, b=b, j=j: e.tensor_copy(out=wbf[b][j], in_=wst[b][j]),
                       r=[f"wst{b}{j}"], w=[f"wbf{b}{j}"])
            wgv = wbf[b][0].rearrange("p (c f) -> p c f", c=8)
            wuv = wbf[b][1].rearrange("p (c f) -> p c f", c=8)
            wdv = wbf[b][2].rearrange("p (c f) -> p c f", c=2)
            for tt in range(NTT):
                zb = tt % 2
                for fc in range(2):
                    pg = 2 * fc
                    pu = 2 * fc + 1
                    for dc in range(8):
                        P.pe(lambda e, dc=dc, fc=fc, tt=tt, pg=pg, wgv=wgv: e.matmul(
                            ps[pg], lhsT=wgv[:, dc, fc * 128:(fc + 1) * 128], rhs=h[:, dc, tsl(tt)],
                            start=(dc == 0), stop=(dc == 7)),
                            r=[f"wbf{b}0", f"h{tt}"], w=[f"ps{pg}"])
                    for dc in range(8):
                        P.pe(lambda e, dc=dc, fc=fc, tt=tt, pu=pu, wuv=wuv: e.matmul(
                            ps[pu], lhsT=wuv[:, dc, fc * 128:(fc + 1) * 128], rhs=h[:, dc, tsl(tt)],
                            start=(dc == 0), stop=(dc == 7)),
                            r=[f"wbf{b}1", f"h{tt}"], w=[f"ps{pu}"])
                    P.act(lambda e, fc=fc, pg=pg: e.activation(out=a_sb[fc], in_=ps[pg], func=AF.Silu),
                          r=[f"ps{pg}"], w=[f"a{fc}"])
                    P.dve(lambda e, fc=fc, pu=pu, zb=zb: e.tensor_tensor(
                        out=z[zb][:, fc, :], in0=a_sb[fc], in1=ps[pu], op=ALU.mult),
                        r=[f"a{fc}", f"ps{pu}"], w=[f"z{zb}"])
                if pend[0] is not None:
                    pend[0]()

                def down(tt=tt, zb=zb, wdv=wdv, b=b):
                    for dc in range(8):
                        pb = 4 + dc % 2
                        for fc in range(2):
                            P.pe(lambda e: e.matmul(
                                ps[pb], lhsT=wdv[:, fc, dc * 128:(dc + 1) * 128], rhs=z[zb][:, fc, :],
                                start=(fc == 0), stop=(fc == 1)),
                                r=[f"wbf{b}2", f"z{zb}"], w=[f"ps{pb}"])
                        P.dve(lambda e: e.scalar_tensor_tensor(
                            out=x[:, dc, tsl(tt)], in0=ps[pb], scalar=0.5, in1=x[:, dc, tsl(tt)],
                            op0=ALU.mult, op1=ALU.add),
                            r=[f"ps{pb}", f"x{tt}_{dc}"], w=[f"x{tt}_{dc}"])
                pend[0] = down
        pend[0]()
        pend[0] = None

    if do_mix:
        for tt in range(NTT):
            norm_to_h(tt, n_ffn)
            P.dma("sp", h_out[:, :, tsl(tt)], h[:, :, tsl(tt)], "hout", r=[f"h{tt}"])
    if do_final:
        for tt in range(NTT):
            norm_stats(tt)
            for dc in range(8):
                P.dve(lambda e, dc=dc, tt=tt: e.scalar_tensor_tensor(
                    out=x[:, dc, tsl(tt)], in0=x[:, dc, tsl(tt)],
                    scalar=gains[:, n_ffn * 8 + dc:n_ffn * 8 + dc + 1],
                    in1=rstd, op0=ALU.mult, op1=ALU.mult),
                    r=[f"x{tt}_{dc}", "gains", "rstd"], w=[f"x{tt}_{dc}"])
            P.dma("sp", y_out[:, :, tsl(tt)], x[:, :, tsl(tt)], "xout", r=XR(tt), out=True)
    else:
        for tt in range(NTT):
            P.dma("sp", x_out[:, :, tsl(tt)], x[:, :, tsl(tt)], "xout", r=XR(tt))

import math

D = 1024
S = 8192
TT = 512
NT = S // TT
QR, KVR, ROPE, NOPE, DV = 512, 256, 64, 128, 128
LATC = 896
EPS = 1e-6
SCALE = (NOPE + ROPE) ** -0.5


def emit_mla(CX, io):
    nc = CX.nc
    P = CX.P
    hsrc = io["hsrc"]
    pos_in = io["pos"]
    wa_in = io["wa"]
    wqb_in = io["wqb"]
    wkvb_in = io["wkvb"]
    c_in = io["consts"]
    masks_in = io["masks"]
    o_out = io["o_out"]

    sb = CX.sb
    stage = sb("stage", [128, 4 * LATC], F32).ap()
    wa = sb("wa_sb", [128, 8, LATC], BF16).ap()
    wqb = sb("wqb_sb", [128, 4, 512], BF16).ap()
    wkvb = sb("wkvb_sb", [128, 2, 512], BF16).ap()
    consts = sb("consts_sb", [128, 8], F32).ap()
    masks = sb("masks_sb", [128, 4, 512], BF16).ap()
    ones = sb("ones", [128, 128], BF16).ap()
    hT = [sb(f"hT{i}", [128, 8, TT], BF16).ap() for i in range(2)]
    lat = sb("lat", [128, 6, TT], F32).ap()
    sq = sb("sq", [128, 6, TT], BF16).ap()
    sd = sb("sd", [128, TT], F32).ap()
    rq = sb("rq", [128, TT], F32).ap()
    rkv = sb("rkv", [128, TT], F32).ap()
    cqn = sb("cqn", [128, 4, TT], BF16).ap()
    ckvn = sb("ckvn", [128, 2, TT], BF16).ap()
    posi = sb("posi", [64, TT], I32).ap()
    ui = sb("ui", [64, TT], I32).ap()
    u = sb("u", [64, TT], F32).ap()
    uc = sb("uc", [64, TT], F32).ap()
    wr = sb("wr", [64, TT], F32).ap()
    cos2 = sb("cos2", [64, TT], F32).ap()
    sin2 = sb("sin2", [64, TT], F32).ap()
    t1 = sb("t1", [64, TT], F32).ap()
    t2 = sb("t2", [64, TT], F32).ap()
    Kn = [sb(f"Kn{i}", [128, S], BF16).ap() for i in range(2)]
    Kr = sb("Kr", [64, S], BF16).ap()
    V = [sb(f"V{i}", [128, S // 128, DV], BF16).ap() for i in range(2)]
    qn = [sb(f"qn{i}", [128, TT], BF16).ap() for i in range(2)]
    qr = [sb(f"qr{i}", [64, TT], BF16).ap() for i in range(2)]
    pT = [sb(f"pT{i}", [128, TT], BF16).ap() for i in range(4)]
    rden = sb("rden", [128, TT], F32).ap()
    osb = [sb(f"osb{i}", [128, TT], BF16).ap() for i in range(2)]
    ps = CX.ps

    P.pool(lambda e: e.memset(ones, 1.0), w=["ones"])
    P.dma("sp", consts, c_in, "consts", w=["consts"])
    P.dma("sp", masks, masks_in, "masks", w=["masks"])
    for hf in range(2):
        P.dma("sp", stage.rearrange("p (c f) -> p c f", c=4), wa_in[:, 4 * hf:4 * hf + 4, :], "stage", w=["stage"])
        P.pool(lambda e, hf=hf: e.tensor_copy(out=wa[:, 4 * hf:4 * hf + 4, :].rearrange("p c f -> p (c f)"), in_=stage),
               r=["stage"], w=["wa"])
    P.dma("sp", stage[:, 0:2048].rearrange("p (c f) -> p c f", c=4), wqb_in, "stage", w=["stage"])
    P.pool(lambda e: e.tensor_copy(out=wqb.rearrange("p c f -> p (c f)"), in_=stage[:, 0:2048]), r=["stage"], w=["wqb"])
    P.dma("sp", stage[:, 0:1024].rearrange("p (c f) -> p c f", c=2), wkvb_in, "stage", w=["stage"])
    P.pool(lambda e: e.tensor_copy(out=wkvb.rearrange("p c f -> p (c f)"), in_=stage[:, 0:1024]), r=["stage"], w=["wkvb"])

    pcount = [0]

    def pbank():
        pcount[0] += 1
        return pcount[0] % 2

    def tsl(t):
        return slice(t * TT, (t + 1) * TT)

    def rope(src_ps, src_sw_ps, dst, rd, wt):
        P.dve(lambda e: e.tensor_tensor(out=t1, in0=src_ps, in1=cos2, op=ALU.mult), r=rd + ["cos2"], w=["t1"])
        P.dve(lambda e: e.tensor_tensor(out=t2, in0=src_sw_ps, in1=sin2, op=ALU.mult), r=rd + ["sin2"], w=["t2"])
        P.dve(lambda e: e.tensor_tensor(out=dst, in0=t1, in1=t2, op=ALU.add), r=["t1", "t2"], w=wt)

    for t in range(NT):
        hb = t % 2
        hsrc(hT[hb], t, f"hT{hb}", [f"hT{hb}"])
        P.dma("sp", posi, pos_in[:, tsl(t)].partition_broadcast(64), "posi", w=["posi"])
        P.dve(lambda e: e.tensor_copy(out=t1, in_=posi), r=["posi"], w=["t1"])
        P.dve(lambda e: e.tensor_scalar(out=u, in0=t1, scalar1=consts[0:64, 6:7], scalar2=None, op0=ALU.mult),
              r=["t1", "consts"], w=["u"])
        P.dve(lambda e: e.tensor_copy(out=ui, in_=u), r=["u"], w=["ui"])
        P.dve(lambda e: e.tensor_copy(out=t2, in_=ui), r=["ui"], w=["t2"])
        P.dve(lambda e: e.tensor_tensor(out=u, in0=u, in1=t2, op=ALU.subtract), r=["u", "t2"], w=["u"])
        P.dve(lambda e: e.tensor_single_scalar(out=wr, in_=u, scalar=0.5, op=ALU.is_gt), r=["u"], w=["wr"])
        P.dve(lambda e: e.tensor_tensor(out=u, in0=u, in1=wr, op=ALU.subtract), r=["u", "wr"], w=["u"])
        P.dve(lambda e: e.tensor_single_scalar(out=wr, in_=u, scalar=-0.5, op=ALU.is_lt), r=["u"], w=["wr"])
        P.dve(lambda e: e.tensor_tensor(out=u, in0=u, in1=wr, op=ALU.add), r=["u", "wr"], w=["u"])
        P.dve(lambda e: e.tensor_scalar_add(out=uc, in0=u, scalar1=0.25), r=["u"], w=["uc"])
        P.dve(lambda e: e.tensor_single_scalar(out=wr, in_=uc, scalar=0.5, op=ALU.is_gt), r=["uc"], w=["wr"])
        P.dve(lambda e: e.tensor_tensor(out=uc, in0=uc, in1=wr, op=ALU.subtract), r=["uc", "wr"], w=["uc"])
        P.act(lambda e: e.activation(out=sin2, in_=u, func=AF.Sin, scale=2 * math.pi), r=["u"], w=["sin2"])
        P.act(lambda e: e.activation(out=cos2, in_=uc, func=AF.Sin, scale=2 * math.pi), r=["uc"], w=["cos2"])
        P.dve(lambda e: e.tensor_scalar(out=sin2, in0=sin2, scalar1=consts[0:64, 7:8], scalar2=None, op0=ALU.mult),
              r=["sin2", "consts"], w=["sin2"])
        for c in range(6):
            pb = pbank()
            for dc in range(8):
                P.pe(lambda e, c=c, dc=dc, pb=pb: e.matmul(ps[pb], lhsT=wa[:, dc, c * 128:(c + 1) * 128],
                                                            rhs=hT[hb][:, dc, :], start=(dc == 0), stop=(dc == 7)),
                     r=["wa", f"hT{hb}"], w=[f"ps{pb}"])
            P.act(lambda e, c=c, pb=pb: e.copy(out=lat[:, c, :], in_=ps[pb]), r=[f"ps{pb}"], w=[f"lat{c}"])
        pbs = []
        for c2 in range(2):
            pb = pbank()
            pbs.append(pb)
            for dc in range(8):
                P.pe(lambda e, c2=c2, dc=dc, pb=pb: e.matmul(ps[pb][0:64, :], lhsT=wa[:, dc, 768 + 64 * c2:832 + 64 * c2],
                                                              rhs=hT[hb][:, dc, :], start=(dc == 0), stop=(dc == 7)),
                     r=["wa", f"hT{hb}"], w=[f"ps{pb}"])
        rope(ps[pbs[0]][0:64, :], ps[pbs[1]][0:64, :], Kr[:, tsl(t)], [f"ps{pbs[0]}", f"ps{pbs[1]}"], [f"Kr{t}"])
        P.act(lambda e: e.activation(out=sq, in_=lat[:, 0:6, :], func=AF.Square),
              r=[f"lat{c}" for c in range(6)], w=["sq"])
        pb = pbank()
        for c in range(4):
            P.pe(lambda e, c=c, pb=pb: e.matmul(ps[pb], lhsT=ones, rhs=sq[:, c, :], start=(c == 0), stop=(c == 3)),
                 r=["ones", "sq"], w=[f"ps{pb}"])
        P.act(lambda e, pb=pb: e.activation(out=sd, in_=ps[pb], func=AF.Sqrt, bias=EPS, scale=1.0 / QR),
              r=[f"ps{pb}"], w=["sd"])
        P.dve(lambda e: e.reciprocal(out=rq, in_=sd), r=["sd"], w=["rq"])
        pb = pbank()
        for c in range(2):
            P.pe(lambda e, c=c, pb=pb: e.matmul(ps[pb], lhsT=ones, rhs=sq[:, 4 + c, :], start=(c == 0), stop=(c == 1)),
                 r=["ones", "sq"], w=[f"ps{pb}"])
        P.act(lambda e, pb=pb: e.activation(out=sd, in_=ps[pb], func=AF.Sqrt, bias=EPS, scale=1.0 / KVR),
              r=[f"ps{pb}"], w=["sd"])
        P.dve(lambda e: e.reciprocal(out=rkv, in_=sd), r=["sd"], w=["rkv"])
        for c in range(4):
            P.dve(lambda e, c=c: e.scalar_tensor_tensor(out=cqn[:, c, :], in0=lat[:, c, :], scalar=consts[:, c:c + 1],
                                                        in1=rq, op0=ALU.mult, op1=ALU.mult),
                  r=[f"lat{c}", "rq", "consts"], w=["cqn"])
        for c in range(2):
            P.dve(lambda e, c=c: e.scalar_tensor_tensor(out=ckvn[:, c, :], in0=lat[:, 4 + c, :],
                                                        scalar=consts[:, 4 + c:5 + c],
                                                        in1=rkv, op0=ALU.mult, op1=ALU.mult),
                  r=[f"lat{4 + c}", "rkv", "consts"], w=["ckvn"])
        for hh in range(2):
            qo = hh * 256
            pb = pbank()
            for c in range(4):
                P.pe(lambda e, c=c, pb=pb, qo=qo: e.matmul(ps[pb], lhsT=wqb[:, c, qo:qo + 128], rhs=cqn[:, c, :],
                                                            start=(c == 0), stop=(c == 3)),
                     r=["wqb", "cqn"], w=[f"ps{pb}"])
            P.act(lambda e, pb=pb, hh=hh: e.copy(out=qn[hh], in_=ps[pb]), r=[f"ps{pb}"], w=[f"qn{hh}"])
            pbs = []
            for c2 in range(2):
                pb = pbank()
                pbs.append(pb)
                for c in range(4):
                    P.pe(lambda e, c=c, c2=c2, pb=pb, qo=qo: e.matmul(
                        ps[pb][0:64, :], lhsT=wqb[:, c, qo + 128 + 64 * c2:qo + 192 + 64 * c2], rhs=cqn[:, c, :],
                        start=(c == 0), stop=(c == 3)),
                        r=["wqb", "cqn"], w=[f"ps{pb}"])
            rope(ps[pbs[0]][0:64, :], ps[pbs[1]][0:64, :], qr[hh], [f"ps{pbs[0]}", f"ps{pbs[1]}"], [f"qr{hh}"])
            ko = hh * 256
            pb = pbank()
            for c in range(2):
                P.pe(lambda e, c=c, pb=pb, ko=ko: e.matmul(ps[pb], lhsT=wkvb[:, c, ko:ko + 128], rhs=ckvn[:, c, :],
                                                            start=(c == 0), stop=(c == 1)),
                     r=["wkvb", "ckvn"], w=[f"ps{pb}"])
            P.act(lambda e, pb=pb, hh=hh, t=t: e.copy(out=Kn[hh][:, tsl(t)], in_=ps[pb]), r=[f"ps{pb}"],
                  w=[f"Kn{hh}_{t}"])
            pb = pbank()
            for j in range(4):
                for c in range(2):
                    P.pe(lambda e, c=c, j=j, pb=pb, ko=ko: e.matmul(
                        ps[pb][:, j * 128:(j + 1) * 128], lhsT=ckvn[:, c, j * 128:(j + 1) * 128],
                        rhs=wkvb[:, c, ko + 128:ko + 256], start=(c == 0), stop=(c == 1)),
                        r=["wkvb", "ckvn"], w=[f"ps{pb}"])
            P.act(lambda e, pb=pb, hh=hh, t=t: e.copy(
                out=V[hh][:, 4 * t:4 * t + 4, :], in_=ps[pb].rearrange("p (j d) -> p j d", j=4)),
                r=[f"ps{pb}"], w=[f"V{hh}_{t}"])
        SB = [2, 3, 6, 7]
        LA = 3
        for hh in range(2):
            ob = 4
            nkb = 4 * t + 4

            def qk(kb):
                sbk = SB[kb % 4]
                j = kb - 4 * t
                q0 = max(j, 0) * 128
                tk = kb // 4
                ksl = slice(kb * 128, (kb + 1) * 128)
                P.pe(lambda e: e.matmul(ps[sbk][:, q0:], lhsT=Kn[hh][:, ksl], rhs=qn[hh][:, q0:], start=True, stop=False),
                     r=[f"Kn{hh}_{tk}", f"qn{hh}"], w=[f"ps{sbk}"])
                P.pe(lambda e: e.matmul(ps[sbk][:, q0:], lhsT=Kr[:, ksl], rhs=qr[hh][:, q0:], start=False, stop=True),
                     r=[f"Kr{tk}", f"qr{hh}"], w=[f"ps{sbk}"])
                pi = kb % 4
                P.act(lambda e: e.activation(out=pT[pi][:, q0:], in_=ps[sbk][:, q0:], func=AF.Exp, scale=SCALE),
                      r=[f"ps{sbk}"], w=[f"pT{pi}"])
                if j >= 0:
                    P.pool(lambda e: e.tensor_tensor(out=pT[pi][:, q0:], in0=pT[pi][:, q0:], in1=masks[:, j, q0:], op=ALU.mult),
                           r=[f"pT{pi}", "masks"], w=[f"pT{pi}"])

            def pv(kb):
                j = kb - 4 * t
                q0 = max(j, 0) * 128
                tk = kb // 4
                pi = kb % 4
                P.pe(lambda e: e.matmul(ps[ob][:, q0:], lhsT=V[hh][:, kb, :], rhs=pT[pi][:, q0:], start=(kb == 0), stop=(kb == nkb - 1)),
                     r=[f"V{hh}_{tk}", f"pT{pi}"], w=[f"ps{ob}"])
                P.pe(lambda e: e.matmul(ps[ob + 1][:, q0:], lhsT=ones, rhs=pT[pi][:, q0:], start=(kb == 0), stop=(kb == nkb - 1)),
                     r=["ones", f"pT{pi}"], w=[f"ps{ob + 1}"])

            for i in range(nkb + LA):
                if i < nkb:
                    qk(i)
                if i - LA >= 0:
                    pv(i - LA)
            P.dve(lambda e: e.reciprocal(out=rden, in_=ps[ob + 1]), r=[f"ps{ob + 1}"], w=["rden"])
            P.dve(lambda e: e.tensor_tensor(out=osb[hh], in0=ps[ob], in1=rden, op=ALU.mult),
                  r=[f"ps{ob}", "rden"], w=[f"osb{hh}"])
            P.dma("sp", o_out[hh * DV:(hh + 1) * DV, tsl(t)], osb[hh], "oout", r=[f"osb{hh}"])


D = 1024
S = 8192
TT = 512
NT = S // TT
LM = 128
NCM = S // LM
EPS = 1e-6
WC = 772


def emit_mls(CX, io):
    nc = CX.nc
    P = CX.P
    hsrc = io["hsrc"]
    w_in = io["w"]
    c_in = io["consts"]
    cm_in = io["cm"]
    sel_in = io["sel"]
    o_out = io["o_out"]

    sb = CX.sb
    stage = sb("stage", [128, 4 * WC], F32).ap()
    w = sb("w_sb", [128, 8, WC], BF16).ap()
    consts = sb("consts_sb", [128, 16], F32).ap()
    cm = sb("cm_sb", [128, 256], BF16).ap()
    ident = cm[:, 0:128]
    tril = cm[:, 128:256]
    sel = sb("sel_sb", [2, 386], F32).ap()
    ones = sb("ones", [128, 128], BF16).ap()
    hT = [sb(f"hT{i}", [128, 8, TT], BF16).ap() for i in range(2)]
    gi = sb("gi", [2, S], F32).ap()
    gf = sb("gf", [2, S], F32).ap()
    mu = sb("mu", [2, S], F32).ap()
    Gcol = sb("Gcol", [128, NCM, 2], F32).ap()
    nmu = [sb(f"nmu{i}", [128, NCM + 1], F32).ap() for i in range(2)]
    pmuq = sb("pmuq", [128, NCM + 1], F32).ap()
    mub = [sb(f"mub{i}", [128, TT], F32).ap() for i in range(2)]
    muq = sb("muq", [128, TT], F32).ap()
    emt = [sb(f"emt{i}", [128, TT], F32).ap() for i in range(2)]
    xq = sb("xq", [128, 3 + TT], F32).ap()
    xk = sb("xk", [128, 3 + TT], F32).ap()
    cacc = sb("cacc", [128, TT], F32).ap()
    csil = sb("csil", [128, TT], F32).ap()
    qT = sb("qT", [128, TT], BF16).ap()
    qsT = sb("qsT", [128, TT], BF16).ap()
    kT = sb("kT", [128, TT], BF16).ap()
    ktok = sb("ktok", [128, 4, 128], F32).ap()
    kw = sb("kw", [128, 128], BF16).ap()
    vtok = [sb(f"vtok{i}", [128, 4, 128], BF16).ap() for i in range(2)]
    sigo = [sb(f"sigo{i}", [128, TT], F32).ap() for i in range(2)]
    Wt = sb("Wt", [128, 128], F32).ap()
    Wm = sb("Wm", [128, 128], F32).ap()
    pT = sb("pT", [128, 128], BF16).ap()
    rb = sb("rb", [128, TT], F32).ap()
    wcol = sb("wcol", [128, 2], F32).ap()
    carry = sb("carry", [128, 1], F32).ap()
    C = sb("C", [128, 128], F32).ap()
    Cb = sb("Cb", [128, 128], BF16).ap()
    Nb = sb("Nb", [128, 128], F32).ap()
    Nbb = sb("Nbb", [128, 128], BF16).ap()
    dn = sb("dn", [128, 128], F32).ap()
    hid = [sb(f"hid{i}", [128, TT], F32).ap() for i in range(2)]
    hsq = sb("hsq", [128, TT], BF16).ap()
    sd = sb("sd", [128, TT], F32).ap()
    rstd = sb("rstd", [128, TT], F32).ap()
    ho = [sb(f"ho{i}", [128, TT], BF16).ap() for i in range(2)]
    ps = CX.ps

    def tsl(t):
        return slice(t * TT, (t + 1) * TT)

    P.pool(lambda e: e.memset(ones, 1.0), w=["ones"])
    P.pool(lambda e: e.memset(C, 0.0), w=["C"])
    P.pool(lambda e: e.memset(Cb, 0.0), w=["Cb"])
    P.pool(lambda e: e.memset(Nb, 0.0), w=["Nb"])
    P.pool(lambda e: e.memset(Nbb, 0.0), w=["Nbb"])
    P.pool(lambda e: e.memset(xq[:, 0:3], 0.0), w=["xq"])
    P.pool(lambda e: e.memset(xk[:, 0:3], 0.0), w=["xk"])
    for i in range(2):
        P.pool(lambda e, i=i: e.memset(nmu[i][:, 0:1], 0.0), w=[f"nmu{i}"])
    P.pool(lambda e: e.memset(pmuq[:, 0:1], 0.0), w=["pmuq"])
    P.dma("sp", consts, c_in, "consts", w=["consts"])
    P.dma("sp", cm, cm_in, "cm", w=["cm"])
    P.dma("sp", sel, sel_in, "sel", w=["sel"])
    for hf in range(2):
        P.dma("sp", stage.rearrange("p (c f) -> p c f", c=4), w_in[:, 4 * hf:4 * hf + 4, :], "stage", w=["stage"])
        P.pool(lambda e: e.tensor_copy(out=w[:, 4 * hf:4 * hf + 4, :].rearrange("p c f -> p (c f)"), in_=stage),
               r=["stage"], w=["w"])

    for t in range(NT):
        hb = t % 2
        hsrc(hT[hb], t, f"hT{hb}", [f"hT{hb}"])
        for g2 in range(2):
            pb = g2
            for dc in range(8):
                P.pe(lambda e: e.matmul(ps[pb][0:2, :], lhsT=w[:, dc, 768 + 2 * g2:770 + 2 * g2], rhs=hT[hb][:, dc, :],
                                        start=(dc == 0), stop=(dc == 7)),
                     r=["w", f"hT{hb}"], w=[f"ps{pb}"])
        P.act(lambda e: e.activation(out=gi[:, tsl(t)], in_=ps[0][0:2, :], func=AF.Identity, bias=consts[0:2, 12:13]),
              r=["ps0", "consts"], w=["gi"])
        P.act(lambda e: e.activation(out=gf[:, tsl(t)], in_=ps[1][0:2, :], func=AF.Exp, bias=consts[0:2, 13:14], scale=-1.0),
              r=["ps1", "consts"], w=["gf"])
    P.act(lambda e: e.activation(out=gf, in_=gf, func=AF.Ln, bias=1.0, scale=1.0), r=["gf"], w=["gf"])
    P.dve(lambda e: e.tensor_scalar(out=gf, in0=gf, scalar1=-0.5, scalar2=None, op0=ALU.mult), r=["gf"], w=["gf"])
    P.dve(lambda e: e.tensor_tensor_scan(out=gf, data0=gf, data1=gf, initial=0.0, op0=ALU.add, op1=ALU.add),
          r=["gf"], w=["gf"])
    P.dve(lambda e: e.tensor_tensor(out=gi, in0=gi, in1=gf, op=ALU.subtract), r=["gi", "gf"], w=["gi"])
    P.dve(lambda e: e.tensor_tensor_scan(out=mu, data0=gi, data1=gi, initial=0.0, op0=ALU.max, op1=ALU.max),
          r=["gi"], w=["mu"])
    P.dve(lambda e: e.tensor_tensor(out=gf, in0=gf, in1=mu, op=ALU.add), r=["gf", "mu"], w=["gf"])
    fm = gf
    id2 = sel[:, 384:386]
    for c in range(NCM):
        P.pe(lambda e: e.matmul(ps[2][:, 2 * c:2 * c + 2], lhsT=gi[:, c * LM:(c + 1) * LM], rhs=id2, start=True, stop=True),
             r=["gi", "sel"], w=["ps2"])
    P.act(lambda e: e.copy(out=Gcol.rearrange("p c h -> p (c h)"), in_=ps[2][:, 0:2 * NCM]), r=["ps2"], w=["Gcol"])

    pcnt = [0]

    def pbank():
        pcnt[0] += 1
        return pcnt[0] % 2

    for t in range(NT):
        hb = t % 2
        hsrc(hT[hb], t, f"hT{hb}", [f"hT{hb}"])
        for hh in range(2):
            P.pe(lambda e: e.matmul(ps[2], lhsT=sel[:, hh * 128:(hh + 1) * 128], rhs=mu[:, tsl(t)], start=True, stop=True),
                 r=["sel", "mu"], w=["ps2"])
            P.act(lambda e: e.copy(out=mub[hh], in_=ps[2]), r=["ps2"], w=[f"mub{hh}"])
            P.act(lambda e: e.activation(out=nmu[hh][:, 4 * t + 1:4 * t + 5], in_=ps[2][:, LM - 1::LM], func=AF.Copy, scale=-1.0),
                  r=["ps2"], w=[f"nmu{hh}"])
            P.pe(lambda e: e.matmul(ps[2], lhsT=sel[:, hh * 128:(hh + 1) * 128], rhs=fm[:, tsl(t)], start=True, stop=True),
                 r=["sel", "gf"], w=["ps2"])
            P.act(lambda e: e.activation(out=emt[hh], in_=ps[2], func=AF.Exp, scale=-1.0), r=["ps2"], w=[f"emt{hh}"])
        P.pe(lambda e: e.matmul(ps[2], lhsT=sel[:, 256:384], rhs=mu[:, tsl(t)], start=True, stop=True),
             r=["sel", "mu"], w=["ps2"])
        P.act(lambda e: e.copy(out=muq, in_=ps[2]), r=["ps2"], w=["muq"])
        P.act(lambda e: e.copy(out=pmuq[:, 4 * t + 1:4 * t + 5], in_=ps[2][:, LM - 1::LM]), r=["ps2"], w=["pmuq"])
        for which, xbuf, dst in ((0, xq, qT), (1, xk, kT)):
            pb = pbank()
            xn = "xq" if which == 0 else "xk"
            for dc in range(8):
                P.pe(lambda e: e.matmul(ps[pb], lhsT=w[:, dc, which * 128:(which + 1) * 128], rhs=hT[hb][:, dc, :],
                                        start=(dc == 0), stop=(dc == 7)),
                     r=["w", f"hT{hb}"], w=[f"ps{pb}"])
            P.act(lambda e: e.copy(out=xbuf[:, 3:3 + TT], in_=ps[pb]), r=[f"ps{pb}"], w=[xn])
            cw = 4 * which
            P.dve(lambda e: e.tensor_scalar(out=cacc, in0=xbuf[:, 0:TT], scalar1=consts[:, cw:cw + 1],
                                            scalar2=consts[:, 8 + which:9 + which], op0=ALU.mult, op1=ALU.add),
                  r=[xn, "consts"], w=["cacc"])
            for j in range(1, 4):
                P.dve(lambda e: e.scalar_tensor_tensor(out=cacc, in0=xbuf[:, j:j + TT], scalar=consts[:, cw + j:cw + j + 1],
                                                       in1=cacc, op0=ALU.mult, op1=ALU.add),
                      r=[xn, "consts", "cacc"], w=["cacc"])
            P.pool(lambda e: e.tensor_copy(out=xbuf[:, 0:3], in_=xbuf[:, TT:TT + 3]), r=[xn, "cacc"], w=[xn])
            P.act(lambda e: e.activation(out=csil, in_=cacc, func=AF.Silu), r=["cacc"], w=["csil"])
            if which == 0:
                P.dve(lambda e: e.tensor_copy(out=dst, in_=csil), r=["csil"], w=["qT"])
            else:
                P.dve(lambda e: e.tensor_scalar(out=dst, in0=csil, scalar1=0.125, scalar2=None, op0=ALU.mult),
                      r=["csil"], w=["kT"])
        for hh in range(2):
            pb = pbank()
            for dc in range(8):
                P.pe(lambda e: e.matmul(ps[pb], lhsT=w[:, dc, 512 + hh * 128:640 + hh * 128], rhs=hT[hb][:, dc, :],
                                        start=(dc == 0), stop=(dc == 7)),
                     r=["w", f"hT{hb}"], w=[f"ps{pb}"])
            P.act(lambda e: e.activation(out=sigo[hh], in_=ps[pb], func=AF.Sigmoid), r=[f"ps{pb}"], w=[f"sigo{hh}"])
        for hh in range(2):
            pb = pbank()
            for j in range(4):
                for dc in range(8):
                    P.pe(lambda e: e.matmul(ps[pb][:, j * 128:(j + 1) * 128], lhsT=hT[hb][:, dc, j * 128:(j + 1) * 128],
                                            rhs=w[:, dc, 256 + hh * 128:384 + hh * 128], start=(dc == 0), stop=(dc == 7)),
                         r=["w", f"hT{hb}"], w=[f"ps{pb}"])
            P.act(lambda e: e.copy(out=vtok[hh].rearrange("p j d -> p (j d)"), in_=ps[pb]), r=[f"ps{pb}"], w=[f"vtok{hh}"])
        pb = pbank()
        for j in range(4):
            P.pe(lambda e: e.matmul(ps[pb][:, j * 128:(j + 1) * 128], lhsT=kT[:, j * 128:(j + 1) * 128], rhs=ident,
                                    start=True, stop=True),
                 r=["kT", "cm"], w=[f"ps{pb}"])
        P.act(lambda e: e.copy(out=ktok.rearrange("p j d -> p (j d)"), in_=ps[pb]), r=[f"ps{pb}"], w=["ktok"])
        for j in range(4):
            c = 4 * t + j
            P.act(lambda e: e.activation(out=rb[:, j * LM:(j + 1) * LM], in_=muq[:, j * LM:(j + 1) * LM], func=AF.Exp,
                                         bias=pmuq[:, c:c + 1], scale=-1.0),
                  r=["muq", "pmuq"], w=["rb"])
        P.dve(lambda e: e.tensor_tensor(out=qsT, in0=qT, in1=rb, op=ALU.mult), r=["qT", "rb"], w=["qsT"])
        for j in range(4):
            c = 4 * t + j
            cs = slice(j * LM, (j + 1) * LM)
            for hh in range(2):
                hp = slice(hh * 64, (hh + 1) * 64)
                P.pe(lambda e: e.matmul(ps[3][:, 0:128], lhsT=kT[hp, cs], rhs=qT[hp, cs], start=True, stop=True),
                     r=["kT", "qT"], w=["ps3"])
                P.act(lambda e: e.activation(out=Wt, in_=mub[hh][:, cs], func=AF.Exp, bias=Gcol[:, c, hh:hh + 1], scale=-1.0),
                      r=[f"mub{hh}", "Gcol"], w=["Wt"])
                P.pool(lambda e: e.tensor_tensor(out=Wm, in0=Wt, in1=tril, op=ALU.mult), r=["Wt", "cm"], w=["Wm"])
                P.dve(lambda e: e.tensor_tensor(out=pT, in0=ps[3][:, 0:128], in1=Wm, op=ALU.mult), r=["ps3", "Wm"], w=["pT"])
                P.pe(lambda e: e.matmul(ps[4][:, 0:128], lhsT=vtok[hh][:, j, :], rhs=pT, start=True, stop=False),
                     r=[f"vtok{hh}", "pT"], w=["ps4"])
                P.pe(lambda e: e.matmul(ps[4][:, 0:128], lhsT=Cb[hp, :], rhs=qsT[hp, cs], start=False, stop=True),
                     r=["Cb", "qsT"], w=["ps4"])
                P.pe(lambda e: e.matmul(ps[5][:, 0:128], lhsT=ones, rhs=pT, start=True, stop=False),
                     r=["ones", "pT"], w=["ps5"])
                P.pe(lambda e: e.matmul(ps[5][:, 0:128], lhsT=Nbb[hp, :], rhs=qsT[hp, cs], start=False, stop=True),
                     r=["Nbb", "qsT"], w=["ps5"])
                P.act(lambda e: e.activation(out=dn, in_=ps[5][:, 0:128], func=AF.Abs), r=["ps5"], w=["dn"])
                P.dve(lambda e: e.tensor_tensor(out=dn, in0=dn, in1=emt[hh][:, cs], op=ALU.max),
                      r=["dn", f"emt{hh}"], w=["dn"])
                P.dve(lambda e: e.reciprocal(out=dn, in_=dn), r=["dn"], w=["dn"])
                P.dve(lambda e: e.tensor_tensor(out=hid[hh][:, cs], in0=ps[4][:, 0:128], in1=dn, op=ALU.mult),
                      r=["ps4", "dn"], w=[f"hid{hh}"])
                P.act(lambda e: e.activation(out=wcol[:, hh:hh + 1], in_=Gcol[:, c, hh:hh + 1], func=AF.Exp,
                                             bias=nmu[hh][:, c + 1:c + 2], scale=1.0),
                      r=["Gcol", f"nmu{hh}"], w=["wcol"])
                P.pool(lambda e: e.tensor_scalar(out=kw[:, hp], in0=ktok[:, j, hp], scalar1=wcol[:, hh:hh + 1], scalar2=None,
                                                 op0=ALU.mult),
                       r=["ktok", "wcol"], w=["kw"])
            P.act(lambda e: e.activation(out=carry, in_=pmuq[:, c + 1:c + 2], func=AF.Exp, bias=pmuq[:, c:c + 1], scale=-1.0),
                  r=["pmuq"], w=["carry"])
            for hh in range(2):
                hp = slice(hh * 64, (hh + 1) * 64)
                P.pe(lambda e: e.matmul(ps[6][hp, 0:128], lhsT=kw[:, hp], rhs=vtok[hh][:, j, :], start=True, stop=True),
                     r=["kw", f"vtok{hh}"], w=["ps6"])
            P.pe(lambda e: e.matmul(ps[7][:, 0:128], lhsT=kw, rhs=ones, start=True, stop=True), r=["kw", "ones"], w=["ps7"])
            P.dve(lambda e: e.scalar_tensor_tensor(out=C, in0=C, scalar=carry, in1=ps[6][:, 0:128], op0=ALU.mult, op1=ALU.add),
                  r=["C", "carry", "ps6"], w=["C"])
            P.pool(lambda e: e.tensor_copy(out=Cb, in_=C), r=["C"], w=["Cb"])
            P.dve(lambda e: e.scalar_tensor_tensor(out=Nb, in0=Nb, scalar=carry, in1=ps[7][:, 0:128], op0=ALU.mult, op1=ALU.add),
                  r=["Nb", "carry", "ps7"], w=["Nb"])
            P.pool(lambda e: e.tensor_copy(out=Nbb, in_=Nb), r=["Nb"], w=["Nbb"])
        for hh in range(2):
            P.act(lambda e: e.activation(out=hsq, in_=hid[hh], func=AF.Square), r=[f"hid{hh}"], w=["hsq"])
            pb = pbank()
            P.pe(lambda e: e.matmul(ps[pb], lhsT=ones, rhs=hsq, start=True, stop=True), r=["ones", "hsq"], w=[f"ps{pb}"])
            P.act(lambda e: e.activation(out=sd, in_=ps[pb], func=AF.Sqrt, bias=EPS, scale=1.0 / 128), r=[f"ps{pb}"], w=["sd"])
            P.dve(lambda e: e.reciprocal(out=rstd, in_=sd), r=["sd"], w=["rstd"])
            P.dve(lambda e: e.scalar_tensor_tensor(out=rstd, in0=rstd, scalar=consts[:, 10 + hh:11 + hh], in1=sigo[hh],
                                                   op0=ALU.mult, op1=ALU.mult),
                  r=["rstd", "consts", f"sigo{hh}"], w=["rstd"])
            P.dve(lambda e: e.tensor_tensor(out=ho[hh], in0=hid[hh], in1=rstd, op=ALU.mult), r=[f"hid{hh}", "rstd"], w=[f"ho{hh}"])
            P.dma("sp", o_out[hh * 128:(hh + 1) * 128, tsl(t)], ho[hh], "oout", r=[f"ho{hh}"])

import math

D = 1024
S = 8192
TT = 512
NT = S // TT
LR = 64
NJ = TT // LR
NCR = S // LR
GN_EPS = 64e-5
DEC = math.exp(-0.5)


def emit_rwk(CX, io, nt=NT):
    nc = CX.nc
    P = CX.P
    hsrc = io["hsrc"]
    wbig_in = io["wbig"]
    w2c_in = io["w2c"]
    g2c_in = io["g2c"]
    mu_in = io["mu"]
    cvec_in = io["cvec"]
    lnb_in = io["lnwb"]
    cf_in = io["cf32"]
    o_out = io["o_out"]

    sb = CX.sb
    stage = sb("stage", [128, 4096], F32).ap()
    wbig = sb("wbig_sb", [128, 8, 1024], BF16).ap()
    w2c = sb("w2c_sb", [64, 512], BF16).ap()
    g2c = sb("g2c_sb", [128, 256], BF16).ap()
    mu = sb("mu_sb", [128, 48], F32).ap()
    cvec = sb("cvec_sb", [128, 16], F32).ap()
    lnwb = sb("lnwb_sb", [128, 256], F32).ap()
    cf = sb("cf_sb", [128, 1025], F32).ap()
    mask320 = cf[:, 0:320]
    identS = cf[:, 320:384]
    onescol = cf[:, 384:385]
    blockones = cf[:, 385:513]
    keep = cf[:, 513:1025]
    hT = [sb(f"hT{i}", [128, 8, 1 + TT], BF16).ap() for i in range(2)]
    xx = sb("xx", [128, 8, TT], F32).ap()
    xm = [sb(f"xm{i}", [128, 8, TT], BF16).ap() for i in range(2)]
    lora = sb("lora", [128, TT], BF16).ap()
    sg = sb("sg", [128, TT], BF16).ap()
    names = ["r", "k", "v", "lg", "ag", "kk", "km", "lp", "tmp", "tmp2", "bt", "kt", "rkr", "ssq"]
    A_ = [{n: sb(f"{n}{p}", [128, TT], F32).ap() for n in names} for p in range(2)]
    ar = [sb(f"ar{p}", [128, NJ, 128], F32).ap() for p in range(2)]
    S0T = [sb(f"S0T{p}", [128, 64], F32).ap() for p in range(2)]
    AMs = [sb(f"AM{p}", [128, 320], F32).ap() for p in range(2)]
    bkPs = [sb(f"bkP{p}", [128, 128], F32).ap() for p in range(2)]
    toks = [sb(f"tok{p}", [128, 192], F32).ap() for p in range(2)]
    Ys = [sb(f"Y{p}", [128, 64], F32).ap() for p in range(2)]
    Ans = [[sb(f"An{p}_{i}", [128, 128], F32).ap() for i in range(2)] for p in range(2)]
    st6s = [sb(f"st6{p}", [128, 6], F32).ap() for p in range(2)]
    mvs = [sb(f"mv{p}", [128, 2], F32).ap() for p in range(2)]
    rss = [sb(f"rs{p}", [128, 1], F32).ap() for p in range(2)]
    bscs = [sb(f"bsc{p}", [128, 1], F32).ap() for p in range(2)]
    yns = [sb(f"yn{p}", [128, 64], F32).ap() for p in range(2)]
    yos = [sb(f"yo{p}", [128, 64], F32).ap() for p in range(2)]
    ofm = [sb(f"ofm{p}", [128, TT], BF16).ap() for p in range(2)]
    ps = CX.ps

    def tsl(t):
        return slice(t * TT, (t + 1) * TT)

    for p in range(2):
        P.pool(lambda e: e.memset(S0T[p], 0.0), w=[f"S0T{p}"])
    P.pool(lambda e: e.memset(hT[1][:, :, 0:1], 0.0), w=["hT1"])
    P.dma("sp", mu, mu_in, "mu", w=["mu"])
    P.dma("sp", cvec, cvec_in, "cvec", w=["cvec"])
    P.dma("sp", lnwb, lnb_in, "lnwb", w=["lnwb"])
    P.dma("sp", cf, cf_in, "cf", w=["cf"])
    for hf in range(2):
        P.dma("sp", stage.rearrange("p (c f) -> p c f", c=4), wbig_in[:, 4 * hf:4 * hf + 4, :], "stage", w=["stage"])
        P.pool(lambda e: e.tensor_copy(out=wbig[:, 4 * hf:4 * hf + 4, :].rearrange("p c f -> p (c f)"), in_=stage),
               r=["stage"], w=["wbig"])
    P.dma("sp", stage[0:64, 0:512], w2c_in, "stage", w=["stage"])
    P.pool(lambda e: e.tensor_copy(out=w2c, in_=stage[0:64, 0:512]), r=["stage"], w=["w2c"])
    P.dma("sp", stage[:, 0:256], g2c_in, "stage", w=["stage"])
    P.pool(lambda e: e.tensor_copy(out=g2c, in_=stage[:, 0:256]), r=["stage"], w=["g2c"])

    pcnt = [0]

    def pbank():
        pcnt[0] += 1
        return pcnt[0] % 2

    xcnt = [0]

    def mix(j, hb):
        xcnt[0] += 1
        b = xcnt[0] % 2
        for dc in range(8):
            P.dve(lambda e: e.scalar_tensor_tensor(out=xm[b][:, dc, :], in0=xx[:, dc, :], scalar=mu[:, j * 8 + dc:j * 8 + dc + 1],
                                                   in1=hT[hb][:, dc, 1:1 + TT], op0=ALU.mult, op1=ALU.add),
                  r=["xx", "mu", f"hT{hb}"], w=[f"xm{b}"])
        return b

    def proj(b, c0, m, hb):
        pb = pbank()
        for dc in range(8):
            P.pe(lambda e: e.matmul(ps[pb][0:m, :], lhsT=wbig[:, dc, c0:c0 + m], rhs=xm[b][:, dc, :],
                                    start=(dc == 0), stop=(dc == 7)),
                 r=["wbig", f"xm{b}"], w=[f"ps{pb}"])
        return pb

    for t in range(nt):
        hb = t % 2
        ob = 1 - hb
        hsrc(hT[hb][:, :, 1:1 + TT], t, f"hT{hb}", [f"hT{hb}"])
        if t > 0:
            P.pool(lambda e: e.tensor_copy(out=hT[hb][:, :, 0:1], in_=hT[ob][:, :, TT:TT + 1]), r=[f"hT{ob}"], w=[f"hT{hb}"])
        else:
            P.pool(lambda e: e.memset(hT[hb][:, :, 0:1], 0.0), w=[f"hT{hb}"])
        P.dve(lambda e: e.tensor_tensor(out=xx, in0=hT[hb][:, :, 0:TT], in1=hT[hb][:, :, 1:1 + TT], op=ALU.subtract),
              r=[f"hT{hb}"], w=["xx"])
        for j, nm, c0 in ((0, "r", 0), (2, "k", 256), (3, "v", 512)):
            b = mix(j, hb)
            for p in range(2):
                pb = proj(b, c0 + 128 * p, 128, hb)
                P.act(lambda e: e.copy(out=A_[p][nm], in_=ps[pb]), r=[f"ps{pb}"], w=[f"{nm}{p}"])
        b = mix(1, hb)
        pb = proj(b, 768, 64, hb)
        P.act(lambda e: e.activation(out=lora[0:64, :], in_=ps[pb][0:64, :], func=AF.Tanh), r=[f"ps{pb}"], w=["lora"])
        for p in range(2):
            pb = pbank()
            P.pe(lambda e: e.matmul(ps[pb], lhsT=w2c[:, 128 * p:128 * p + 128], rhs=lora[0:64, :], start=True, stop=True),
                 r=["w2c", "lora"], w=[f"ps{pb}"])
            P.act(lambda e: e.activation(out=A_[p]["lg"], in_=ps[pb], func=AF.Sigmoid, bias=cvec[:, 8 * p:8 * p + 1]),
                  r=[f"ps{pb}", "cvec"], w=[f"lg{p}"])
            P.pool(lambda e: e.tensor_scalar(out=A_[p]["lg"], in0=A_[p]["lg"], scalar1=-DEC, scalar2=None, op0=ALU.mult),
                   r=[f"lg{p}"], w=[f"lg{p}"])
        b = mix(4, hb)
        pb = proj(b, 832, 64, hb)
        P.act(lambda e: e.copy(out=lora[0:64, :], in_=ps[pb][0:64, :]), r=[f"ps{pb}"], w=["lora"])
        for p in range(2):
            pb = pbank()
            P.pe(lambda e: e.matmul(ps[pb], lhsT=w2c[:, 256 + 128 * p:256 + 128 * p + 128], rhs=lora[0:64, :], start=True, stop=True),
                 r=["w2c", "lora"], w=[f"ps{pb}"])
            P.act(lambda e: e.activation(out=A_[p]["ag"], in_=ps[pb], func=AF.Sigmoid, bias=cvec[:, 8 * p + 1:8 * p + 2]),
                  r=[f"ps{pb}", "cvec"], w=[f"ag{p}"])
        b = mix(5, hb)
        pb = proj(b, 896, 128, hb)
        P.act(lambda e: e.activation(out=sg, in_=ps[pb], func=AF.Sigmoid), r=[f"ps{pb}"], w=["sg"])

        for p in range(2):
            a = A_[p]
            cv = lambda i: cvec[:, 8 * p + i:8 * p + i + 1]
            R = lambda *n: [f"{x}{p}" for x in n]
            P.pool(lambda e: e.tensor_scalar(out=a["kk"], in0=a["k"], scalar1=cv(2), scalar2=None, op0=ALU.mult),
                   r=R("k") + ["cvec"], w=R("kk"))
            P.pool(lambda e: e.tensor_tensor(out=a["tmp"], in0=a["kk"], in1=a["kk"], op=ALU.mult), r=R("kk"), w=R("tmp"))
            pb = pbank()
            P.pe(lambda e: e.matmul(ps[pb], lhsT=blockones, rhs=a["tmp"], start=True, stop=True), r=["cf"] + R("tmp"), w=[f"ps{pb}"])
            P.act(lambda e: e.activation(out=a["ssq"], in_=ps[pb], func=AF.Sqrt), r=[f"ps{pb}"], w=R("ssq"))
            P.dve(lambda e: e.tensor_scalar_max(out=a["ssq"], in0=a["ssq"], scalar1=1e-12), r=R("ssq"), w=R("ssq"))
            P.dve(lambda e: e.reciprocal(out=a["ssq"], in_=a["ssq"]), r=R("ssq"), w=R("ssq"))
            P.dve(lambda e: e.tensor_tensor(out=a["kk"], in0=a["kk"], in1=a["ssq"], op=ALU.mult), r=R("kk", "ssq"), w=R("kk"))
            P.dve(lambda e: e.tensor_scalar(out=a["km"], in0=a["ag"], scalar1=cv(3), scalar2=cv(4), op0=ALU.mult, op1=ALU.add),
                  r=R("ag") + ["cvec"], w=R("km"))
            P.dve(lambda e: e.tensor_tensor(out=a["km"], in0=a["km"], in1=a["k"], op=ALU.mult), r=R("km", "k"), w=R("km"))
            P.dve(lambda e: e.scalar_tensor_tensor(out=a["rkr"], in0=a["r"], scalar=cv(5), in1=a["km"], op0=ALU.mult, op1=ALU.mult),
                  r=R("r", "km") + ["cvec"], w=R("rkr"))
            P.dve(lambda e: e.tensor_tensor_scan(out=a["lp"], data0=keep, data1=a["lg"], initial=0.0, op0=ALU.mult, op1=ALU.add),
                  r=["cf"] + R("lg"), w=R("lp"))
            arv = ar[p]
            v3 = lambda x: x.rearrange("p (j l) -> p j l", l=LR)
            P.act(lambda e: e.activation(out=a["tmp"], in_=a["lp"], func=AF.Exp), r=R("lp"), w=R("tmp"))
            P.dve(lambda e: e.tensor_tensor(out=arv[:, :, 64:128], in0=v3(a["r"]), in1=v3(a["tmp"]), op=ALU.mult),
                  r=R("r", "tmp"), w=R("ar"))
            P.pool(lambda e: e.tensor_tensor(out=a["tmp2"], in0=a["lp"], in1=a["lg"], op=ALU.subtract), r=R("lp", "lg"), w=R("tmp2"))
            P.act(lambda e: e.activation(out=a["tmp2"], in_=a["tmp2"], func=AF.Exp), r=R("tmp2"), w=R("tmp2"))
            P.dve(lambda e: e.scalar_tensor_tensor(out=arv[:, :, 0:64], in0=v3(a["kk"]), scalar=-1.0, in1=v3(a["tmp2"]),
                                                   op0=ALU.mult, op1=ALU.mult),
                  r=R("kk", "tmp2"), w=R("ar"))
            P.act(lambda e: e.activation(out=a["tmp2"], in_=a["lp"], func=AF.Exp, scale=-1.0), r=R("lp"), w=R("tmp2"))
            P.pool(lambda e: e.tensor_tensor(out=a["bt"], in0=a["kk"], in1=a["ag"], op=ALU.mult), r=R("kk", "ag"), w=R("bt"))
            P.pool(lambda e: e.tensor_tensor(out=a["bt"], in0=a["bt"], in1=a["tmp2"], op=ALU.mult), r=R("bt", "tmp2"), w=R("bt"))
            P.dve(lambda e: e.tensor_tensor(out=a["kt"], in0=a["km"], in1=a["tmp2"], op=ALU.mult), r=R("km", "tmp2"), w=R("kt"))

        if True:
            def chunk(p, j):
                CH = 4 if p == 0 else 0
                SQ = 5 if p == 0 else 1
                a = A_[p]
                cv = lambda i: cvec[:, 8 * p + i:8 * p + i + 1]
                R = lambda *n: [f"{x}{p}" for x in n]
                arv = ar[p]
                AM = AMs[p]
                bkP = bkPs[p]
                tok = toks[p]
                Y = Ys[p]
                An = Ans[p]
                st6 = st6s[p]
                mv = mvs[p]
                rs = rss[p]
                bsc = bscs[p]
                yn = yns[p]
                yo = yos[p]
                c = NJ * t + j
                cs = slice(j * LR, (j + 1) * LR)
                PL = a["tmp"][:, j * LR + LR - 1:j * LR + LR]
                H = [slice(0, 64), slice(64, 128)]
                for hp in H:
                    P.pe(lambda e: e.matmul(ps[2][hp, 0:128], lhsT=a["bt"][hp, cs], rhs=arv[hp, j, :], start=True, stop=True),
                         r=R("bt", "ar"), w=["ps2"])
                    P.pe(lambda e: e.matmul(ps[2][hp, 128:256], lhsT=a["kt"][hp, cs], rhs=arv[hp, j, :], start=True, stop=True),
                         r=R("kt", "ar"), w=["ps2"])
                    P.pe(lambda e: e.matmul(ps[2][hp, 256:320], lhsT=arv[hp, j, 0:64], rhs=a["bt"][hp, cs], start=True, stop=True),
                         r=R("bt", "ar"), w=["ps2"])
                P.dve(lambda e: e.tensor_tensor(out=AM, in0=ps[2][:, 0:320], in1=mask320, op=ALU.mult), r=["ps2", "cf"], w=[f"AM{p}"])
                yield
                P.pool(lambda e: e.tensor_scalar(out=bkP[:, 0:64], in0=a["bt"][:, cs], scalar1=PL, scalar2=None, op0=ALU.mult),
                       r=R("bt", "tmp"), w=[f"bkP{p}"])
                P.pool(lambda e: e.tensor_scalar(out=bkP[:, 64:128], in0=a["kt"][:, cs], scalar1=PL, scalar2=None, op0=ALU.mult),
                       r=R("kt", "tmp"), w=[f"bkP{p}"])
                for hp in H:
                    P.pe(lambda e: e.matmul(ps[3][hp, 0:64], lhsT=a["v"][hp, cs], rhs=identS[hp, :], start=True, stop=True),
                         r=R("v") + ["cf"], w=["ps3"])
                    P.pe(lambda e: e.matmul(ps[3][hp, 64:128], lhsT=bkP[hp, 0:64], rhs=identS[hp, :], start=True, stop=True),
                         r=[f"bkP{p}", "cf"], w=["ps3"])
                    P.pe(lambda e: e.matmul(ps[3][hp, 128:192], lhsT=bkP[hp, 64:128], rhs=identS[hp, :], start=True, stop=True),
                         r=[f"bkP{p}", "cf"], w=["ps3"])
                P.act(lambda e: e.copy(out=tok, in_=ps[3][:, 0:192]), r=["ps3"], w=[f"tok{p}"])
                yield
                vtok = tok[:, 0:64]
                btPtok = tok[:, 64:128]
                ktPtok = tok[:, 128:192]
                for hp in H:
                    P.pe(lambda e: e.matmul(ps[CH][hp, 0:64], lhsT=arv[hp, j, 0:64], rhs=S0T[p][hp, :], start=True, stop=False),
                         r=R("ar", "S0T"), w=[f"ps{CH}"])
                    P.pe(lambda e: e.matmul(ps[CH][hp, 0:64], lhsT=AM[hp, 128:192], rhs=vtok[hp, :], start=False, stop=True),
                         r=[f"AM{p}", f"tok{p}"], w=[f"ps{CH}"])
                P.act(lambda e: e.copy(out=Y, in_=ps[CH][:, 0:64]), r=[f"ps{CH}"], w=[f"Y{p}"])
                yield
                Acur = AM[:, 0:64]
                ATcur = AM[:, 256:320]
                an, atn = f"AM{p}", f"AM{p}"
                for kq in range(6):
                    for hp in H:
                        P.pe(lambda e: e.matmul(ps[CH][hp, 0:64], lhsT=Acur[hp, :], rhs=Y[hp, :], start=True, stop=True),
                             r=[an, f"Y{p}"], w=[f"ps{CH}"])
                    P.dve(lambda e: e.tensor_tensor(out=Y, in0=ps[CH][:, 0:64], in1=Y, op=ALU.add), r=[f"ps{CH}", f"Y{p}"], w=[f"Y{p}"])
                    yield
                    if kq < 5:
                        for hp in H:
                            P.pe(lambda e: e.matmul(ps[SQ][hp, 0:64], lhsT=ATcur[hp, :], rhs=Acur[hp, :], start=True, stop=True),
                                 r=[an], w=[f"ps{SQ}"])
                            P.pe(lambda e: e.matmul(ps[SQ][hp, 64:128], lhsT=Acur[hp, :], rhs=ATcur[hp, :], start=True, stop=True),
                                 r=[an], w=[f"ps{SQ}"])
                        nb = kq % 2
                        P.act(lambda e: e.copy(out=An[nb], in_=ps[SQ][:, 0:128]), r=[f"ps{SQ}"], w=[f"An{p}_{nb}"])
                        yield
                        Acur = An[nb][:, 0:64]
                        ATcur = An[nb][:, 64:128]
                        an = f"An{p}_{nb}"
                for hi, hp in enumerate(H):
                    P.pe(lambda e: e.matmul(ps[6][hp, 0:64], lhsT=arv[hp, j, 64:128], rhs=S0T[p][hp, :], start=True, stop=False),
                         r=R("ar", "S0T"), w=["ps6"])
                    P.pe(lambda e: e.matmul(ps[6][hp, 0:64], lhsT=AM[hp, 64:128], rhs=Y[hp, :], start=False, stop=False),
                         r=[f"AM{p}", f"Y{p}"], w=["ps6"])
                    P.pe(lambda e: e.matmul(ps[6][hp, 0:64], lhsT=AM[hp, 192:256], rhs=vtok[hp, :], start=False, stop=True),
                         r=[f"AM{p}", f"tok{p}"], w=["ps6"])
                    P.pe(lambda e: e.matmul(ps[6][hp, 64:65], lhsT=a["rkr"][hp, cs], rhs=onescol[hp, :], start=True, stop=True),
                         r=R("rkr") + ["cf"], w=["ps6"])
                    P.pe(lambda e: e.matmul(ps[6][hp, 128:192], lhsT=sg[:, cs], rhs=g2c[:, 128 * p + 64 * hi:128 * p + 64 * hi + 64],
                                            start=True, stop=True),
                         r=["sg", "g2c"], w=["ps6"])
                for hp in H:
                    P.pe(lambda e: e.matmul(ps[7][hp, 0:64], lhsT=btPtok[hp, :], rhs=Y[hp, :], start=True, stop=False),
                         r=[f"tok{p}", f"Y{p}"], w=["ps7"])
                    P.pe(lambda e: e.matmul(ps[7][hp, 0:64], lhsT=ktPtok[hp, :], rhs=vtok[hp, :], start=False, stop=True),
                         r=[f"tok{p}"], w=["ps7"])
                P.dve(lambda e: e.scalar_tensor_tensor(out=S0T[p], in0=S0T[p], scalar=PL, in1=ps[7][:, 0:64], op0=ALU.mult, op1=ALU.add),
                      r=R("S0T", "tmp") + ["ps7"], w=R("S0T"))
                P.dve(lambda e: e.bn_stats(out=st6, in_=ps[6][:, 0:64]), r=["ps6"], w=[f"st6{p}"])
                P.dve(lambda e: e.bn_aggr(out=mv, in_=st6), r=[f"st6{p}"], w=[f"mv{p}"])
                P.act(lambda e: e.activation(out=rs, in_=mv[:, 1:2], func=AF.Sqrt, bias=GN_EPS), r=[f"mv{p}"], w=[f"rs{p}"])
                P.dve(lambda e: e.reciprocal(out=rs, in_=rs), r=[f"rs{p}"], w=[f"rs{p}"])
                P.dve(lambda e: e.tensor_scalar(out=yn, in0=ps[6][:, 0:64], scalar1=mv[:, 0:1], scalar2=rs, op0=ALU.subtract, op1=ALU.mult),
                      r=["ps6", f"mv{p}", f"rs{p}"], w=[f"yn{p}"])
                P.pool(lambda e: e.tensor_tensor(out=yn, in0=yn, in1=lnwb[:, 128 * p:128 * p + 64], op=ALU.mult), r=[f"yn{p}", "lnwb"], w=[f"yn{p}"])
                P.pool(lambda e: e.tensor_tensor(out=yn, in0=yn, in1=lnwb[:, 128 * p + 64:128 * p + 128], op=ALU.add), r=[f"yn{p}", "lnwb"], w=[f"yn{p}"])
                P.act(lambda e: e.copy(out=bsc, in_=ps[6][:, 64:65]), r=["ps6"], w=[f"bsc{p}"])
                P.dve(lambda e: e.scalar_tensor_tensor(out=yn, in0=vtok, scalar=bsc, in1=yn, op0=ALU.mult, op1=ALU.add),
                      r=[f"tok{p}", f"bsc{p}", f"yn{p}"], w=[f"yn{p}"])
                P.dve(lambda e: e.tensor_tensor(out=yo, in0=ps[6][:, 128:192], in1=yn, op=ALU.mult),
                      r=["ps6", f"yn{p}"], w=[f"yo{p}"])
                for hp in H:
                    P.pe(lambda e: e.matmul(ps[3][hp, 256:320], lhsT=yo[hp, :], rhs=identS[hp, :], start=True, stop=True),
                         r=[f"yo{p}", "cf"], w=["ps3"])
                P.act(lambda e: e.copy(out=ofm[p][:, cs], in_=ps[3][:, 256:320]), r=["ps3"], w=[f"ofm{p}"])
        for j in range(NJ):
            gens = [chunk(0, j), chunk(1, j)]
            while gens:
                for g in list(gens):
                    try:
                        next(g)
                    except StopIteration:
                        gens.remove(g)
        for p in range(2):
            P.dma("sp", o_out[128 * p:128 * p + 128, tsl(t)], ofm[p], "oout", r=[f"ofm{p}"])

import ml_dtypes as _mld

_PROGS = {}


def _prog(key, fn):
    if key not in _PROGS:
        _PROGS[key] = fn()
    return _PROGS[key]


def _gl(g):
    return np.ascontiguousarray(np.asarray(g, np.float32).reshape(8, 128).T)


def _mla_inputs(z, j, hT_b, pos_b, c):
    hp = c % 4
    wa = z['mla_w_a'][j]
    sw = np.concatenate([np.arange(32, 64), np.arange(0, 32)])
    wa_ext = np.concatenate([wa, wa[:, 768:832][:, sw]], axis=1)
    wqb = z['mla_w_qb'][j]
    wkvb = z['mla_w_kvb'][j]
    cols = []
    for hh in range(2):
        hd = 2 * hp + hh
        blk = wqb[:, hd * 192:(hd + 1) * 192]
        cols.append(np.concatenate([blk[:, :128], blk[:, 128:192], blk[:, 128:192][:, sw]], axis=1))
    wqb_c = np.concatenate(cols, axis=1)
    wkvb_c = np.concatenate([wkvb[:, (2 * hp + hh) * 256:(2 * hp + hh + 1) * 256] for hh in range(2)], axis=1)
    consts = np.zeros((128, 8), np.float32)
    consts[:, 0:4] = z['mla_q_norm'][j].reshape(4, 128).T
    consts[:, 4:6] = z['mla_kv_norm'][j].reshape(2, 128).T
    inv = 1.0 / (10000.0 ** (np.arange(0, 64, 2, dtype=np.float32) / 64))
    consts[0:64, 6] = np.concatenate([inv, inv]) / (2 * math.pi)
    consts[0:64, 7] = np.concatenate([-np.ones(32), np.ones(32)])
    k = np.arange(128)[:, None]
    q = np.arange(512)[None, :]
    masks = np.stack([((jj * 128 + k) <= q) for jj in range(4)], axis=1).astype(_mld.bfloat16)
    return {'h_in': hT_b, 'pos': np.ascontiguousarray(pos_b.reshape(1, -1).astype(np.int32)),
            'wa': np.ascontiguousarray(wa_ext), 'wqb': np.ascontiguousarray(wqb_c),
            'wkvb': np.ascontiguousarray(wkvb_c), 'consts': consts, 'masks': np.ascontiguousarray(masks)}


def _mls_inputs(z, j, hT_b, c):
    hp = c % 4
    hA, hB = 2 * hp, 2 * hp + 1
    W = z['ml_w_in'][j]
    cols = np.concatenate([np.arange(hA * 64, hA * 64 + 128), 512 + np.arange(hA * 64, hA * 64 + 128),
                           1024 + np.arange(hA * 128, hA * 128 + 256), 2048 + np.arange(hA * 128, hA * 128 + 256),
                           np.array([3072 + hA, 3072 + hB, 3080 + hA, 3080 + hB])])
    w_c = np.ascontiguousarray(W[:, cols])
    consts = np.zeros((128, 16), np.float32)
    cw = z['ml_conv_w'][j]
    cb = z['ml_conv_b'][j]
    consts[:, 0:4] = cw[:, hA * 64:hA * 64 + 128].T
    consts[:, 4:8] = cw[:, 512 + hA * 64:512 + hA * 64 + 128].T
    consts[:, 8] = cb[hA * 64:hA * 64 + 128]
    consts[:, 9] = cb[512 + hA * 64:512 + hA * 64 + 128]
    on = z['ml_out_norm'][j]
    consts[:, 10] = on[hA * 128:(hA + 1) * 128]
    consts[:, 11] = on[hB * 128:(hB + 1) * 128]
    bif = z['ml_b_if'][j]
    consts[0:2, 12] = bif[[hA, hB]]
    consts[0:2, 13] = -bif[[8 + hA, 8 + hB]]
    cm = np.zeros((128, 256), np.float32)
    cm[:, 0:128] = np.eye(128)
    s = np.arange(128)[:, None]
    t = np.arange(128)[None, :]
    cm[:, 128:256] = (s <= t)
    sel = np.zeros((2, 386), np.float32)
    sel[0, 0:128] = 1
    sel[1, 128:256] = 1
    sel[0, 256:320] = 1
    sel[1, 320:384] = 1
    sel[0, 384] = 1
    sel[1, 385] = 1
    return {'h_in': hT_b, 'w': w_c, 'consts': consts, 'cm': cm.astype(_mld.bfloat16), 'sel': sel}


def _rwk_consts():
    cf = np.zeros((128, 1025), np.float32)
    s = np.arange(64)[:, None]
    t = np.arange(64)[None, :]
    su = (s < t).astype(np.float32)
    ui = (s <= t).astype(np.float32)
    m = np.concatenate([su, ui, su, ui, su.T], axis=1)
    cf[:, 0:320] = np.concatenate([m, m], axis=0)
    cf[:, 320:384] = np.concatenate([np.eye(64), np.eye(64)], axis=0)
    cf[:, 384] = 1.0
    bo = np.zeros((128, 128), np.float32)
    bo[:64, :64] = 1
    bo[64:, 64:] = 1
    cf[:, 385:513] = bo
    keep = np.ones(512, np.float32)
    keep[::64] = 0
    cf[:, 513:1025] = keep[None, :]
    return cf


def _rwk_inputs(z, j, hT_b, c):
    q = c % 4
    ch = slice(q * 256, (q + 1) * 256)
    wbig = np.concatenate([z['rw_w_r'][j][:, ch], z['rw_w_k'][j][:, ch], z['rw_w_v'][j][:, ch],
                           z['rw_w1'][j], z['rw_a1'][j], z['rw_g1'][j]], axis=1)
    w2c = np.concatenate([z['rw_w2'][j][:, ch], z['rw_a2'][j][:, ch]], axis=1)
    g2c = z['rw_g2'][j][:, ch]
    mu = z['rw_mu'][j]
    mu_l = np.ascontiguousarray(mu.reshape(6, 8, 128).transpose(2, 0, 1).reshape(128, 48))
    cvec = np.zeros((128, 16), np.float32)
    rk = z['rw_r_k'][j].reshape(-1)
    one = np.ones(128, np.float32)
    for p in range(2):
        cc = slice(q * 256 + p * 128, q * 256 + (p + 1) * 128)
        cvec[:, 8 * p + 0] = z['rw_w0'][j][cc]
        cvec[:, 8 * p + 1] = z['rw_a0'][j][cc]
        cvec[:, 8 * p + 2] = z['rw_k_k'][j][cc]
        cvec[:, 8 * p + 3] = z['rw_k_a'][j][cc]
        cvec[:, 8 * p + 4] = 1.0 - z['rw_k_a'][j][cc]
        cvec[:, 8 * p + 5] = rk[cc]
    lnwb = np.zeros((128, 256), np.float32)
    for p in range(2):
        for h in range(2):
            cc = slice(q * 256 + p * 128 + h * 64, q * 256 + p * 128 + (h + 1) * 64)
            lnwb[h * 64:(h + 1) * 64, p * 128:p * 128 + 64] = z['rw_ln_w'][j][cc][None, :]
            lnwb[h * 64:(h + 1) * 64, p * 128 + 64:p * 128 + 128] = z['rw_ln_b'][j][cc][None, :]
    return {'h_in': hT_b, 'wbig': np.ascontiguousarray(wbig), 'w2c': np.ascontiguousarray(w2c),
            'g2c': np.ascontiguousarray(g2c), 'mu': mu_l, 'cvec': cvec, 'lnwb': lnwb, 'cf32': _rwk_consts()}


def _build_fused(nl=4):
    nc = bass.Bass("TRN2", target_bir_lowering=False)
    C = Ctx(nc)
    P = C.P
    dt = nc.dram_tensor
    RG = [[0, 1, 2, 3], [4, 5, 6, 7]]
    U32 = mybir.dt.uint32
    fm = lambda ap: ap.rearrange("(c p) t -> p c t", p=128)
    x_ext = fm(dt("x_in", [1024, 2048], F32, kind="ExternalInput").ap())
    y_ext = fm(dt("y_out", [1024, 2048], F32, kind="ExternalOutput").ap())
    idx_in = dt("idx", [128, 8], U32, kind="ExternalInput").ap()
    xbuf = fm(dt("xbuf", [1024, 2048], F32).ap())
    hbuf_t = dt("hbuf", [1024, 2048], BF16).ap()
    hall_t = dt("hall", [4096, 2048], BF16).ap()
    opart_t = dt("opart", [256, 8192], BF16).ap()
    oall_t = dt("oall", [1024, 8192], BF16).ap()
    hall_v = hall_t.rearrange("(k q c p) f -> q p k c f", k=4, q=4, p=128)
    oall_rows = oall_t.rearrange("r (q f) -> (r q) f", q=4)

    def hsrc(dst, t, key, wn):
        for k in range(4):
            P.dma("sp", dst[:, 2 * k:2 * k + 2, :], hall_v[t // 4][:, k, :, (t % 4) * 512:(t % 4 + 1) * 512], key, w=wn)

    def ffw_decl(l, i):
        wv = lambda ap: ap.rearrange("(c p) f -> p c f", p=128)
        return (wv(dt(f"wg_{l}_{i}", [1024, 2816], F32, kind="ExternalInput").ap()),
                wv(dt(f"wu_{l}_{i}", [1024, 2816], F32, kind="ExternalInput").ap()),
                wv(dt(f"wd_{l}_{i}", [2816, 1024], F32, kind="ExternalInput").ap()))

    def gather_h():
        P.barrier()
        for k in range(4):
            P.op("pool", lambda e: e.collective_compute("AllGather", ALU.bypass, replica_groups=RG,
                                                         ins=[hbuf_t[k * 256:(k + 1) * 256, :].opt()],
                                                         outs=[hall_t[k * 1024:(k + 1) * 1024, :].opt()]),
                 r=[], w=[f"hall{k}"], dma="cc", inc=1)
        P.barrier()
        C.reset()

    def gather_o():
        P.barrier()
        for k in range(4):
            P.op("pool", lambda e: e.collective_compute("AllGather", ALU.bypass, replica_groups=RG,
                                                         ins=[opart_t[k * 64:(k + 1) * 64, :].opt()],
                                                         outs=[oall_t[k * 256:(k + 1) * 256, :].opt()]),
                 r=[], w=[f"oall{k}"], dma="cc", inc=1)
        P.barrier()
        C.reset()

    g0 = dt("gains_a", [128, 16], F32, kind="ExternalInput").ap()
    emit_tp(C, {"x_in": x_ext, "ffw": [ffw_decl(0, 0)], "gains": g0, "x_out": xbuf, "h_out": fm(hbuf_t)}, 1, False, True, False)
    gather_h()
    for l in range(nl):
        kind = l % 3
        wv = lambda ap: ap.rearrange("(c p) f -> p c f", p=128)
        if kind == 0:
            io = {"hsrc": hsrc, "pos": dt(f"m{l}_pos", [1, 8192], I32, kind="ExternalInput").ap(),
                  "wa": wv(dt(f"m{l}_wa", [1024, 896], F32, kind="ExternalInput").ap()),
                  "wqb": wv(dt(f"m{l}_wqb", [512, 512], F32, kind="ExternalInput").ap()),
                  "wkvb": wv(dt(f"m{l}_wkvb", [256, 512], F32, kind="ExternalInput").ap()),
                  "consts": dt(f"m{l}_consts", [128, 8], F32, kind="ExternalInput").ap(),
                  "masks": dt(f"m{l}_masks", [128, 4, 512], BF16, kind="ExternalInput").ap(),
                  "o_out": opart_t}
            emit_mla(C, io)
        elif kind == 1:
            io = {"hsrc": hsrc, "w": wv(dt(f"m{l}_w", [1024, 772], F32, kind="ExternalInput").ap()),
                  "consts": dt(f"m{l}_consts", [128, 16], F32, kind="ExternalInput").ap(),
                  "cm": dt(f"m{l}_cm", [128, 256], BF16, kind="ExternalInput").ap(),
                  "sel": dt(f"m{l}_sel", [2, 386], F32, kind="ExternalInput").ap(),
                  "o_out": opart_t}
            emit_mls(C, io)
        else:
            io = {"hsrc": hsrc, "wbig": wv(dt(f"m{l}_wbig", [1024, 1024], F32, kind="ExternalInput").ap()),
                  "w2c": dt(f"m{l}_w2c", [64, 512], F32, kind="ExternalInput").ap(),
                  "g2c": dt(f"m{l}_g2c", [128, 256], F32, kind="ExternalInput").ap(),
                  "mu": dt(f"m{l}_mu", [128, 48], F32, kind="ExternalInput").ap(),
                  "cvec": dt(f"m{l}_cvec", [128, 16], F32, kind="ExternalInput").ap(),
                  "lnwb": dt(f"m{l}_lnwb", [128, 256], F32, kind="ExternalInput").ap(),
                  "cf32": dt(f"m{l}_cf32", [128, 1025], F32, kind="ExternalInput").ap(),
                  "o_out": opart_t}
            emit_rwk(C, io)
        gather_o()
        wo = wv(dt(f"wo_{l}", [1024, 1024], F32, kind="ExternalInput").ap())
        if l < nl - 1:
            gk = dt(f"gains_{l}", [128, 24], F32, kind="ExternalInput").ap()
            emit_tp(C, {"x_in": xbuf, "oall": oall_rows, "wo": wo, "idx": idx_in, "ffw": [ffw_decl(l, 1), ffw_decl(l + 1, 0)],
                        "gains": gk, "x_out": xbuf, "h_out": fm(hbuf_t)}, 2, True, True, False)
            gather_h()
        else:
            gk = dt(f"gains_{l}", [128, 16], F32, kind="ExternalInput").ap()
            emit_tp(C, {"x_in": xbuf, "oall": oall_rows, "wo": wo, "idx": idx_in, "ffw": [ffw_decl(l, 1)],
                        "gains": gk, "y_out": y_ext}, 1, True, False, True)
    P.emit()
    return nc


def kernel(nl=4, **z):
    z = {k: np.asarray(v) for k, v in z.items()}
    cores = list(range(8))
    x = z['x'].astype(np.float32)
    nc = _prog(('fused', nl), lambda: _build_fused(nl))
    ims = []
    pp = np.arange(128)[:, None]
    cc = np.arange(8)[None, :]
    for c in cores:
        b, q = c // 4, c % 4
        m = {'x_in': np.ascontiguousarray(x[b, q * 2048:(q + 1) * 2048, :].T),
             'idx': (((((cc * 128 + pp) % 256) // 64) * 256 + ((cc * 128 + pp) // 256) * 64 + (cc * 128 + pp) % 64) * 4 + q).astype(np.uint32)}
        for l in range(nl):
            for i in range(2):
                if l == nl - 1 and i == 1 and False:
                    continue
                m[f'wg_{l}_{i}'] = z['ffn_w_gate'][l, i]
                m[f'wu_{l}_{i}'] = z['ffn_w_up'][l, i]
                m[f'wd_{l}_{i}'] = z['ffn_w_down'][l, i]
        m['gains_a'] = np.concatenate([_gl(z['ffn_norm'][0, 0]), _gl(z['mix_norm'][0])], axis=1)
        dummy_h = None
        for l in range(nl):
            kind, j = l % 3, l // 3
            if kind == 0:
                d = _mla_inputs(z, j, dummy_h, z['positions'][b], c)
                wo = z['mla_w_o'][j]
            elif kind == 1:
                d = _mls_inputs(z, j, dummy_h, c)
                wo = z['ml_w_o'][j]
            else:
                d = _rwk_inputs(z, j, dummy_h, c)
                wo = z['rw_w_o'][j]
            for k, v in d.items():
                if k != 'h_in':
                    m[f'm{l}_{k}'] = v
            m[f'wo_{l}'] = np.ascontiguousarray(wo)
            if l < nl - 1:
                m[f'gains_{l}'] = np.concatenate([_gl(z['ffn_norm'][l, 1]), _gl(z['ffn_norm'][l + 1, 0]),
                                                  _gl(z['mix_norm'][l + 1])], axis=1)
            else:
                m[f'gains_{l}'] = np.concatenate([_gl(z['ffn_norm'][l, 1]), _gl(z['final_norm'])], axis=1)
        ims.append(m)
    res = run_bass_kernel_spmd(nc, ims, core_ids=cores)
    out = np.zeros((2, 8192, 1024), np.float32)
    for c in cores:
        out[c // 4, (c % 4) * 2048:(c % 4 + 1) * 2048, :] = res.results[c]['y_out'].T
    return out
```
